# Optimizing a Trainium2 kernel written in Bass

```python
import math
import jax
import jax.numpy as jnp
from jax import lax
import numpy as np

D_MODEL = 1024
BATCH = 8
SEQ = 4096
DEPTH = 2

CHUNK = 64
N_EVEN = (DEPTH + 1) // 2
N_ODD = DEPTH // 2
RMS_EPS = 1e-6

S5_WIDTH = D_MODEL // 2
S5_GROUP = 16
S5_GROUPS = S5_WIDTH // S5_GROUP
S5_STATE = 64
SB_HEAD_DIM = 64
SB_WIDTH = D_MODEL // 2
SB_HEADS = SB_WIDTH // SB_HEAD_DIM
SB_QBLOCK = 128
EVEN_IN = S5_WIDTH + 3 * SB_WIDTH
EVEN_MIX = S5_WIDTH + SB_WIDTH

RW_WIDTH = D_MODEL // 2
RW_HEAD = 64
RW_HEADS = RW_WIDTH // RW_HEAD
RW_DECAY_LORA = 64
RW_AAA_LORA = 64
RW_GATE_LORA = 128
RW_IN = 3 * RW_WIDTH + RW_DECAY_LORA + RW_AAA_LORA + RW_GATE_LORA
RW_LN_EPS = 64e-5

M2_INNER = D_MODEL // 2
M2_HEADDIM = 64
M2_HEADS = M2_INNER // M2_HEADDIM
M2_GROUPS = 2
M2_STATE = 128
M2_CONV = 4
M2_CONV_DIM = M2_INNER + 2 * M2_GROUPS * M2_STATE
M2_IN = M2_INNER + M2_CONV_DIM + M2_HEADS
ODD_IN = RW_IN + M2_IN
ODD_MIX = RW_WIDTH + M2_INNER

MOE_GROUPS = 4
MOE_PER_GROUP = 8
MOE_EXPERTS = MOE_GROUPS * MOE_PER_GROUP
MOE_TOPK = 2
MOE_HIDDEN = 512
MOE_BLOCK = 128

kernel_name = 'hybrid_s5_stickbreak_rwkv7_ssd_hmoe'


def rmsnorm(x, w):
    xf = x.astype(jnp.float32)
    xf = xf * lax.rsqrt(jnp.mean(xf * xf, axis=-1, keepdims=True) + RMS_EPS)
    return xf.astype(x.dtype) * w


def modulate(h, shift, scale):
    return h * (1.0 + scale[:, None, :]) + shift[:, None, :]


def token_shift(z):
    return jnp.pad(z[:, :-1], ((0, 0), (1, 0), (0, 0)))


def _cmul(ar, ai, br, bi):
    return ar * br - ai * bi, ar * bi + ai * br


def s5_mixer(u, a_re, a_im, log_dt, b_re, b_im, c_re, c_im, d_skip, w_glu):
    f32 = jnp.float32
    bsz, seq, _ = u.shape
    a_re = jnp.minimum(a_re.astype(f32), -1e-4)
    a_im = a_im.astype(f32)
    dt = jnp.exp(log_dt.astype(f32))[:, None]
    mag = jnp.exp(dt * a_re)
    abar_re, abar_im = mag * jnp.cos(dt * a_im), mag * jnp.sin(dt * a_im)
    den = a_re * a_re + a_im * a_im
    num_re, num_im = abar_re - 1.0, abar_im
    coef_re = (num_re * a_re + num_im * a_im) / den
    coef_im = (num_im * a_re - num_re * a_im) / den
    bb_re, bb_im = _cmul(coef_re[..., None], coef_im[..., None], b_re.astype(f32), b_im.astype(f32))
    c_re = c_re.astype(f32)
    c_im = c_im.astype(f32)
    d_skip = d_skip.astype(f32)
    n_chunks = seq // CHUNK
    uc = u.astype(f32).reshape(bsz, n_chunks, CHUNK, S5_GROUPS, S5_GROUP).transpose(1, 0, 2, 3, 4)

    def combine(e1, e2):
        ar1, ai1, br1, bi1 = e1
        ar2, ai2, br2, bi2 = e2
        ar, ai = _cmul(ar2, ai2, ar1, ai1)
        tr, ti = _cmul(ar2, ai2, br1, bi1)
        return ar, ai, tr + br2, ti + bi2

    def chunk_step(carry, u_c):
        h_re, h_im = carry
        bu_re = jnp.einsum('blgp,gnp->blgn', u_c, bb_re)
        bu_im = jnp.einsum('blgp,gnp->blgn', u_c, bb_im)
        ar = jnp.broadcast_to(abar_re, bu_re.shape)
        ai = jnp.broadcast_to(abar_im, bu_re.shape)
        pa_re, pa_im, s_re, s_im = lax.associative_scan(combine, (ar, ai, bu_re, bu_im), axis=1)
        cr, ci = _cmul(pa_re, pa_im, h_re[:, None], h_im[:, None])
        s_re = s_re + cr
        s_im = s_im + ci
        y = (jnp.einsum('blgn,gpn->blgp', s_re, c_re)
             - jnp.einsum('blgn,gpn->blgp', s_im, c_im)
             + d_skip * u_c)
        return (s_re[:, -1], s_im[:, -1]), y

    zeros = jnp.zeros((bsz, S5_GROUPS, S5_STATE), f32)
    _, ys = lax.scan(chunk_step, (zeros, zeros), uc)
    y = jax.nn.gelu(ys.transpose(1, 0, 2, 3, 4).reshape(bsz, seq, S5_WIDTH)).astype(u.dtype)
    g = y @ w_glu
    return g[..., :S5_WIDTH] * jax.nn.sigmoid(g[..., S5_WIDTH:])


def stick_breaking_attention(q, k, v):
    seq = q.shape[1]
    q, k, v = (jnp.swapaxes(z, 1, 2) for z in (q, k, v))
    scale = SB_HEAD_DIM ** -0.5
    outs = []
    for blk in range(seq // SB_QBLOCK):
        s0 = blk * SB_QBLOCK
        s1 = s0 + SB_QBLOCK
        z = jnp.einsum('bhqd,bhkd->bhqk', q[:, :, s0:s1], k[:, :, :s1]).astype(jnp.float32) * scale
        mask = jnp.arange(s1)[None, :] < jnp.arange(s0, s1)[:, None]
        log_keep = jnp.where(mask, jax.nn.log_sigmoid(-z), 0.0)
        rc = lax.cumsum(log_keep, axis=3, reverse=True)
        suffix = jnp.concatenate([rc[..., 1:], jnp.zeros_like(rc[..., :1])], axis=-1)
        w = jnp.where(mask, jnp.exp(jax.nn.log_sigmoid(z) + suffix), 0.0)
        outs.append(jnp.einsum('bhqk,bhkd->bhqd', w.astype(v.dtype), v[:, :, :s1]))
    return jnp.swapaxes(jnp.concatenate(outs, axis=2), 1, 2)


def even_mixer(h, w_in, w_out, a_re, a_im, log_dt, b_re, b_im, c_re, c_im, d_skip, w_glu):
    bsz, seq, _ = h.shape
    proj = h @ w_in
    u = proj[..., :S5_WIDTH]
    q, k, v = (proj[..., S5_WIDTH + i * SB_WIDTH:S5_WIDTH + (i + 1) * SB_WIDTH]
               .reshape(bsz, seq, SB_HEADS, SB_HEAD_DIM) for i in range(3))
    y_a = s5_mixer(u, a_re, a_im, log_dt, b_re, b_im, c_re, c_im, d_skip, w_glu)
    y_b = stick_breaking_attention(q, k, v).reshape(bsz, seq, SB_WIDTH)
    return jnp.concatenate([y_a, y_b], axis=-1) @ w_out


def rwkv7_recurrence(r, decay, k, v, a, b):
    bsz, _, heads, n = r.shape

    def step(state, inp):
        r_t, d_t, k_t, v_t, a_t, b_t = inp
        sa = jnp.einsum('bhvk,bhk->bhv', state, a_t)
        state = (state * d_t[:, :, None, :] + sa[..., None] * b_t[:, :, None, :]
                 + v_t[..., None] * k_t[:, :, None, :])
        return state, jnp.einsum('bhvk,bhk->bhv', state, r_t)

    xs = tuple(jnp.swapaxes(z, 0, 1) for z in (r, decay, k, v, a, b))
    _, ys = lax.scan(step, jnp.zeros((bsz, heads, n, n), jnp.float32), xs)
    return jnp.swapaxes(ys, 0, 1)


def rwkv7_mixer(p, mu, w0, w2, a0, a2, g2, k_k, k_a, r_k, ln_w, ln_b):
    bsz, seq, _ = p.shape
    p = p + (token_shift(p) - p) * mu
    i1, i2, i3 = RW_WIDTH, 2 * RW_WIDTH, 3 * RW_WIDTH
    i4 = i3 + RW_DECAY_LORA
    i5 = i4 + RW_AAA_LORA
    r, k, v, xw, xa, xg = jnp.split(p.astype(jnp.float32), [i1, i2, i3, i4, i5], axis=-1)
    w = -jax.nn.softplus(-(w0 + jnp.tanh(xw) @ w2)) - 0.5
    decay = jnp.exp(-jnp.exp(w))
    a = jax.nn.sigmoid(a0 + xa @ a2)
    g = jax.nn.sigmoid(xg) @ g2

    def heads(z):
        return z.reshape(bsz, seq, RW_HEADS, RW_HEAD)

    kk = heads(k * k_k)
    kk = kk / jnp.maximum(jnp.linalg.norm(kk, axis=-1, keepdims=True), 1e-12)
    k = k * (1.0 + (a - 1.0) * k_a)
    r_h, k_h, v_h, a_h = heads(r), heads(k), heads(v), heads(a)
    y = rwkv7_recurrence(r_h, heads(decay), k_h, v_h, -kk, kk * a_h)
    mean = jnp.mean(y, axis=-1, keepdims=True)
    var = jnp.mean(jnp.square(y - mean), axis=-1, keepdims=True)
    y = ((y - mean) * lax.rsqrt(var + RW_LN_EPS)).reshape(bsz, seq, RW_WIDTH) * ln_w + ln_b
    bonus = jnp.sum(r_h * k_h * r_k, axis=-1, keepdims=True) * v_h
    y = (y + bonus.reshape(bsz, seq, RW_WIDTH)) * g
    return y.astype(p.dtype)


def causal_dwconv(x, w, b):
    width, ch = w.shape
    y = lax.conv_general_dilated(x, w[:, None, :].astype(x.dtype), window_strides=(1,),
                                 padding=[(width - 1, 0)],
                                 dimension_numbers=('NWC', 'WIO', 'NWC'),
                                 feature_group_count=ch)
    return y + b


def segsum(z):
    n = z.shape[-1]
    cs = jnp.cumsum(z, axis=-1)
    diff = cs[..., :, None] - cs[..., None, :]
    return jnp.where(jnp.tril(jnp.ones((n, n), bool)), diff, -jnp.inf)


def ssd_chunked(x, dt, a, bm, cm):
    bsz, seq, nh, hp = x.shape
    n_chunks = seq // CHUNK
    xd = (x * dt[..., None]).reshape(bsz, n_chunks, CHUNK, nh, hp)
    a_dt = (a * dt).reshape(bsz, n_chunks, CHUNK, nh).transpose(0, 3, 1, 2)
    bc = bm.reshape(bsz, n_chunks, CHUNK, nh, -1)
    cc = cm.reshape(bsz, n_chunks, CHUNK, nh, -1)
    a_cs = jnp.cumsum(a_dt, axis=-1)
    l_mat = jnp.exp(segsum(a_dt))
    y_diag = jnp.einsum('bclhn,bcshn,bhcls,bcshp->bclhp', cc, bc, l_mat, xd)
    decay_states = jnp.exp(a_cs[..., -1:] - a_cs)
    states = jnp.einsum('bclhn,bhcl,bclhp->bchpn', bc, decay_states, xd)
    states = jnp.concatenate([jnp.zeros_like(states[:, :1]), states], axis=1)
    decay_chunk = jnp.exp(segsum(jnp.pad(a_cs[..., -1], ((0, 0), (0, 0), (1, 0)))))
    states = jnp.einsum('bhzc,bchpn->bzhpn', decay_chunk, states)[:, :-1]
    y_off = jnp.einsum('bclhn,bchpn,bhcl->bclhp', cc, states, jnp.exp(a_cs))
    return (y_diag + y_off).reshape(bsz, seq, nh, hp)


def mamba2_mixer(p, conv_w, conv_b, dt_bias, a_log, d_skip, norm_w):
    f32 = jnp.float32
    bsz, seq, _ = p.shape
    z = p[..., :M2_INNER]
    xbc = jax.nn.silu(causal_dwconv(p[..., M2_INNER:M2_INNER + M2_CONV_DIM], conv_w, conv_b))
    dt_raw = p[..., M2_INNER + M2_CONV_DIM:]
    xs = xbc[..., :M2_INNER].astype(f32).reshape(bsz, seq, M2_HEADS, M2_HEADDIM)
    heads_per_group = M2_HEADS // M2_GROUPS
    bm = xbc[..., M2_INNER:M2_INNER + M2_GROUPS * M2_STATE].astype(f32).reshape(bsz, seq, M2_GROUPS, M2_STATE)
    cm = xbc[..., M2_INNER + M2_GROUPS * M2_STATE:].astype(f32).reshape(bsz, seq, M2_GROUPS, M2_STATE)
    bm = jnp.repeat(bm, heads_per_group, axis=2)
    cm = jnp.repeat(cm, heads_per_group, axis=2)
    dt = jax.nn.softplus(dt_raw.astype(f32) + dt_bias.astype(f32))
    a = -jnp.exp(a_log.astype(f32))
    y = ssd_chunked(xs, dt, a, bm, cm) + d_skip.astype(f32)[:, None] * xs
    y = y.reshape(bsz, seq, M2_INNER) * jax.nn.silu(z.astype(f32))
    yg = y.reshape(bsz, seq, M2_GROUPS, M2_INNER // M2_GROUPS)
    yg = yg * lax.rsqrt(jnp.mean(yg * yg, axis=-1, keepdims=True) + RMS_EPS)
    return (yg.reshape(bsz, seq, M2_INNER) * norm_w).astype(p.dtype)


def odd_mixer(h, w_in, w_out, rw_mu, rw_w0, rw_w2, rw_a0, rw_a2, rw_g2, rw_k_k, rw_k_a,
              rw_r_k, rw_ln_w, rw_ln_b, m2_conv_w, m2_conv_b, m2_dt_bias, m2_a_log, m2_d, m2_norm_w):
    proj = h @ w_in
    y_c = rwkv7_mixer(proj[..., :RW_IN], rw_mu, rw_w0, rw_w2, rw_a0, rw_a2, rw_g2,
                      rw_k_k, rw_k_a, rw_r_k, rw_ln_w, rw_ln_b)
    y_d = mamba2_mixer(proj[..., RW_IN:], m2_conv_w, m2_conv_b, m2_dt_bias, m2_a_log, m2_d, m2_norm_w)
    return jnp.concatenate([y_c, y_d], axis=-1) @ w_out


def routed_experts(t, expert_idx, gates, w_gate, w_up, w_down):
    n_tok, d = t.shape
    n_assign = n_tok * MOE_TOPK
    flat_e = expert_idx.reshape(-1)
    flat_tok = jnp.repeat(jnp.arange(n_tok, dtype=jnp.int32), MOE_TOPK)
    flat_g = gates.reshape(-1)
    order = jnp.argsort(flat_e)
    sorted_e = flat_e[order]
    counts = jnp.bincount(flat_e, length=MOE_EXPERTS)
    padded = ((counts + MOE_BLOCK - 1) // MOE_BLOCK) * MOE_BLOCK
    pad_end = jnp.cumsum(padded)
    pad_start = pad_end - padded
    start = jnp.cumsum(counts) - counts
    dest = pad_start[sorted_e] + jnp.arange(n_assign) - start[sorted_e]
    n_blocks = (n_assign + MOE_EXPERTS * (MOE_BLOCK - 1) + MOE_BLOCK - 1) // MOE_BLOCK
    n_rows = n_blocks * MOE_BLOCK
    row_tok = jnp.full((n_rows,), n_tok, jnp.int32).at[dest].set(flat_tok[order])
    row_gate = jnp.zeros((n_rows,), jnp.float32).at[dest].set(flat_g[order])
    blk_expert = jnp.minimum(jnp.searchsorted(pad_end, jnp.arange(n_blocks) * MOE_BLOCK, side='right'),
                             MOE_EXPERTS - 1)
    t_pad = jnp.concatenate([t, jnp.zeros((1, d), t.dtype)], axis=0)
    xs = t_pad[row_tok].reshape(n_blocks, MOE_BLOCK, d)

    def expert_block(args):
        xb, e = args
        return (jax.nn.silu(xb @ w_gate[e]) * (xb @ w_up[e])) @ w_down[e]

    ys = lax.map(expert_block, (xs, blk_expert)).reshape(n_rows, d)
    out = jnp.zeros((n_tok + 1, d), ys.dtype).at[row_tok].add(ys * row_gate[:, None].astype(ys.dtype))
    return out[:n_tok]


def hierarchical_moe(h, w_grp, b_grp, w_exp, b_exp, w_gate, w_up, w_down):
    bsz, seq, d = h.shape
    t = h.reshape(-1, d)
    n_tok = t.shape[0]
    grp_prob = jax.nn.softmax((t @ w_grp + b_grp).astype(jnp.float32), axis=-1)
    grp_p, grp_idx = lax.top_k(grp_prob, 1)
    exp_logits = (t @ w_exp + b_exp).astype(jnp.float32).reshape(n_tok, MOE_GROUPS, MOE_PER_GROUP)
    in_grp = exp_logits[jnp.arange(n_tok), grp_idx[:, 0]]
    top_logit, top_local = lax.top_k(in_grp, MOE_TOPK)
    gates = grp_p * jax.nn.softmax(top_logit, axis=-1)
    expert_idx = grp_idx * MOE_PER_GROUP + top_local
    return routed_experts(t, expert_idx, gates, w_gate, w_up, w_down).reshape(bsz, seq, d)


def setup_inputs(seed: int = 0) -> dict:
    key = jax.random.key(seed)
    ks = iter(jax.random.split(key, 64))
    f32 = jnp.float32
    D = D_MODEL

    def nrm(shape, scale):
        return scale * jax.random.normal(next(ks), shape, f32)

    def uni(shape, lo, hi):
        return jax.random.uniform(next(ks), shape, f32, lo, hi)

    log_dt_lo, log_dt_hi = math.log(1e-3), math.log(1e-1)
    m2_dt = jnp.exp(uni((N_ODD, M2_HEADS), log_dt_lo, log_dt_hi))
    return {
        'x': nrm((BATCH, SEQ, D), 1.0),
        'c': nrm((BATCH, D), 1.0),
        'ada_w': nrm((DEPTH, D, 6 * D), 0.5 * D ** -0.5),
        'ada_b': nrm((DEPTH, 6 * D), 0.02),
        'norm_mix_w': 1.0 + nrm((DEPTH, D), 0.05),
        'norm_ffn_w': 1.0 + nrm((DEPTH, D), 0.05),
        'even_w_in': nrm((N_EVEN, D, EVEN_IN), D ** -0.5),
        'even_w_out': nrm((N_EVEN, EVEN_MIX, D), EVEN_MIX ** -0.5),
        's5_a_re': -0.5 + nrm((N_EVEN, S5_GROUPS, S5_STATE), 0.01),
        's5_a_im': math.pi * jnp.arange(S5_STATE, dtype=f32)[None, None, :] + nrm((N_EVEN, S5_GROUPS, S5_STATE), 0.01),
        's5_log_dt': uni((N_EVEN, S5_GROUPS), log_dt_lo, log_dt_hi),
        's5_b_re': nrm((N_EVEN, S5_GROUPS, S5_STATE, S5_GROUP), (2 * S5_GROUP) ** -0.5),
        's5_b_im': nrm((N_EVEN, S5_GROUPS, S5_STATE, S5_GROUP), (2 * S5_GROUP) ** -0.5),
        's5_c_re': nrm((N_EVEN, S5_GROUPS, S5_GROUP, S5_STATE), S5_STATE ** -0.5),
        's5_c_im': nrm((N_EVEN, S5_GROUPS, S5_GROUP, S5_STATE), S5_STATE ** -0.5),
        's5_d': nrm((N_EVEN, S5_GROUPS, S5_GROUP), 1.0),
        's5_w_glu': nrm((N_EVEN, S5_WIDTH, 2 * S5_WIDTH), S5_WIDTH ** -0.5),
        'odd_w_in': nrm((N_ODD, D, ODD_IN), D ** -0.5),
        'odd_w_out': nrm((N_ODD, ODD_MIX, D), ODD_MIX ** -0.5),
        'rw_mu': uni((N_ODD, RW_IN), 0.0, 1.0),
        'rw_w0': uni((N_ODD, RW_WIDTH), -5.0, 0.0),
        'rw_w2': nrm((N_ODD, RW_DECAY_LORA, RW_WIDTH), 0.1),
        'rw_a0': nrm((N_ODD, RW_WIDTH), 0.1),
        'rw_a2': nrm((N_ODD, RW_AAA_LORA, RW_WIDTH), 0.1),
        'rw_g2': nrm((N_ODD, RW_GATE_LORA, RW_WIDTH), RW_GATE_LORA ** -0.5),
        'rw_k_k': 0.85 + nrm((N_ODD, RW_WIDTH), 0.02),
        'rw_k_a': 1.0 + nrm((N_ODD, RW_WIDTH), 0.02),
        'rw_r_k': nrm((N_ODD, RW_HEADS, RW_HEAD), 0.1),
        'rw_ln_w': 1.0 + nrm((N_ODD, RW_WIDTH), 0.05),
        'rw_ln_b': nrm((N_ODD, RW_WIDTH), 0.02),
        'm2_conv_w': nrm((N_ODD, M2_CONV, M2_CONV_DIM), M2_CONV ** -0.5),
        'm2_conv_b': nrm((N_ODD, M2_CONV_DIM), 0.02),
        'm2_dt_bias': m2_dt + jnp.log(-jnp.expm1(-m2_dt)),
        'm2_a_log': jnp.log(uni((N_ODD, M2_HEADS), 1.0, 16.0)),
        'm2_d': 1.0 + nrm((N_ODD, M2_HEADS), 0.1),
        'm2_norm_w': 1.0 + nrm((N_ODD, M2_INNER), 0.05),
        'moe_w_grp': nrm((DEPTH, D, MOE_GROUPS), D ** -0.5),
        'moe_b_grp': nrm((DEPTH, MOE_GROUPS), 0.01),
        'moe_w_exp': nrm((DEPTH, D, MOE_EXPERTS), D ** -0.5),
        'moe_b_exp': nrm((DEPTH, MOE_EXPERTS), 0.01),
        'moe_w_gate': nrm((DEPTH, MOE_EXPERTS, D, MOE_HIDDEN), D ** -0.5),
        'moe_w_up': nrm((DEPTH, MOE_EXPERTS, D, MOE_HIDDEN), D ** -0.5),
        'moe_w_down': nrm((DEPTH, MOE_EXPERTS, MOE_HIDDEN, D), MOE_HIDDEN ** -0.5),
        'final_norm_w': 1.0 + nrm((D,), 0.05),
    }


def reference(x, c, ada_w, ada_b, norm_mix_w, norm_ffn_w, even_w_in, even_w_out,
              s5_a_re, s5_a_im, s5_log_dt, s5_b_re, s5_b_im, s5_c_re, s5_c_im, s5_d, s5_w_glu,
              odd_w_in, odd_w_out, rw_mu, rw_w0, rw_w2, rw_a0, rw_a2, rw_g2, rw_k_k, rw_k_a,
              rw_r_k, rw_ln_w, rw_ln_b, m2_conv_w, m2_conv_b, m2_dt_bias, m2_a_log, m2_d, m2_norm_w,
              moe_w_grp, moe_b_grp, moe_w_exp, moe_b_exp, moe_w_gate, moe_w_up, moe_w_down,
              final_norm_w):
    cond = jax.nn.silu(c)
    for i in range(DEPTH):
        j = i // 2
        shift1, scale1, gate1, shift2, scale2, gate2 = jnp.split(cond @ ada_w[i] + ada_b[i], 6, axis=-1)
        h = modulate(rmsnorm(x, norm_mix_w[i]), shift1, scale1)
        if i % 2 == 0:
            y = even_mixer(h, even_w_in[j], even_w_out[j], s5_a_re[j], s5_a_im[j], s5_log_dt[j],
                           s5_b_re[j], s5_b_im[j], s5_c_re[j], s5_c_im[j], s5_d[j], s5_w_glu[j])
        else:
            y = odd_mixer(h, odd_w_in[j], odd_w_out[j], rw_mu[j], rw_w0[j], rw_w2[j], rw_a0[j],
                          rw_a2[j], rw_g2[j], rw_k_k[j], rw_k_a[j], rw_r_k[j], rw_ln_w[j], rw_ln_b[j],
                          m2_conv_w[j], m2_conv_b[j], m2_dt_bias[j], m2_a_log[j], m2_d[j], m2_norm_w[j])
        x = x + gate1[:, None, :] * y
        h = modulate(rmsnorm(x, norm_ffn_w[i]), shift2, scale2)
        x = x + gate2[:, None, :] * hierarchical_moe(h, moe_w_grp[i], moe_b_grp[i], moe_w_exp[i], moe_b_exp[i],
                                                     moe_w_gate[i], moe_w_up[i], moe_w_down[i])
    return rmsnorm(x, final_norm_w)
```

```python
import math
from contextlib import ExitStack
import numpy as np
import concourse.bass as bass
import concourse.mybir as mybir
from concourse.bass_utils import run_bass_kernel_spmd

F32 = mybir.dt.float32
BF16 = mybir.dt.bfloat16
I32 = mybir.dt.int32
ALU = mybir.AluOpType
AF = mybir.ActivationFunctionType
AX = mybir.AxisListType
PI = math.pi
D = 1024
DBG = {'norm': 9}


class KB:
    NDMA = 10
    THRESH = 10 ** 9

    def __init__(self, nc):
        self.nc = nc
        self.E = {'pe': nc.tensor, 'act': nc.scalar, 'dve': nc.vector, 'pool': nc.gpsimd, 'sp': nc.sync}
        names = ['pe', 'act', 'dve', 'pool']
        self.dq = {}
        for q in ('sp', 'act', 'pool'):
            dn = ['d_%s%d' % (q, j) for j in range(self.NDMA)]
            names += dn
            self.dq[q] = [dn, 0]
        self.banks = [{n: nc.alloc_semaphore('%s_b%d' % (n, b)) for n in names} for b in range(2)]
        self.bank = 0
        self.semh = self.banks[0]
        self.cnt = {n: 0 for n in names}
        self.seen = {e: {} for e in self.E}
        self.lastw = {}
        self.readers = {}
        self.dummy = None
        self.nswitch = 0

    def _wait(self, eng, deps):
        for s, v in deps.items():
            if eng == 'pe' and s == 'pe':
                continue
            if self.seen[eng].get(s, 0) < v:
                self.E[eng].wait_ge(self.semh[s], v)
                self.seen[eng][s] = v

    def _deps(self, reads, writes):
        deps = {}
        for r in reads:
            for s, v in self.lastw.get(r, {}).items():
                deps[s] = max(deps.get(s, 0), v)
        for w in writes:
            for s, v in self.lastw.get(w, {}).items():
                deps[s] = max(deps.get(s, 0), v)
            for s, v in self.readers.get(w, {}).items():
                deps[s] = max(deps.get(s, 0), v)
        return deps

    def _record(self, ev, reads, writes):
        s, v = ev
        for r in reads:
            d = self.readers.setdefault(r, {})
            d[s] = max(d.get(s, 0), v)
        for w in writes:
            self.lastw[w] = {s: v}
            self.readers[w] = {}

    def op(self, eng, fname, *args, reads=(), writes=(), **kw):
        self._wait(eng, self._deps(reads, writes))
        inst = getattr(self.E[eng], fname)(*args, **kw)
        self.cnt[eng] += 1
        inst.then_inc(self.semh[eng], 1)
        self._record((eng, self.cnt[eng]), reads, writes)
        return inst

    def dma(self, q, out, in_, reads=(), writes=(), **kw):
        names, idx = self.dq[q]
        n = names[idx % len(names)]
        self.dq[q][1] = idx + 1
        deps = self._deps(reads, writes)
        if self.cnt[n] > 0:
            deps[n] = max(deps.get(n, 0), self.cnt[n])
        self._wait(q, deps)
        inst = self.E[q].dma_start(out=out, in_=in_, **kw)
        self.cnt[n] += 16
        inst.then_inc(self.semh[n], 16)
        self._record((n, self.cnt[n]), reads, writes)
        return inst

    def barrier(self, sw=True):
        allv = {s: v for s, v in self.cnt.items() if v > 0}
        for e in self.E:
            self._wait(e, dict(allv))
        self.lastw = {}
        self.readers = {}
        if sw and max(self.cnt.values()) > self.THRESH:
            self._switch()

    def checkpoint(self):
        if max(self.cnt.values()) > self.THRESH:
            self.barrier()

    def _switch(self):
        other = 1 - self.bank
        pool = self.E['pool']
        for h in self.banks[other].values():
            pool.sem_clear(h)
        inst = pool.memset(self.dummy[:], 0.0)
        self.cnt['pool'] += 1
        inst.then_inc(self.semh['pool'], 1)
        v = self.cnt['pool']
        for e in ('pe', 'act', 'dve', 'sp'):
            self.E[e].wait_ge(self.semh['pool'], v)
        self.bank = other
        self.semh = self.banks[other]
        for n in self.cnt:
            self.cnt[n] = 0
        self.seen = {e: {} for e in self.E}
        self.lastw = {}
        self.readers = {}
        self.nswitch += 1

    def finish(self):
        allv = {s: v for s, v in self.cnt.items() if v > 0}
        self._wait('sp', allv)


INPUT_SHAPES = {
    'x': None, 'c': [1, D], 'ada_w': [2, D, 6 * D], 'ada_b': [2, 6 * D], 'norm_mix_w': [2, D], 'norm_ffn_w': [2, D],
    'even_w_in': [1, D, 2048], 'even_w_out': [1, D, D], 's5_a_re': [1, 32, 64], 's5_a_im': [1, 32, 64],
    's5_log_dt': [1, 32], 's5_b_re': [1, 32, 64, 16], 's5_b_im': [1, 32, 64, 16], 's5_c_re': [1, 32, 16, 64],
    's5_c_im': [1, 32, 16, 64], 's5_d': [1, 32, 16], 's5_w_glu': [1, 512, 1024], 'odd_w_in': [1, D, 3336],
    'odd_w_out': [1, D, D], 'rw_mu': [1, 1792], 'rw_w0': [1, 512], 'rw_w2': [1, 64, 512], 'rw_a0': [1, 512],
    'rw_a2': [1, 64, 512], 'rw_g2': [1, 128, 512], 'rw_k_k': [1, 512], 'rw_k_a': [1, 512], 'rw_r_k': [1, 8, 64],
    'rw_ln_w': [1, 512], 'rw_ln_b': [1, 512], 'm2_conv_w': [1, 4, 1024], 'm2_conv_b': [1, 1024], 'm2_dt_bias': [1, 8],
    'm2_a_log': [1, 8], 'm2_d': [1, 8], 'm2_norm_w': [1, 512], 'moe_w_grp': [2, D, 4], 'moe_b_grp': [2, 4],
    'moe_w_exp': [2, D, 32], 'moe_b_exp': [2, 32], 'moe_w_gate': [2, 32, D, 512], 'moe_w_up': [2, 32, D, 512],
    'moe_w_down': [2, 32, 512, D], 'final_norm_w': [1, D],
}


class Mod:
    def __init__(self, T, dbg=(), nlayers=2, nexp=32, lsel=None):
        self.T = T
        self.lsel = lsel
        self.NT = T // 128
        self.TB = min(512, T)
        self.NB = T // self.TB
        self.TPB = self.TB // 128
        self.dbg = set(dbg)
        self.nlayers = nlayers
        self.nexp = nexp
        nc = self.nc = bass.Bass("TRN2", target_bir_lowering=False)
        self.k = KB(nc)
        self.I = {}
        for name, shp in INPUT_SHAPES.items():
            shp = [T, D] if name == 'x' else shp
            if nexp == 0 and name in ('moe_w_gate', 'moe_w_up', 'moe_w_down'):
                shp = [2, 1] + list(shp[2:])
            if lsel is not None and shp[0] == 2:
                shp = [1] + list(shp[1:])
            self.I[name] = nc.dram_tensor(name, list(shp), F32, kind="ExternalInput").ap()
        self.out = nc.dram_tensor('out', [T, D], F32, kind="ExternalOutput").ap()
        self.dbg_out = {}
        self.ps = [nc.alloc_psum_tensor('ps%d' % i, [128, 512], F32) for i in range(8)]
        self.uid = 0

    def scr(self, name, shape, dt=F32):
        return self.nc.dram_tensor(name, list(shape), dt, kind="Internal").ap()

    def tap(self, name, shape):
        if name in self.dbg:
            t = self.nc.dram_tensor('dbg_' + name, list(shape), F32, kind="ExternalOutput").ap()
            self.dbg_out[name] = t
            return t
        return None

    def sb(self, st, name, shape, dt=F32):
        self.nid = getattr(self, 'nid', 0) + 1
        return st.enter_context(self.nc.sbuf_tensor('%s_%d' % (name, self.nid), list(shape), dt))

    def consts(self, st):
        k = self.k
        self.ident = self.sb(st, 'ident', [128, 128])
        k.op('pool', 'memset', self.ident[:], 1.0, writes=['ident'])
        k.op('pool', 'affine_select', self.ident[:], self.ident[:], pattern=[[1, 128]], compare_op=ALU.is_equal,
             fill=0.0, base=0, channel_multiplier=-1, reads=['ident'], writes=['ident'])
        self.ones = self.sb(st, 'ones', [128, 128])
        k.op('pool', 'memset', self.ones[:], 1.0, writes=['ones'])
        self.negones = self.sb(st, 'negones', [128, 128])
        k.op('pool', 'memset', self.negones[:], -1.0, writes=['negones'])
        self.negtri = self.sb(st, 'negtri', [128, 128])
        k.op('pool', 'memset', self.negtri[:], -1.0, writes=['negtri'])
        k.op('pool', 'affine_select', self.negtri[:], self.negtri[:], pattern=[[-1, 128]], compare_op=ALU.is_ge,
             fill=0.0, base=0, channel_multiplier=1, reads=['negtri'], writes=['negtri'])
        self.blk = self.sb(st, 'blk', [128, 128])
        k.op('pool', 'memset', self.blk[:], 0.0, writes=['blk'])
        k.op('pool', 'memset', self.blk[0:64, 0:64], 1.0, reads=['blk'], writes=['blk'])
        k.op('pool', 'memset', self.blk[64:128, 64:128], 1.0, reads=['blk'], writes=['blk'])
        self.cst = self.sb(st, 'cst', [128, 4])
        k.dummy = self.sb(st, 'kdummy', [128, 4])
        k.op('pool', 'memset', self.cst[:, 0:1], -PI, writes=['cst'])
        k.op('pool', 'memset', self.cst[:, 1:2], 1.0, reads=['cst'], writes=['cst'])
        k.op('pool', 'memset', self.cst[:, 2:3], 0.0, reads=['cst'], writes=['cst'])
        self.modbc = self.sb(st, 'modbc', [128, 6 * D])

    def adaln(self, li):
        k, nc = self.k, self.nc
        li = 0 if self.lsel is not None else li
        with ExitStack() as st:
            ccol = self.sb(st, 'ccol', [128, 8])
            condbc = self.sb(st, 'condbc', [128, 8, 128])
            adab = self.sb(st, 'adab', [1, 6 * D])
            wch = [self.sb(st, 'adaw%d' % i, [128, 8, 512]) for i in range(2)]
            self.normw = self.sb(st, 'normw', [128, 2 * D])
            k.dma('sp', ccol[:], self.I['c'].rearrange("o (kc k) -> k (o kc)", k=128), writes=['ccol'],
                  allow_slow_non_contiguous=True)
            k.op('act', 'activation', ccol[:], ccol[:], AF.Silu, reads=['ccol'], writes=['ccol'])
            for kc in range(8):
                k.op('dve', 'tensor_scalar', condbc[:, kc, :], self.ones[:], ccol[:, kc:kc + 1], None, ALU.mult,
                     reads=['ccol', 'ones'], writes=['condbc'])
            k.dma('sp', adab[:], self.I['ada_b'][li:li + 1, :], writes=['adab'])
            wv = self.I['ada_w'][li].rearrange("(kc k) n -> k kc n", k=128)
            for n in range(12):
                k.checkpoint()
                b = n % 2
                k.dma('pool' if DBG.get('pooldma') else ('sp' if b == 0 else 'act'), wch[b][:], wv[:, :, n * 512:(n + 1) * 512], writes=['adaw%d' % b])
                p = self.ps[n % 2]
                pk = 'ps%d' % (n % 2)
                for kc in range(8):
                    k.op('pe', 'matmul', p[:], condbc[:, kc, :], wch[b][:, kc, :], start=(kc == 0), stop=False,
                         reads=['condbc', 'adaw%d' % b], writes=[pk])
                k.op('pe', 'matmul', p[:], self.ones[0:1, :], adab[0:1, n * 512:(n + 1) * 512], start=False, stop=True,
                     reads=['ones', 'adab'], writes=[pk])
                k.op('act', 'activation', self.modbc[:, n * 512:(n + 1) * 512], p[:], AF.Copy, reads=[pk], writes=['modbc'])
            k.dma('sp', self.normw[:, 0:D], self.I['norm_mix_w'][li:li + 1, :].partition_broadcast(128).rearrange("p o n -> p (o n)"),
                  writes=['normw'])
            k.dma('sp', self.normw[:, D:2 * D], self.I['norm_ffn_w'][li:li + 1, :].partition_broadcast(128).rearrange("p o n -> p (o n)"),
                  reads=['normw'], writes=['normw'])
            for slot, wo in ((1, 0), (4, D)):
                k.op('dve', 'scalar_tensor_tensor', self.modbc[:, slot * D:(slot + 1) * D], self.modbc[:, slot * D:(slot + 1) * D],
                     1.0, self.normw[:, wo:wo + D], ALU.add, ALU.mult, reads=['modbc', 'normw'], writes=['modbc'])
            k.barrier()

    def norm_tile(self, bufs, xsrc, tile, gA, gB, hT_dst, hT32_dst=None, keys=()):
        k = self.k
        i = self.uid
        self.uid += 1
        b = i % 2
        xt, h32, sq, ss = bufs['xt'][b], bufs['h32'][b], bufs['sq'], bufs['ss']
        kx, kh = 'xt%d' % b, 'h32%d' % b
        k.dma('sp', xt[:], xsrc[tile * 128:(tile + 1) * 128, :], reads=['xres'], writes=[kx])
        k.op('dve', 'tensor_tensor', sq[:], xt[:], xt[:], ALU.mult, reads=[kx], writes=['sq'])
        c = ss[:, (i % 8):(i % 8) + 1]
        k.op('dve', 'tensor_reduce', c, sq[:], AX.X, ALU.add, reads=['sq'], writes=['ss'])
        k.op('dve', 'tensor_scalar', c, c, 1.0 / D, 1e-6, ALU.mult, ALU.add, reads=['ss'], writes=['ss'])
        k.op('act', 'activation', c, c, AF.Sqrt, reads=['ss'], writes=['ss'])
        k.op('dve', 'reciprocal', c, c, reads=['ss'], writes=['ss'])
        k.op('dve', 'scalar_tensor_tensor', h32[:], xt[:], c, gA, ALU.mult, ALU.mult, reads=[kx, 'ss', 'modbc'], writes=[kh])
        if DBG['norm'] < 1:
            return
        k.op('pool', 'tensor_tensor', h32[:], h32[:], gB, ALU.add, reads=[kh, 'modbc'], writes=[kh])
        if DBG['norm'] < 2:
            return
        pa = (i % 2) * 2
        for half in range(2):
            p = self.ps[pa + half]
            pk = 'ps%d' % (pa + half)
            for j in range(4):
                kc = half * 4 + j
                k.op('pe', 'transpose', p[:, j * 128:(j + 1) * 128], h32[:, kc * 128:(kc + 1) * 128], self.ident[:],
                     reads=[kh, 'ident'], writes=[pk])
            pv = p[:].rearrange("p (a b) -> p a b", a=4)
            if hT32_dst is None:
                k.op('act', 'activation', hT_dst[:, half * 4:half * 4 + 4, :], pv, AF.Copy, reads=[pk], writes=list(keys))
            else:
                k.op('dve', 'tensor_copy', hT32_dst[:, half * 4:half * 4 + 4, :], pv, reads=[pk], writes=['hT32'])
                k.op('act', 'activation', hT_dst[:, half * 4:half * 4 + 4, :], hT32_dst[:, half * 4:half * 4 + 4, :], AF.Copy,
                     reads=['hT32'], writes=list(keys))

    def norm_bufs(self, st):
        return {'xt': [self.sb(st, 'xt%d' % i, [128, D]) for i in range(2)],
                'h32': [self.sb(st, 'h32%d' % i, [128, D]) for i in range(2)],
                'sq': self.sb(st, 'sq', [128, D]), 'ss': self.sb(st, 'ss', [128, 8])}

    def even_mixer(self, xin, xout):
        k, nc, T, NT, TB, NB, TPB = self.k, self.nc, self.T, self.NT, self.TB, self.NB, self.TPB
        I = self.I
        yb_d = self.scr('yb_d', [8, 64, T], BF16)
        uT_d = self.scr('uT_d', [4, 128, T], BF16)
        ya_d = self.scr('ya_d', [4, 128, T], BF16)
        with ExitStack() as st0:
            with ExitStack() as st1:
                qT = self.sb(st1, 'qT', [128, 4, T], BF16)
                kT = self.sb(st1, 'kT', [128, 4, T], BF16)
                vtok = self.sb(st1, 'vtok', [128, NT, 512], BF16)
                with ExitStack() as st:
                    win = self.sb(st, 'win', [128, 8, 2048], BF16)
                    wv = I['even_w_in'][0].rearrange("(kc k) n -> k kc n", k=128)
                    for kc in range(8):
                        k.dma('pool', win[:, kc, :], wv[:, kc, :], writes=['win'])
                    bufs = self.norm_bufs(st)
                    hT = [self.sb(st, 'hT%d' % i, [128, 8, TB], BF16) for i in range(2)]
                    ustg = [self.sb(st, 'ustg%d' % i, [128, TB], BF16) for i in range(2)]
                    for tb in range(NB):
                        k.checkpoint()
                        hb = hT[tb % 2]
                        hk = 'hT%d' % (tb % 2)
                        for j in range(TPB):
                            self.norm_tile(bufs, xin, tb * TPB + j, self.modbc[:, D:2 * D], self.modbc[:, 0:D],
                                           hb[:, :, j * 128:(j + 1) * 128], keys=[hk])
                        tsl = slice(tb * TB, (tb + 1) * TB)
                        cnt = 0
                        for dst, c0, scale, dk in ((None, 0, 1.0, 'uT'), (qT, 512, 0.125, 'qT'), (kT, 1024, 1.0, 'kT')):
                            for mt in range(4):
                                pi = 4 + cnt % 4
                                cnt += 1
                                p, pk = self.ps[pi], 'ps%d' % pi
                                for kc in range(8):
                                    k.op('pe', 'matmul', p[:, 0:TB], win[:, kc, c0 + mt * 128:c0 + (mt + 1) * 128], hb[:, kc, :],
                                         start=(kc == 0), stop=(kc == 7), reads=['win', hk], writes=[pk])
                                if dst is None:
                                    us, usk = ustg[mt % 2], 'ustg%d' % (mt % 2)
                                    k.op('act', 'activation', us[:], p[:, 0:TB], AF.Copy, reads=[pk], writes=[usk])
                                    k.dma('sp', uT_d[mt, :, tsl], us[:], reads=[usk], writes=['uT_d'])
                                else:
                                    k.op('act', 'activation', dst[:, mt, tsl], p[:, 0:TB], AF.Copy, scale=scale, reads=[pk], writes=[dk])
                        for j in range(TPB):
                            pi = 4 + cnt % 4
                            cnt += 1
                            p, pk = self.ps[pi], 'ps%d' % pi
                            for kc in range(8):
                                k.op('pe', 'matmul', p[:], hb[:, kc, j * 128:(j + 1) * 128], win[:, kc, 1536:2048],
                                     start=(kc == 0), stop=(kc == 7), reads=['win', hk], writes=[pk])
                            k.op('dve', 'tensor_copy', vtok[:, tb * TPB + j, :], p[:], reads=[pk], writes=['vtok'])
                    k.barrier()
                with ExitStack() as st:
                    E = [self.sb(st, 'sbE%d' % i, [128, TB]) for i in range(2)]
                    SP = [self.sb(st, 'sbSP%d' % i, [128, TB]) for i in range(2)]
                    ACC = [self.sb(st, 'sbAcc%d' % i, [128, TB]) for i in range(2)]
                    W = [self.sb(st, 'sbW%d' % i, [128, TB], BF16) for i in range(2)]
                    YO = [self.sb(st, 'sbY%d' % i, [64, TB], BF16) for i in range(2)]
                    it = 0
                    hq = 0
                    for h in range(8):
                        ft, pr = h // 2, slice(64 * (h % 2), 64 * (h % 2) + 64)
                        for qb in range(NB):
                            k.checkpoint()
                            hq += 1
                            pc, pck = self.ps[6 + hq % 2], 'ps%d' % (6 + hq % 2)
                            acc, ack = ACC[hq % 2], 'sbAcc%d' % (hq % 2)
                            qsl = slice(qb * TB, (qb + 1) * TB)
                            kts = list(range((qb + 1) * TPB - 1, -1, -1))
                            for n_, kt in enumerate(kts):
                                it += 1
                                b = it % 2
                                pz, pzk = self.ps[b], 'ps%d' % b
                                pa, pak = self.ps[2 + b], 'ps%d' % (2 + b)
                                m = kt - qb * TPB
                                ksl = slice(kt * 128, (kt + 1) * 128)
                                k.op('pe', 'matmul', pz[:, 0:TB], kT[pr, ft, ksl], qT[pr, ft, qsl], start=True, stop=True,
                                     reads=['kT', 'qT'], writes=[pzk])
                                k.op('act', 'activation', E[b][:], pz[:, 0:TB], AF.Exp, reads=[pzk], writes=['sbE%d' % b])
                                k.op('act', 'activation', SP[b][:], E[b][:], AF.Ln, bias=1.0, reads=['sbE%d' % b], writes=['sbSP%d' % b])
                                if m >= 0:
                                    k.op('pool', 'affine_select', SP[b][:], SP[b][:], pattern=[[1, TB]], compare_op=ALU.is_gt,
                                         fill=0.0, base=-128 * m, channel_multiplier=-1, reads=['sbSP%d' % b], writes=['sbSP%d' % b])
                                k.op('pe', 'matmul', pa[:, 0:TB], kT[pr, ft, ksl], qT[pr, ft, qsl], start=True, stop=False,
                                     reads=['kT', 'qT'], writes=[pak])
                                k.op('pe', 'matmul', pa[:, 0:TB], self.negtri[:], SP[b][:], start=False, stop=(n_ == 0),
                                     reads=['negtri', 'sbSP%d' % b], writes=[pak])
                                if n_ > 0:
                                    k.op('pe', 'matmul', pa[:, 0:TB], self.negones[:], acc[:], start=False, stop=True,
                                         reads=['negones', ack], writes=[pak])
                                k.op('act', 'activation', W[b][:], pa[:, 0:TB], AF.Exp, reads=[pak], writes=['sbW%d' % b])
                                if m >= 0:
                                    k.op('pool', 'affine_select', W[b][:], W[b][:], pattern=[[1, TB]], compare_op=ALU.is_gt,
                                         fill=0.0, base=-128 * m, channel_multiplier=-1, reads=['sbW%d' % b], writes=['sbW%d' % b])
                                if n_ == 0:
                                    k.op('pool', 'tensor_copy', acc[:], SP[b][:], reads=['sbSP%d' % b], writes=[ack])
                                elif n_ < len(kts) - 1:
                                    k.op('pool', 'tensor_tensor', acc[:], acc[:], SP[b][:], ALU.add, reads=['sbSP%d' % b, ack], writes=[ack])
                                k.op('pe', 'matmul', pc[0:64, 0:TB], vtok[:, kt, h * 64:(h + 1) * 64], W[b][:],
                                     start=(n_ == 0), stop=(n_ == len(kts) - 1), reads=['vtok', 'sbW%d' % b], writes=[pck])
                            yo, yok = YO[hq % 2], 'sbY%d' % (hq % 2)
                            k.op('dve', 'tensor_copy', yo[:], pc[0:64, 0:TB], reads=[pck], writes=[yok])
                            k.dma('sp', yb_d[h, :, qsl], yo[:], reads=[yok], writes=['yb_d'])
                    k.barrier()
            with ExitStack() as st1:
                with ExitStack() as st:
                    uT = self.sb(st, 'uT', [128, 4, T], BF16)
                    k.dma('sp', uT[:], uT_d.rearrange("i p t -> p i t"), reads=['uT_d'], writes=['uT'])
                    self.s5(uT, ya_d)
                with ExitStack() as st:
                    wglu = self.sb(st, 'wglu', [128, 4, 1024], BF16)
                    wout = self.sb(st, 'wout', [128, 8, 1024], BF16)
                    k.dma('pool', wglu[:], I['s5_w_glu'][0].rearrange("(kc k) n -> k kc n", k=128), writes=['wglu'])
                    wov = I['even_w_out'][0].rearrange("(kc k) n -> k kc n", k=128)
                    for kc in range(8):
                        k.dma('pool', wout[:, kc, :], wov[:, kc, :], writes=['wout'])
                    ymT = [self.sb(st, 'ymT%d' % i, [128, 8, TB], BF16) for i in range(2)]
                    yaB = [self.sb(st, 'yaB%d' % i, [128, 4, TB], BF16) for i in range(2)]
                    sg = [self.sb(st, 'sg%d' % i, [128, TB]) for i in range(2)]
                    xt = [self.sb(st, 'oxt%d' % i, [128, D]) for i in range(2)]
                    tmp = [self.sb(st, 'otmp%d' % i, [128, D]) for i in range(2)]
                    cnt = 0
                    for tb in range(NB):
                        k.checkpoint()
                        ym, ymk = ymT[tb % 2], 'ymT%d' % (tb % 2)
                        tsl = slice(tb * TB, (tb + 1) * TB)
                        k.dma('act', ym[:, 4:8, :], yb_d.rearrange("(ft hp) d t -> (hp d) ft t", hp=2)[:, :, tsl],
                              reads=['yb_d'], writes=[ymk])
                        yaT, yak = yaB[tb % 2], 'yaB%d' % (tb % 2)
                        k.dma('sp', yaT[:], ya_d.rearrange("i p t -> p i t")[:, :, tsl], reads=['ya_d'], writes=[yak])
                        for mt in range(4):
                            cnt += 1
                            b = cnt % 2
                            p1, p1k, p2, p2k = self.ps[b], 'ps%d' % b, self.ps[2 + b], 'ps%d' % (2 + b)
                            for kc in range(4):
                                k.op('pe', 'matmul', p1[:, 0:TB], wglu[:, kc, mt * 128:(mt + 1) * 128], yaT[:, kc, :],
                                     start=(kc == 0), stop=(kc == 3), reads=['wglu', yak], writes=[p1k])
                            for kc in range(4):
                                k.op('pe', 'matmul', p2[:, 0:TB], wglu[:, kc, 512 + mt * 128:512 + (mt + 1) * 128], yaT[:, kc, :],
                                     start=(kc == 0), stop=(kc == 3), reads=['wglu', yak], writes=[p2k])
                            k.op('act', 'activation', sg[b][:], p2[:, 0:TB], AF.Sigmoid, reads=[p2k], writes=['sg%d' % b])
                            k.op('dve', 'tensor_tensor', ym[:, mt, :], p1[:, 0:TB], sg[b][:], ALU.mult, reads=[p1k, 'sg%d' % b], writes=[ymk])
                        for j in range(TPB):
                            tile = tb * TPB + j
                            cnt += 1
                            b = cnt % 2
                            k.dma('sp', xt[b][:], xin[tile * 128:(tile + 1) * 128, :], reads=['xres'], writes=['oxt%d' % b])
                            for nh in range(2):
                                p, pk = self.ps[4 + 2 * b + nh], 'ps%d' % (4 + 2 * b + nh)
                                for kc in range(8):
                                    k.op('pe', 'matmul', p[:], ym[:, kc, j * 128:(j + 1) * 128], wout[:, kc, nh * 512:(nh + 1) * 512],
                                         start=(kc == 0), stop=(kc == 7), reads=[ymk, 'wout'], writes=[pk])
                                k.op('dve', 'tensor_tensor', tmp[b][:, nh * 512:(nh + 1) * 512], p[:], self.modbc[:, 2 * D + nh * 512:2 * D + (nh + 1) * 512],
                                     ALU.mult, reads=[pk, 'modbc'], writes=['otmp%d' % b])
                            k.op('pool', 'tensor_tensor', tmp[b][:], tmp[b][:], xt[b][:], ALU.add, reads=['otmp%d' % b, 'oxt%d' % b], writes=['otmp%d' % b])
                            k.dma('sp', xout[tile * 128:(tile + 1) * 128, :], tmp[b][:], reads=['otmp%d' % b], writes=['xres2'])
                    k.barrier()

    def s5(self, uT, ya_d):
        k, nc, T, NT, TB, NB = self.k, self.nc, self.T, self.NT, self.TB, self.NB
        I = self.I
        with ExitStack() as st:
            sb = lambda n, s, d=F32: self.sb(st, n, s, d)
            are, aim, dt = sb('are', [128, 16]), sb('aim', [128, 16]), sb('s5dt', [128, 16])
            for gl in range(2):
                ps_ = slice(64 * gl, 64 * gl + 64)
                k.dma('sp', are[ps_, :], I['s5_a_re'][0].rearrange("(pr gl) n -> gl n pr", gl=2)[gl], writes=['are'], allow_slow_non_contiguous=True)
                k.dma('sp', aim[ps_, :], I['s5_a_im'][0].rearrange("(pr gl) n -> gl n pr", gl=2)[gl], writes=['aim'], allow_slow_non_contiguous=True)
                k.dma('sp', dt[ps_, :].rearrange("p (o n) -> p o n", o=1),
                      I['s5_log_dt'].rearrange("o (pr gl) -> gl o pr", gl=2)[gl].partition_broadcast(64), writes=['s5dt'],
                      allow_slow_non_contiguous=True)
            V = {n: sb('s5_' + n, [128, 16]) for n in ('rho', 'th', 'q', 'cs', 'sn', 'abr', 'abi', 'den', 't1', 't2', 'cr', 'ci', 'ncr', 'nci')}
            qi = sb('s5qi', [128, 16], I32)
            KS = ['s5v']

            def o(eng, f, *a, **kw):
                k.op(eng, f, *a, reads=KS + ['are', 'aim', 's5dt', 'cst'], writes=KS, **kw)
            o('dve', 'tensor_scalar', are[:], are[:], -1e-4, None, ALU.min)
            o('act', 'activation', dt[:], dt[:], AF.Exp)
            o('dve', 'tensor_tensor', V['rho'][:], dt[:], are[:], ALU.mult)
            o('act', 'activation', V['rho'][:], V['rho'][:], AF.Exp)
            o('dve', 'tensor_tensor', V['th'][:], dt[:], aim[:], ALU.mult)

            def sincos(dst_s, dst_c, src, qf, qint, shape_key):
                o('dve', 'tensor_scalar', qint, src, 1.0 / (2 * PI), None, ALU.mult)
                o('dve', 'scalar_tensor_tensor', qf, qint, -2 * PI, src, ALU.mult, ALU.add)
                o('dve', 'tensor_scalar', qf, qf, PI, -PI, ALU.min, ALU.max)
                o('act', 'activation', dst_s, qf, AF.Sin)
                o('dve', 'scalar_tensor_tensor', qf, qf, -1.0, qf, ALU.mult, ALU.max)
                o('act', 'activation', dst_c, qf, AF.Sin, scale=-1.0, bias=self.cst[:, 3:4])
            k.op('pool', 'memset', self.cst[:, 3:4], PI / 2, reads=['cst'], writes=['cst'])
            sincos(V['sn'][:], V['cs'][:], V['th'][:], V['q'][:], qi[:], None)
            o('dve', 'tensor_tensor', V['abr'][:], V['rho'][:], V['cs'][:], ALU.mult)
            o('dve', 'tensor_tensor', V['abi'][:], V['rho'][:], V['sn'][:], ALU.mult)
            o('dve', 'tensor_scalar', V['abr'][:], V['abr'][:], -1.0, None, ALU.add)
            o('dve', 'tensor_tensor', V['den'][:], are[:], are[:], ALU.mult)
            o('dve', 'tensor_tensor', V['t1'][:], aim[:], aim[:], ALU.mult)
            o('dve', 'tensor_tensor', V['den'][:], V['den'][:], V['t1'][:], ALU.add)
            o('dve', 'reciprocal', V['den'][:], V['den'][:])
            o('dve', 'tensor_tensor', V['t1'][:], V['abr'][:], are[:], ALU.mult)
            o('dve', 'tensor_tensor', V['t2'][:], V['abi'][:], aim[:], ALU.mult)
            o('dve', 'tensor_tensor', V['t1'][:], V['t1'][:], V['t2'][:], ALU.add)
            o('dve', 'tensor_tensor', V['cr'][:], V['t1'][:], V['den'][:], ALU.mult)
            o('dve', 'tensor_tensor', V['t1'][:], V['abi'][:], are[:], ALU.mult)
            o('dve', 'tensor_tensor', V['t2'][:], V['abr'][:], aim[:], ALU.mult)
            o('dve', 'tensor_tensor', V['t1'][:], V['t1'][:], V['t2'][:], ALU.subtract)
            o('dve', 'tensor_tensor', V['ci'][:], V['t1'][:], V['den'][:], ALU.mult)
            o('dve', 'tensor_scalar', V['ncr'][:], V['cr'][:], -1.0, None, ALU.mult)
            o('dve', 'tensor_scalar', V['nci'][:], V['ci'][:], -1.0, None, ALU.mult)
            CmA, CmB = sb('CmA', [128, 16, 128], BF16), sb('CmB', [128, 16, 128], BF16)
            BmR, BmI = sb('BmR', [128, 16, 128], BF16), sb('BmI', [128, 16, 128], BF16)
            k.op('pool', 'memset', CmA[:], 0.0, writes=['CmA'])
            k.op('pool', 'memset', CmB[:], 0.0, writes=['CmB'])
            ctr, cti = sb('ctr', [128, 16, 16]), sb('cti', [128, 16, 16])
            stg = [sb('bstg%d' % i, [128, 128]) for i in range(2)]
            ct1, ct2 = sb('ct1', [128, 16]), sb('ct2', [128, 16])
            dcol = sb('s5dcol', [128, 4])
            k.dma('sp', dcol[:], I['s5_d'].rearrange("o g p -> o (g p)").rearrange("o (ft p) -> p (o ft)", p=128), writes=['s5dcol'],
                  allow_slow_non_contiguous=True)
            for gl in range(2):
                ps_ = slice(64 * gl, 64 * gl + 64)
                for pr in range(16):
                    k.checkpoint()
                    k.dma('sp', ctr[ps_, pr, :], I['s5_c_re'][0, 2 * pr + gl].rearrange("p n -> n p"), writes=['ctr'], allow_slow_non_contiguous=True)
                    k.dma('act', cti[ps_, pr, :], I['s5_c_im'][0, 2 * pr + gl].rearrange("p n -> n p"), writes=['cti'], allow_slow_non_contiguous=True)
            for pr in range(16):
                k.checkpoint()
                c0 = 32 * (pr % 4)
                k.op('dve', 'tensor_scalar', ct1[:], ctr[:, pr, :], V['cr'][:, pr:pr + 1], None, ALU.mult, reads=KS + ['ctr'], writes=['ct1'])
                k.op('dve', 'scalar_tensor_tensor', ct1[:], cti[:, pr, :], V['nci'][:, pr:pr + 1], ct1[:], ALU.mult, ALU.add,
                     reads=KS + ['cti', 'ct1'], writes=['ct1'])
                k.op('dve', 'tensor_scalar', ct2[:], ctr[:, pr, :], V['nci'][:, pr:pr + 1], None, ALU.mult, reads=KS + ['ctr'], writes=['ct2'])
                k.op('dve', 'scalar_tensor_tensor', ct2[:], cti[:, pr, :], V['ncr'][:, pr:pr + 1], ct2[:], ALU.mult, ALU.add,
                     reads=KS + ['cti', 'ct2'], writes=['ct2'])
                for gl in range(2):
                    ps_ = slice(64 * gl, 64 * gl + 64)
                    k.op('dve', 'tensor_copy', CmA[ps_, pr, c0 + 16 * gl:c0 + 16 * gl + 16], ct1[ps_, :], reads=['ct1', 'CmA'], writes=['CmA'])
                    k.op('dve', 'tensor_copy', CmB[ps_, pr, c0 + 16 * gl:c0 + 16 * gl + 16], ct2[ps_, :], reads=['ct2', 'CmB'], writes=['CmB'])
                for ri, (src, dstm, dk) in enumerate((('s5_b_re', BmR, 'BmR'), ('s5_b_im', BmI, 'BmI'))):
                    sg_, sk = stg[ri], 'bstg%d' % ri
                    k.op('pool', 'memset', sg_[:], 0.0, writes=[sk])
                    for gl in range(2):
                        r0 = c0 + 16 * gl
                        k.dma('sp', sg_[r0:r0 + 16, 64 * gl:64 * gl + 64], I[src][0, 2 * pr + gl].rearrange("n p -> p n"),
                              reads=[sk], writes=[sk], allow_slow_non_contiguous=True)
                    k.op('dve', 'tensor_copy', dstm[:, pr, :], sg_[:], reads=[sk], writes=[dk])
            itf = sb('s5itf', [128, T])
            with ExitStack() as stt:
                iti0 = self.sb(stt, 's5iti', [128, T], I32)
                k.op('pool', 'iota', iti0[:], pattern=[[1, T]], base=0, channel_multiplier=0, writes=['s5iti'])
                k.op('dve', 'tensor_copy', itf[:], iti0[:], reads=['s5iti'], writes=['s5itf'])
                k.barrier()
            Ct, St = sb('s5C', [128, T]), sb('s5S', [128, T])
            Xr, Xi = sb('s5Xr', [128, T]), sb('s5Xi', [128, T])
            yo = [sb('s5yo%d' % i, [128, T], BF16) for i in range(1)]
            yacc = sb('s5yacc', [128, T])
            tm = [sb('s5tm%d' % i, [128, TB]) for i in range(4)]
            Hr = [sb('s5Hr%d' % i, [128, TB], BF16) for i in range(2)]
            Hi = [sb('s5Hi%d' % i, [128, TB], BF16) for i in range(2)]
            for pr in range(16):
                k.checkpoint()
                ft = pr // 4
                k.op('dve', 'tensor_scalar', Xr[:], itf[:], V['th'][:, pr:pr + 1], None, ALU.mult, reads=KS + ['s5itf', 's5Xr'], writes=['s5Xr'])
                k.op('dve', 'tensor_scalar', Xi[:].bitcast(I32), Xr[:], 1.0 / (2 * PI), None, ALU.mult, reads=['s5Xr', 's5Xi'], writes=['s5Xi'])
                k.op('dve', 'scalar_tensor_tensor', Xr[:], Xi[:].bitcast(I32), -2 * PI, Xr[:], ALU.mult, ALU.add, reads=['s5Xi', 's5Xr'], writes=['s5Xr'])
                k.op('dve', 'tensor_scalar', Xr[:], Xr[:], PI, -PI, ALU.min, ALU.max, reads=['s5Xr'], writes=['s5Xr'])
                k.op('act', 'activation', St[:], Xr[:], AF.Sin, reads=['s5Xr', 's5S'], writes=['s5S'])
                k.op('dve', 'scalar_tensor_tensor', Xr[:], Xr[:], -1.0, Xr[:], ALU.mult, ALU.max, reads=['s5Xr', 's5S'], writes=['s5Xr'])
                k.op('act', 'activation', Ct[:], Xr[:], AF.Sin, scale=-1.0, bias=self.cst[:, 3:4], reads=['s5Xr', 's5C', 'cst'], writes=['s5C'])
                for tb in range(NB):
                    k.checkpoint()
                    tsl = slice(tb * TB, (tb + 1) * TB)
                    pA, pAk, pB, pBk = self.ps[(tb % 2) * 2], 'ps%d' % ((tb % 2) * 2), self.ps[(tb % 2) * 2 + 1], 'ps%d' % ((tb % 2) * 2 + 1)
                    k.op('pe', 'matmul', pA[:, 0:TB], BmR[:, pr, :], uT[:, ft, tsl], start=True, stop=True, reads=['BmR', 'uT'], writes=[pAk])
                    k.op('pe', 'matmul', pB[:, 0:TB], BmI[:, pr, :], uT[:, ft, tsl], start=True, stop=True, reads=['BmI', 'uT'], writes=[pBk])
                    k.op('dve', 'tensor_tensor', tm[0][:], pA[:, 0:TB], Ct[:, tsl], ALU.mult, reads=[pAk, 's5C'], writes=['s5tm0'])
                    k.op('dve', 'tensor_tensor', tm[1][:], pB[:, 0:TB], St[:, tsl], ALU.mult, reads=[pBk, 's5S'], writes=['s5tm1'])
                    k.op('pool', 'tensor_tensor', Xr[:, tsl], tm[0][:], tm[1][:], ALU.add, reads=['s5tm0', 's5tm1', 's5Xr'], writes=['s5Xr'])
                    k.op('dve', 'tensor_tensor', tm[2][:], pB[:, 0:TB], Ct[:, tsl], ALU.mult, reads=[pBk, 's5C'], writes=['s5tm2'])
                    k.op('dve', 'tensor_tensor', tm[3][:], pA[:, 0:TB], St[:, tsl], ALU.mult, reads=[pAk, 's5S'], writes=['s5tm3'])
                    k.op('pool', 'tensor_tensor', Xi[:, tsl], tm[2][:], tm[3][:], ALU.subtract, reads=['s5tm2', 's5tm3', 's5Xi'], writes=['s5Xi'])
                rb = V['rho'][:, pr:pr + 1].to_broadcast([128, T])
                k.op('dve', 'tensor_tensor_scan', Xr[:], rb, Xr[:], 0.0, ALU.mult, ALU.add, reads=KS + ['s5Xr'], writes=['s5Xr'])
                k.op('dve', 'tensor_tensor_scan', Xi[:], rb, Xi[:], 0.0, ALU.mult, ALU.add, reads=KS + ['s5Xi'], writes=['s5Xi'])
                for tb in range(NB):
                    k.checkpoint()
                    tsl = slice(tb * TB, (tb + 1) * TB)
                    b = tb % 2
                    k.op('dve', 'tensor_tensor', tm[0][:], Xr[:, tsl], Ct[:, tsl], ALU.mult, reads=['s5Xr', 's5C'], writes=['s5tm0'])
                    k.op('pool', 'tensor_tensor', tm[1][:], Xi[:, tsl], St[:, tsl], ALU.mult, reads=['s5Xi', 's5S'], writes=['s5tm1'])
                    k.op('dve', 'tensor_tensor', Hr[b][:], tm[0][:], tm[1][:], ALU.subtract, reads=['s5tm0', 's5tm1'], writes=['s5Hr%d' % b])
                    k.op('dve', 'tensor_tensor', tm[2][:], Xr[:, tsl], St[:, tsl], ALU.mult, reads=['s5Xr', 's5S'], writes=['s5tm2'])
                    k.op('pool', 'tensor_tensor', tm[3][:], Xi[:, tsl], Ct[:, tsl], ALU.mult, reads=['s5Xi', 's5C'], writes=['s5tm3'])
                    k.op('dve', 'tensor_tensor', Hi[b][:], tm[2][:], tm[3][:], ALU.add, reads=['s5tm2', 's5tm3'], writes=['s5Hi%d' % b])
                    p, pk = self.ps[4 + b], 'ps%d' % (4 + b)
                    k.op('pe', 'matmul', p[:, 0:TB], CmA[:, pr, :], Hr[b][:], start=True, stop=False, reads=['CmA', 's5Hr%d' % b], writes=[pk])
                    k.op('pe', 'matmul', p[:, 0:TB], CmB[:, pr, :], Hi[b][:], start=False, stop=True, reads=['CmB', 's5Hi%d' % b], writes=[pk])
                    if pr % 4 == 0:
                        k.op('dve', 'scalar_tensor_tensor', yacc[:, tsl], uT[:, ft, tsl], dcol[:, ft:ft + 1], p[:, 0:TB], ALU.mult, ALU.add,
                             reads=['uT', 's5dcol', pk, 's5yacc'], writes=['s5yacc'])
                    else:
                        k.op('dve', 'tensor_tensor', yacc[:, tsl], yacc[:, tsl], p[:, 0:TB], ALU.add, reads=[pk, 's5yacc'], writes=['s5yacc'])
                if pr % 4 == 3:
                    k.op('pool', 'tensor_tensor', Xr[:], yacc[:], yacc[:], ALU.mult, reads=['s5yacc', 's5Xr'], writes=['s5Xr'])
                    k.op('dve', 'tensor_scalar', Xr[:], Xr[:], 0.044715, 1.0, ALU.mult, ALU.add, reads=['s5Xr'], writes=['s5Xr'])
                    k.op('pool', 'tensor_tensor', Xr[:], Xr[:], yacc[:], ALU.mult, reads=['s5yacc', 's5Xr'], writes=['s5Xr'])
                    k.op('act', 'activation', Xr[:], Xr[:], AF.Sigmoid, scale=1.5957691216, reads=['s5Xr'], writes=['s5Xr'])
                    k.op('dve', 'tensor_tensor', yo[0][:], Xr[:], yacc[:], ALU.mult, reads=['s5Xr', 's5yacc', 's5yo'], writes=['s5yo'])
                    k.dma('sp', ya_d[ft], yo[0][:], reads=['s5yo'], writes=['ya_d'])
            k.barrier()


    def outproj(self, wname, xin, xout, fill):
        k, T, TB, NB, TPB = self.k, self.T, self.TB, self.NB, self.TPB
        with ExitStack() as st:
            wout = self.sb(st, 'wout', [128, 8, 1024], BF16)
            wov = self.I[wname][0].rearrange("(kc k) n -> k kc n", k=128)
            for kc in range(8):
                k.dma('pool', wout[:, kc, :], wov[:, kc, :], writes=['wout'])
            ymT = [self.sb(st, 'ymT%d' % i, [128, 8, TB], BF16) for i in range(2)]
            xt = [self.sb(st, 'oxt%d' % i, [128, D]) for i in range(2)]
            tmp = [self.sb(st, 'otmp%d' % i, [128, D]) for i in range(2)]
            cnt = 0
            for tb in range(NB):
                k.checkpoint()
                ym, ymk = ymT[tb % 2], 'ymT%d' % (tb % 2)
                fill(st, tb, ym, ymk)
                for j in range(TPB):
                    tile = tb * TPB + j
                    cnt += 1
                    b = cnt % 2
                    k.dma('sp', xt[b][:], xin[tile * 128:(tile + 1) * 128, :], reads=['xres'], writes=['oxt%d' % b])
                    for nh in range(2):
                        p, pk = self.ps[4 + 2 * b + nh], 'ps%d' % (4 + 2 * b + nh)
                        for kc in range(8):
                            k.op('pe', 'matmul', p[:], ym[:, kc, j * 128:(j + 1) * 128], wout[:, kc, nh * 512:(nh + 1) * 512],
                                 start=(kc == 0), stop=(kc == 7), reads=[ymk, 'wout'], writes=[pk])
                        k.op('dve', 'tensor_tensor', tmp[b][:, nh * 512:(nh + 1) * 512], p[:], self.modbc[:, 2 * D + nh * 512:2 * D + (nh + 1) * 512],
                             ALU.mult, reads=[pk, 'modbc'], writes=['otmp%d' % b])
                    k.op('pool', 'tensor_tensor', tmp[b][:], tmp[b][:], xt[b][:], ALU.add, reads=['otmp%d' % b, 'oxt%d' % b], writes=['otmp%d' % b])
                    k.dma('sp', xout[tile * 128:(tile + 1) * 128, :], tmp[b][:], reads=['otmp%d' % b], writes=['xres2'])
            k.barrier()

    def odd_mixer(self, xin, xout):
        k, nc, T, NT, TB, NB, TPB = self.k, self.nc, self.T, self.NT, self.TB, self.NB, self.TPB
        I = self.I
        pT = self.scr('projT', [27, 128, T])
        F5 = self.scr('F5', [20, 128, T])
        vT_d = self.scr('vT_d', [4, 128, T])
        gT_d = self.scr('gT_d', [4, 128, T])
        bon_d = self.scr('bon_d', [4, 128, T])
        yr_d = self.scr('yr_d', [4, 128, T])
        ys_d = self.scr('ys_d', [8, 64, T])
        ymr_d = self.scr('ymr_d', [4, 128, T], BF16)
        yms_d = self.scr('yms_d', [4, 128, T], BF16)

        def col(st, name, src, n):
            t = self.sb(st, name, [128, n])
            k.dma('sp', t[:], src, writes=[name], allow_slow_non_contiguous=True)
            return t
        with ExitStack() as st:
            win = self.sb(st, 'owin', [128, 8, 3336], BF16)
            wv = I['odd_w_in'][0].rearrange("(kc k) n -> k kc n", k=128)
            for kc in range(8):
                k.dma('pool', win[:, kc, :], wv[:, kc, :], writes=['owin'])
            bufs = self.norm_bufs(st)
            hT = [self.sb(st, 'hT%d' % i, [128, 8, TB], BF16) for i in range(2)]
            stg = [self.sb(st, 'pstg%d' % i, [128, TB]) for i in range(4)]
            cnt = 0
            for tb in range(NB):
                k.checkpoint()
                hb, hk = hT[tb % 2], 'hT%d' % (tb % 2)
                for j in range(TPB):
                    self.norm_tile(bufs, xin, tb * TPB + j, self.modbc[:, D:2 * D], self.modbc[:, 0:D], hb[:, :, j * 128:(j + 1) * 128], keys=[hk])
                tsl = slice(tb * TB, (tb + 1) * TB)
                for mt in range(27):
                    M = 128 if mt < 26 else 8
                    cnt += 1
                    pi = 4 + cnt % 4
                    p, pk = self.ps[pi], 'ps%d' % pi
                    sg_, sk = stg[cnt % 4], 'pstg%d' % (cnt % 4)
                    for kc in range(8):
                        k.op('pe', 'matmul', p[0:M, 0:TB], win[:, kc, mt * 128:mt * 128 + M], hb[:, kc, :], start=(kc == 0), stop=(kc == 7),
                             reads=['owin', hk], writes=[pk])
                    if cnt % 2:
                        k.op('act', 'activation', sg_[0:M, :], p[0:M, 0:TB], AF.Copy, reads=[pk], writes=[sk])
                    else:
                        k.op('dve', 'tensor_copy', sg_[0:M, :], p[0:M, 0:TB], reads=[pk], writes=[sk])
                    k.dma('sp' if cnt % 2 else 'act', pT[mt, 0:M, tsl], sg_[0:M, :], reads=[sk], writes=['projT'])
            k.barrier()
        with ExitStack() as st:
            sb = lambda n, s, d=F32: self.sb(st, n, s, d)
            mu = col(st, 'rwmu', I['rw_mu'].rearrange("o (mt p) -> p (o mt)", p=128), 14)
            omu = sb('rwomu', [128, 14])
            k.op('dve', 'tensor_scalar', omu[:], mu[:], -1.0, 1.0, ALU.mult, ALU.add, reads=['rwmu'], writes=['rwomu'])
            cw0 = col(st, 'rww0', I['rw_w0'].rearrange("o (mt p) -> p (o mt)", p=128), 4)
            ca0 = col(st, 'rwa0', I['rw_a0'].rearrange("o (mt p) -> p (o mt)", p=128), 4)
            ckk = col(st, 'rwkk', I['rw_k_k'].rearrange("o (mt p) -> p (o mt)", p=128), 4)
            cka = col(st, 'rwka', I['rw_k_a'].rearrange("o (mt p) -> p (o mt)", p=128), 4)
            crk = col(st, 'rwrk', I['rw_r_k'].rearrange("o h n -> o (h n)").rearrange("o (mt p) -> p (o mt)", p=128), 4)
            omka = sb('rwomka', [128, 4])
            k.op('dve', 'tensor_scalar', omka[:], cka[:], -1.0, 1.0, ALU.mult, ALU.add, reads=['rwka'], writes=['rwomka'])
            w2 = sb('rww2', [64, 512], BF16)
            a2 = sb('rwa2', [128, 512], BF16)
            g2 = sb('rwg2', [128, 512], BF16)
            k.dma('pool', w2[:], I['rw_w2'][0], writes=['rww2'])
            k.dma('pool', a2[64:128, :], I['rw_a2'][0], writes=['rwa2'])
            k.dma('pool', g2[:], I['rw_g2'][0], writes=['rwg2'])
            P = [sb('rwP%d' % i, [128, TB]) for i in range(14)]
            psh = [sb('rwsh%d' % i, [128, TB]) for i in range(2)]
            thb = sb('rwth', [128, TB], BF16)
            sgb = sb('rwsg', [128, TB], BF16)
            tA, tB_, tC, tD = sb('rwtA', [128, TB]), sb('rwtB', [128, TB]), sb('rwtC', [128, TB]), sb('rwtD', [128, TB])
            aT = sb('rwaT', [128, TB])
            PK = ['rwP']
            for tb in range(NB):
                k.checkpoint()
                t0 = tb * TB
                tsl = slice(t0, t0 + TB)
                for mt in range(14):
                    sh, shk = psh[mt % 2], 'rwsh%d' % (mt % 2)
                    k.dma('sp', P[mt][:], pT[mt, :, tsl], reads=['projT'], writes=['rwP%d' % mt])
                    if tb == 0:
                        k.op('pool', 'memset', sh[:, 0:1], 0.0, writes=[shk])
                        k.dma('act', sh[:, 1:TB], pT[mt, :, 0:TB - 1], reads=['projT', shk], writes=[shk])
                    else:
                        k.dma('act', sh[:, :], pT[mt, :, t0 - 1:t0 + TB - 1], reads=['projT'], writes=[shk])
                    k.op('dve', 'tensor_scalar', P[mt][:], P[mt][:], omu[:, mt:mt + 1], None, ALU.mult, reads=['rwP%d' % mt, 'rwomu'], writes=['rwP%d' % mt])
                    k.op('dve', 'scalar_tensor_tensor', P[mt][:], sh[:], mu[:, mt:mt + 1], P[mt][:], ALU.mult, ALU.add,
                         reads=[shk, 'rwmu', 'rwP%d' % mt], writes=['rwP%d' % mt])
                Pk = lambda i: 'rwP%d' % i
                k.op('act', 'activation', thb[0:64, :], P[12][0:64, :], AF.Tanh, reads=[Pk(12)], writes=['rwth'])
                k.op('act', 'activation', thb[64:128, :], P[12][64:128, :], AF.Copy, reads=[Pk(12), 'rwth'], writes=['rwth'])
                k.op('act', 'activation', sgb[:], P[13][:], AF.Sigmoid, reads=[Pk(13)], writes=['rwsg'])
                for ft in range(4):
                    fsl = slice(ft * 128, (ft + 1) * 128)
                    r_, k_, v_ = P[ft], P[4 + ft], P[8 + ft]
                    p0, p1, p2, p3 = self.ps[0], self.ps[1], self.ps[2], self.ps[3]
                    k.op('pe', 'matmul', p0[:, 0:TB], w2[0:64, fsl], thb[0:64, :], start=True, stop=True, reads=['rww2', 'rwth'], writes=['ps0'])
                    k.op('act', 'activation', tA[:], p0[:, 0:TB], AF.Sigmoid, bias=cw0[:, ft:ft + 1], reads=['ps0', 'rww0', 'rwtA'], writes=['rwtA'])
                    k.op('act', 'activation', tA[:], tA[:], AF.Exp, scale=-0.6065306597, reads=['rwtA'], writes=['rwtA'])
                    k.dma('sp', F5[4 + ft, :, tsl], tA[:], reads=['rwtA'], writes=['F5'])
                    k.op('pe', 'matmul', p1[:, 0:TB], a2[64:128, fsl], thb[64:128, :], start=True, stop=True, reads=['rwa2', 'rwth'], writes=['ps1'])
                    k.op('act', 'activation', aT[:], p1[:, 0:TB], AF.Sigmoid, bias=ca0[:, ft:ft + 1], reads=['ps1', 'rwa0', 'rwaT'], writes=['rwaT'])
                    k.op('pe', 'matmul', p2[:, 0:TB], g2[:, fsl], sgb[:], start=True, stop=True, reads=['rwg2', 'rwsg'], writes=['ps2'])
                    k.op('act', 'activation', tB_[:], p2[:, 0:TB], AF.Copy, reads=['ps2', 'rwtB'], writes=['rwtB'])
                    k.dma('act', gT_d[ft, :, tsl], tB_[:], reads=['rwtB'], writes=['gT_d'])
                    k.op('dve', 'tensor_scalar', tC[:], k_[:], ckk[:, ft:ft + 1], None, ALU.mult, reads=[Pk(4 + ft), 'rwkk', 'rwtC'], writes=['rwtC'])
                    k.op('pool', 'tensor_tensor', tD[:], tC[:], tC[:], ALU.mult, reads=['rwtC', 'rwtD'], writes=['rwtD'])
                    k.op('pe', 'matmul', p3[:, 0:TB], self.blk[:], tD[:], start=True, stop=True, reads=['blk', 'rwtD'], writes=['ps3'])
                    k.op('act', 'activation', tD[:], p3[:, 0:TB], AF.Sqrt, reads=['ps3', 'rwtD'], writes=['rwtD'])
                    k.op('dve', 'tensor_scalar', tD[:], tD[:], 1e-12, None, ALU.max, reads=['rwtD'], writes=['rwtD'])
                    k.op('dve', 'reciprocal', tD[:], tD[:], reads=['rwtD'], writes=['rwtD'])
                    k.op('dve', 'tensor_tensor', tC[:], tC[:], tD[:], ALU.mult, reads=['rwtC', 'rwtD'], writes=['rwtC'])
                    k.op('dve', 'tensor_tensor', tD[:], tC[:], aT[:], ALU.mult, reads=['rwtC', 'rwaT', 'rwtD'], writes=['rwtD'])
                    k.dma('sp', F5[8 + ft, :, tsl], tD[:], reads=['rwtD'], writes=['F5'])
                    k.op('dve', 'tensor_scalar', tC[:], tC[:], -1.0, None, ALU.mult, reads=['rwtC'], writes=['rwtC'])
                    k.dma('act', F5[0 + ft, :, tsl], tC[:], reads=['rwtC'], writes=['F5'])
                    k.op('dve', 'tensor_scalar', aT[:], aT[:], cka[:, ft:ft + 1], omka[:, ft:ft + 1], ALU.mult, ALU.add, reads=['rwaT', 'rwka', 'rwomka'], writes=['rwaT'])
                    k.op('dve', 'tensor_tensor', k_[:], k_[:], aT[:], ALU.mult, reads=[Pk(4 + ft), 'rwaT'], writes=[Pk(4 + ft)])
                    k.dma('sp', F5[12 + ft, :, tsl], k_[:], reads=[Pk(4 + ft)], writes=['F5'])
                    k.dma('act', F5[16 + ft, :, tsl], r_[:], reads=[Pk(ft)], writes=['F5'])
                    k.dma('sp', vT_d[ft, :, tsl], v_[:], reads=[Pk(8 + ft)], writes=['vT_d'])
                    k.op('dve', 'scalar_tensor_tensor', tA[:], r_[:], crk[:, ft:ft + 1], k_[:], ALU.mult, ALU.mult, reads=[Pk(ft), Pk(4 + ft), 'rwrk', 'rwtA'], writes=['rwtA'])
                    k.op('pe', 'matmul', p0[:, 0:TB], self.blk[:], tA[:], start=True, stop=True, reads=['blk', 'rwtA'], writes=['ps0'])
                    k.op('dve', 'tensor_tensor', tB_[:], p0[:, 0:TB], v_[:], ALU.mult, reads=['ps0', Pk(8 + ft), 'rwtB'], writes=['rwtB'])
                    k.dma('act', bon_d[ft, :, tsl], tB_[:], reads=['rwtB'], writes=['bon_d'])
            k.barrier()
        with ExitStack() as st:
            sb = lambda n, s, d=F32: self.sb(st, n, s, d)
            SEL = sb('rwSEL', [128, 64, 128], BF16)
            k.op('pool', 'memset', SEL[:], 0.0, writes=['rwSEL'])
            k.op('pool', 'memset', SEL[0:64, :, 0:64], 1.0, reads=['rwSEL'], writes=['rwSEL'])
            k.op('pool', 'memset', SEL[64:128, :, 64:128], 1.0, reads=['rwSEL'], writes=['rwSEL'])
            k.op('pool', 'affine_select', SEL[0:64, :, 0:64], SEL[0:64, :, 0:64], pattern=[[1, 64], [0, 64]], compare_op=ALU.is_equal,
                 fill=0.0, base=0, channel_multiplier=-1, reads=['rwSEL'], writes=['rwSEL'])
            k.op('pool', 'affine_select', SEL[64:128, :, 64:128], SEL[64:128, :, 64:128], pattern=[[1, 64], [0, 64]], compare_op=ALU.is_equal,
                 fill=0.0, base=0, channel_multiplier=-1, reads=['rwSEL'], writes=['rwSEL'])
            Fb = [sb('rwF%d' % i, [128, 20, 128]) for i in range(2)]
            vcb = [sb('rwvc%d' % i, [128, 4, 64]) for i in range(2)]
            RBh = [sb('rwRh%d' % i, [128, 20, 64], BF16) for i in range(2)]
            RBl = [sb('rwRl%d' % i, [128, 20, 64], BF16) for i in range(2)]
            yb = [sb('rwyb%d' % i, [128, 4, 64]) for i in range(2)]
            S = sb('rwS', [128, 4, 64])
            tmp = sb('rwtmp', [128, 4, 64])
            sa = sb('rwsa', [128, 4])
            k.op('dve', 'memset', S[:], 0.0, writes=['rwS'])
            SK = ['rwS']
            F5v = F5.rearrange("i p t -> p i t")
            for tl in range(T // 64):
                k.checkpoint()
                b = tl % 2
                tsl = slice(tl * 64, tl * 64 + 64)
                Fk, vk, Rk, yk = 'rwF%d' % b, 'rwvc%d' % b, 'rwR%d' % b, 'rwyb%d' % b
                for g4 in range(5):
                    k.dma('sp', Fb[b][:, g4 * 4:g4 * 4 + 4, 0:64], F5v[:, g4 * 4:g4 * 4 + 4, tsl], reads=['F5', Fk], writes=[Fk])
                    k.dma('act', Fb[b][:, g4 * 4:g4 * 4 + 4, 64:128], F5v[:, g4 * 4:g4 * 4 + 4, tsl], reads=['F5', Fk], writes=[Fk])
                k.dma('sp', vcb[b][:], vT_d.rearrange("i p t -> p i t")[:, :, tsl], reads=['vT_d'], writes=[vk])
                for g4 in range(5):
                    p, pk = self.ps[g4 % 2], 'ps%d' % (g4 % 2)
                    for j in range(4):
                        i = g4 * 4 + j
                        k.op('pe', 'transpose', p[:, j * 128:(j + 1) * 128], Fb[b][:, i, :], self.ident[:], reads=[Fk, 'ident'], writes=[pk])
                    pv = p[:].rearrange("p (a b) -> p a b", a=4)
                    for hp in range(2):
                        ps_ = slice(64 * hp, 64 * hp + 64)
                        k.op('act', 'activation', RBh[b][ps_, g4 * 4:g4 * 4 + 4, :], pv[ps_, :, 64 * hp:64 * hp + 64], AF.Copy, reads=[pk, Rk], writes=[Rk])
                    for hp in range(2):
                        ps_ = slice(64 * hp, 64 * hp + 64)
                        k.op('dve', 'tensor_tensor', RBl[b][ps_, g4 * 4:g4 * 4 + 4, :], pv[ps_, :, 64 * hp:64 * hp + 64], RBh[b][ps_, g4 * 4:g4 * 4 + 4, :],
                             ALU.subtract, reads=[pk, Rk], writes=[Rk])
                for s_ in range(64):
                    bs = s_ % 2
                    banks = [(self.ps[2 + 3 * bs + i], 'ps%d' % (2 + 3 * bs + i)) for i in range(3)]
                    for bi, (i0, n) in enumerate(((0, 8), (8, 8), (16, 4))):
                        p, pk = banks[bi]
                        w_ = n * 64
                        k.op('pe', 'matmul', p[:, 0:w_], SEL[:, s_, :], RBh[b][:, i0:i0 + n, :], start=True, stop=False, reads=['rwSEL', Rk], writes=[pk])
                        k.op('pe', 'matmul', p[:, 0:w_], SEL[:, s_, :], RBl[b][:, i0:i0 + n, :], start=False, stop=True, reads=['rwSEL', Rk], writes=[pk])
                    v3 = lambda p, c0: p[:, c0:c0 + 256].rearrange("p (a b) -> p a b", a=4)
                    a_bc, d_bc = v3(banks[0][0], 0), v3(banks[0][0], 256)
                    b_bc, k_bc = v3(banks[1][0], 0), v3(banks[1][0], 256)
                    r_bc = v3(banks[2][0], 0)
                    k0, k1, k2 = banks[0][1], banks[1][1], banks[2][1]
                    k.op('dve', 'tensor_tensor', tmp[:], S[:], a_bc, ALU.mult, reads=SK + [k0, 'rwtmp'], writes=['rwtmp'])
                    k.op('dve', 'tensor_reduce', sa[:], tmp[:], AX.X, ALU.add, reads=['rwtmp', 'rwsa'], writes=['rwsa'])
                    k.op('dve', 'tensor_tensor', S[:], S[:], d_bc, ALU.mult, reads=SK + [k0], writes=SK)
                    for hq in range(4):
                        k.op('dve', 'scalar_tensor_tensor', S[:, hq, :], b_bc[:, hq, :], sa[:, hq:hq + 1], S[:, hq, :], ALU.mult, ALU.add,
                             reads=SK + [k1, 'rwsa'], writes=SK)
                    for hq in range(4):
                        k.op('dve', 'scalar_tensor_tensor', S[:, hq, :], k_bc[:, hq, :], vcb[b][:, hq, s_:s_ + 1], S[:, hq, :], ALU.mult, ALU.add,
                             reads=SK + [k1, vk], writes=SK)
                    k.op('dve', 'tensor_tensor', tmp[:], S[:], r_bc, ALU.mult, reads=SK + [k2, 'rwtmp'], writes=['rwtmp'])
                    k.op('dve', 'tensor_reduce', yb[b][:, :, s_:s_ + 1], tmp[:], AX.X, ALU.add, reads=['rwtmp', yk], writes=[yk])
                k.dma('sp', yr_d.rearrange("i p t -> p i t")[:, :, tsl], yb[b][:], reads=[yk], writes=['yr_d'])
            k.barrier()
        with ExitStack() as st:
            sb = lambda n, s, d=F32: self.sb(st, n, s, d)
            lnw = col(st, 'rwlnw', I['rw_ln_w'].rearrange("o (mt p) -> p (o mt)", p=128), 4)
            lnb = col(st, 'rwlnb', I['rw_ln_b'].rearrange("o (mt p) -> p (o mt)", p=128), 4)
            blks = sb('blks', [128, 128])
            k.op('dve', 'tensor_scalar', blks[:], self.blk[:], 1.0 / 64, None, ALU.mult, reads=['blk'], writes=['blks'])
            Y = [sb('c2y%d' % i, [128, TB]) for i in range(2)]
            Q = [sb('c2q%d' % i, [128, TB]) for i in range(2)]
            Bn = [sb('c2b%d' % i, [128, TB]) for i in range(2)]
            G = [sb('c2g%d' % i, [128, TB]) for i in range(2)]
            O = [sb('c2o%d' % i, [128, TB], BF16) for i in range(2)]
            it = 0
            for tb in range(NB):
                k.checkpoint()
                tsl = slice(tb * TB, (tb + 1) * TB)
                for ft in range(4):
                    it += 1
                    b = it % 2
                    yk, qk, bk, gk, ok = 'c2y%d' % b, 'c2q%d' % b, 'c2b%d' % b, 'c2g%d' % b, 'c2o%d' % b
                    p0, p0k, p1, p1k = self.ps[b], 'ps%d' % b, self.ps[2 + b], 'ps%d' % (2 + b)
                    k.dma('sp', Y[b][:], yr_d[ft, :, tsl], reads=['yr_d'], writes=[yk])
                    k.dma('act', Bn[b][:], bon_d[ft, :, tsl], reads=['bon_d'], writes=[bk])
                    k.dma('sp', G[b][:], gT_d[ft, :, tsl], reads=['gT_d'], writes=[gk])
                    k.op('pe', 'matmul', p0[:, 0:TB], blks[:], Y[b][:], start=True, stop=True, reads=['blks', yk], writes=[p0k])
                    k.op('dve', 'tensor_tensor', Y[b][:], Y[b][:], p0[:, 0:TB], ALU.subtract, reads=[yk, p0k], writes=[yk])
                    k.op('pool', 'tensor_tensor', Q[b][:], Y[b][:], Y[b][:], ALU.mult, reads=[yk, qk], writes=[qk])
                    k.op('pe', 'matmul', p1[:, 0:TB], blks[:], Q[b][:], start=True, stop=True, reads=['blks', qk], writes=[p1k])
                    k.op('dve', 'tensor_scalar', Q[b][:], p1[:, 0:TB], 64e-5, None, ALU.add, reads=[p1k, qk], writes=[qk])
                    k.op('act', 'activation', Q[b][:], Q[b][:], AF.Sqrt, reads=[qk], writes=[qk])
                    k.op('dve', 'reciprocal', Q[b][:], Q[b][:], reads=[qk], writes=[qk])
                    k.op('dve', 'tensor_tensor', Y[b][:], Y[b][:], Q[b][:], ALU.mult, reads=[yk, qk], writes=[yk])
                    k.op('dve', 'tensor_scalar', Y[b][:], Y[b][:], lnw[:, ft:ft + 1], lnb[:, ft:ft + 1], ALU.mult, ALU.add, reads=[yk, 'rwlnw', 'rwlnb'], writes=[yk])
                    k.op('pool', 'tensor_tensor', Y[b][:], Y[b][:], Bn[b][:], ALU.add, reads=[yk, bk], writes=[yk])
                    k.op('dve', 'tensor_tensor', O[b][:], Y[b][:], G[b][:], ALU.mult, reads=[yk, gk, ok], writes=[ok])
                    k.dma('act', ymr_d[ft, :, tsl], O[b][:], reads=[ok], writes=['ymr_d'])
            k.barrier()
        self.ssd(pT, ys_d, yms_d)

        def fill(st, tb, ym, ymk):
            tsl = slice(tb * TB, (tb + 1) * TB)
            k.dma('sp', ym[:, 0:4, :], ymr_d.rearrange("i p t -> p i t")[:, :, tsl], reads=['ymr_d'], writes=[ymk])
            k.dma('act', ym[:, 4:8, :], yms_d.rearrange("i p t -> p i t")[:, :, tsl], reads=['yms_d'], writes=[ymk])
        self.outproj('odd_w_out', xin, xout, fill)

    def ssd(self, pT, ys_d, yms_d):
        k, nc, T, NT, TB, NB, TPB = self.k, self.nc, self.T, self.NT, self.TB, self.NB, self.TPB
        I = self.I
        xc_d = self.scr('xc_d', [4, 128, T])
        with ExitStack() as st0:
            sb0 = lambda n, s, d=F32: self.sb(st0, n, s, d)
            BT, CT = sb0('ssBT', [128, 2, T], BF16), sb0('ssCT', [128, 2, T], BF16)
            xdt = sb0('ssxdt', [128, NT, 512], BF16)
            cs = sb0('sscs', [8, T])
            dtT = sb0('ssdt', [8, T])
            ncsT = sb0('ssncsT', [128, NT, 8])
            dtk = sb0('ssdtk', [128, NT, 8])
            with ExitStack() as st:
                sb = lambda n, s, d=F32: self.sb(st, n, s, d)
                cw = sb('sscw', [128, 8, 4])
                for w in range(4):
                    k.dma('sp', cw[:, :, w], I['m2_conv_w'][0, w:w + 1, :].rearrange("o (mt p) -> p (o mt)", p=128), reads=['sscw'], writes=['sscw'],
                          allow_slow_non_contiguous=True)
                cb = sb('sscb', [128, 8])
                k.dma('sp', cb[:], I['m2_conv_b'].rearrange("o (mt p) -> p (o mt)", p=128), writes=['sscb'], allow_slow_non_contiguous=True)
                dtb = sb('ssdtb', [8, 1])
                alog = sb('ssalog', [8, 1])
                k.dma('sp', dtb[:], I['m2_dt_bias'].rearrange("o h -> h o"), writes=['ssdtb'], allow_slow_non_contiguous=True)
                k.dma('sp', alog[:], I['m2_a_log'].rearrange("o h -> h o"), writes=['ssalog'], allow_slow_non_contiguous=True)
                k.op('act', 'activation', alog[:], alog[:], AF.Exp, reads=['ssalog'], writes=['ssalog'])
                k.op('dve', 'tensor_scalar', alog[:], alog[:], -1.0, None, ALU.mult, reads=['ssalog'], writes=['ssalog'])
                k.dma('sp', dtT[:], pT[26, 0:8, :], reads=['projT'], writes=['ssdt'])
                k.op('act', 'activation', dtT[:], dtT[:], AF.Exp, bias=dtb[:, 0:1], reads=['ssdt', 'ssdtb'], writes=['ssdt'])
                k.op('act', 'activation', dtT[:], dtT[:], AF.Ln, bias=1.0, reads=['ssdt'], writes=['ssdt'])
                k.op('dve', 'tensor_scalar', cs[:], dtT[:], alog[:, 0:1], None, ALU.mult, reads=['ssdt', 'ssalog'], writes=['sscs'])
                k.op('dve', 'tensor_tensor_scan', cs[:], self.cst[0:8, 1:2].to_broadcast([8, T]), cs[:], 0.0, ALU.mult, ALU.add, reads=['sscs', 'cst'], writes=['sscs'])
                for tl in range(NT):
                    p, pk = self.ps[tl % 2], 'ps%d' % (tl % 2)
                    k.op('pe', 'transpose', p[:, 0:8], cs[0:8, tl * 128:(tl + 1) * 128], self.ident[0:8, 0:8], reads=['sscs', 'ident'], writes=[pk])
                    k.op('pe', 'transpose', p[:, 8:16], dtT[0:8, tl * 128:(tl + 1) * 128], self.ident[0:8, 0:8], reads=['ssdt', 'ident'], writes=[pk])
                    k.op('dve', 'tensor_scalar', ncsT[:, tl, :], p[:, 0:8], -1.0, None, ALU.mult, reads=[pk], writes=['ssncsT'])
                    k.op('dve', 'tensor_copy', dtk[:, tl, :], p[:, 8:16], reads=[pk], writes=['ssdtk'])
                inb = [sb('ssin%d' % i, [128, TB + 3]) for i in range(2)]
                acc = [sb('ssacc%d' % i, [128, TB]) for i in range(2)]
                xcb = [sb('ssxc%d' % i, [128, TB]) for i in range(2)]
                it = 0
                for tb in range(NB):
                    k.checkpoint()
                    t0 = tb * TB
                    tsl = slice(t0, t0 + TB)
                    for c8 in range(8):
                        it += 1
                        b = it % 2
                        ik, ak, xk = 'ssin%d' % b, 'ssacc%d' % b, 'ssxc%d' % b
                        mt = 18 + c8
                        if tb == 0:
                            k.op('pool', 'memset', inb[b][:, 0:3], 0.0, writes=[ik])
                            k.dma('sp', inb[b][:, 3:TB + 3], pT[mt, :, 0:TB], reads=['projT', ik], writes=[ik])
                        else:
                            k.dma('sp', inb[b][:], pT[mt, :, t0 - 3:t0 + TB], reads=['projT'], writes=[ik])
                        k.op('dve', 'tensor_scalar', acc[b][:], inb[b][:, 3:TB + 3], cw[:, c8, 3:4], None, ALU.mult, reads=[ik, 'sscw', ak], writes=[ak])
                        for w in range(3):
                            k.op('dve', 'scalar_tensor_tensor', acc[b][:], inb[b][:, w:w + TB], cw[:, c8, w:w + 1], acc[b][:], ALU.mult, ALU.add,
                                 reads=[ik, 'sscw', ak], writes=[ak])
                        if c8 < 4:
                            k.op('act', 'activation', xcb[b][:], acc[b][:], AF.Silu, bias=cb[:, c8:c8 + 1], reads=[ak, 'sscb', xk], writes=[xk])
                            k.dma('act', xc_d[c8, :, tsl], xcb[b][:], reads=[xk], writes=['xc_d'])
                            for j in range(TPB):
                                tl = tb * TPB + j
                                p, pk = self.ps[2 + (it + j) % 2], 'ps%d' % (2 + (it + j) % 2)
                                k.op('pe', 'transpose', p[:, 0:128], xcb[b][:, j * 128:(j + 1) * 128], self.ident[:], reads=[xk, 'ident'], writes=[pk])
                                k.op('dve', 'tensor_tensor', xdt[:, tl, c8 * 128:(c8 + 1) * 128].rearrange("p (a b) -> p a b", a=2),
                                     p[:, 0:128].rearrange("p (a b) -> p a b", a=2),
                                     dtk[:, tl, 2 * c8:2 * c8 + 2].unsqueeze(2).to_broadcast([128, 2, 64]), ALU.mult, reads=[pk, 'ssdtk'], writes=['ssxdt'])
                        else:
                            dst = BT if c8 < 6 else CT
                            k.op('act', 'activation', dst[:, c8 % 2, tsl], acc[b][:], AF.Silu, bias=cb[:, c8:c8 + 1], reads=[ak, 'sscb'], writes=['ssBT' if c8 < 6 else 'ssCT'])
                k.barrier()
            with ExitStack() as st:
                sb = lambda n, s, d=F32: self.sb(st, n, s, d)
                csb = [sb('sscsb%d' % i, [128, TB]) for i in range(2)]
                dec = [sb('ssdec%d' % i, [128, TB]) for i in range(2)]
                Gm = [sb('ssG%d' % i, [128, TB], BF16) for i in range(2)]
                YO = [sb('ssY%d' % i, [64, TB]) for i in range(2)]
                it = 0
                hq = 0
                for h in range(8):
                    g = h // 4
                    for qb in range(NB):
                        k.checkpoint()
                        hq += 1
                        u = hq % 2
                        qsl = slice(qb * TB, (qb + 1) * TB)
                        pc, pck = self.ps[6 + u], 'ps%d' % (6 + u)
                        pb_, pbk = self.ps[4 + u], 'ps%d' % (4 + u)
                        k.op('pe', 'matmul', pb_[:, 0:TB], self.ident[0:8, h:h + 1].to_broadcast([8, 128]), cs[0:8, qsl], start=True, stop=True,
                             reads=['ident', 'sscs'], writes=[pbk])
                        k.op('act', 'activation', csb[u][:], pb_[:, 0:TB], AF.Copy, reads=[pbk], writes=['sscsb%d' % u])
                        nk = (qb + 1) * TPB
                        for kt in range(nk):
                            it += 1
                            b = it % 2
                            m = kt - qb * TPB
                            pz, pzk = self.ps[b], 'ps%d' % b
                            ksl = slice(kt * 128, (kt + 1) * 128)
                            k.op('pe', 'matmul', pz[:, 0:TB], BT[:, g, ksl], CT[:, g, qsl], start=True, stop=True, reads=['ssBT', 'ssCT'], writes=[pzk])
                            k.op('dve', 'tensor_scalar', dec[b][:], csb[u][:], ncsT[:, kt, h:h + 1], 0.0, ALU.add, ALU.min, reads=['sscsb%d' % u, 'ssncsT', 'ssdec%d' % b], writes=['ssdec%d' % b])
                            k.op('act', 'activation', dec[b][:], dec[b][:], AF.Exp, reads=['ssdec%d' % b], writes=['ssdec%d' % b])
                            if m >= 0:
                                k.op('pool', 'affine_select', dec[b][:], dec[b][:], pattern=[[1, TB]], compare_op=ALU.is_ge, fill=0.0,
                                     base=-128 * m, channel_multiplier=-1, reads=['ssdec%d' % b], writes=['ssdec%d' % b])
                            k.op('dve', 'tensor_tensor', Gm[b][:], pz[:, 0:TB], dec[b][:], ALU.mult, reads=[pzk, 'ssdec%d' % b, 'ssG%d' % b], writes=['ssG%d' % b])
                            k.op('pe', 'matmul', pc[0:64, 0:TB], xdt[:, kt, h * 64:(h + 1) * 64], Gm[b][:], start=(kt == 0), stop=(kt == nk - 1),
                                 reads=['ssxdt', 'ssG%d' % b], writes=[pck])
                        k.op('dve', 'tensor_copy', YO[u][:], pc[0:64, 0:TB], reads=[pck, 'ssY%d' % u], writes=['ssY%d' % u])
                        k.dma('sp', ys_d[h, :, qsl], YO[u][:], reads=['ssY%d' % u], writes=['ys_d'])
                k.barrier()
        with ExitStack() as st:
            sb = lambda n, s, d=F32: self.sb(st, n, s, d)
            dcol = sb('ssdcol', [128, 4])
            for hp in range(2):
                k.dma('sp', dcol[64 * hp:64 * hp + 64, :].rearrange("p (o n) -> p o n", o=1),
                      I['m2_d'].rearrange("o (ft hp) -> hp o ft", hp=2)[hp].partition_broadcast(64), writes=['ssdcol'], allow_slow_non_contiguous=True)
            nw = sb('ssnw', [128, 4])
            k.dma('sp', nw[:], I['m2_norm_w'].rearrange("o (mt p) -> p (o mt)", p=128), writes=['ssnw'], allow_slow_non_contiguous=True)
            Y = [sb('spy%d' % i, [128, TB]) for i in range(4)]
            X = [sb('spx%d' % i, [128, TB]) for i in range(2)]
            Z = [sb('spz%d' % i, [128, TB]) for i in range(2)]
            Q = [sb('spq%d' % i, [128, TB]) for i in range(2)]
            O = [sb('spo%d' % i, [128, TB], BF16) for i in range(2)]
            ysv = ys_d.rearrange("(ft hp) d t -> (hp d) ft t", hp=2)
            it = 0
            for tb in range(NB):
                k.checkpoint()
                tsl = slice(tb * TB, (tb + 1) * TB)
                for g in range(2):
                    p, pk = self.ps[(tb * 2 + g) % 2], 'ps%d' % ((tb * 2 + g) % 2)
                    for f2 in range(2):
                        ft = 2 * g + f2
                        it += 1
                        b = it % 2
                        yk, xk, zk, qk = 'spy%d' % ft, 'spx%d' % b, 'spz%d' % b, 'spq%d' % b
                        k.dma('sp', Y[ft][:], ysv[:, ft, tsl], reads=['ys_d'], writes=[yk])
                        k.dma('act', X[b][:], xc_d[ft, :, tsl], reads=['xc_d'], writes=[xk])
                        k.dma('sp', Z[b][:], pT[14 + ft, :, tsl], reads=['projT'], writes=[zk])
                        k.op('dve', 'scalar_tensor_tensor', Y[ft][:], X[b][:], dcol[:, ft:ft + 1], Y[ft][:], ALU.mult, ALU.add, reads=[xk, 'ssdcol', yk], writes=[yk])
                        k.op('act', 'activation', Z[b][:], Z[b][:], AF.Silu, reads=[zk], writes=[zk])
                        k.op('dve', 'tensor_tensor', Y[ft][:], Y[ft][:], Z[b][:], ALU.mult, reads=[yk, zk], writes=[yk])
                        k.op('pool', 'tensor_tensor', Q[b][:], Y[ft][:], Y[ft][:], ALU.mult, reads=[yk, qk], writes=[qk])
                        k.op('pe', 'matmul', p[:, 0:TB], self.ones[:], Q[b][:], start=(f2 == 0), stop=(f2 == 1), reads=['ones', qk], writes=[pk])
                    k.op('dve', 'tensor_scalar', Q[0][:], p[:, 0:TB], 1.0 / 256, 1e-6, ALU.mult, ALU.add, reads=[pk, 'spq0'], writes=['spq0'])
                    k.op('act', 'activation', Q[0][:], Q[0][:], AF.Sqrt, reads=['spq0'], writes=['spq0'])
                    k.op('dve', 'reciprocal', Q[0][:], Q[0][:], reads=['spq0'], writes=['spq0'])
                    for f2 in range(2):
                        ft = 2 * g + f2
                        ok = 'spo%d' % f2
                        k.op('dve', 'scalar_tensor_tensor', O[f2][:], Y[ft][:], nw[:, ft:ft + 1], Q[0][:], ALU.mult, ALU.mult,
                             reads=['spy%d' % ft, 'ssnw', 'spq0', ok], writes=[ok])
                        k.dma('act', yms_d[ft, :, tsl], O[f2][:], reads=[ok], writes=['yms_d'])
            k.barrier()

    def moe(self, li, xin, xout, final=False):
        k, nc, T, NT, TB = self.k, self.nc, self.T, self.NT, self.TB
        li = 0 if self.lsel is not None else li
        I = self.I
        HT = min(1024, T)
        NH = T // HT
        HTT = HT // 128
        HNB = HT // TB
        TPB = self.TPB
        gA, gB, g2 = self.modbc[:, 4 * D:5 * D], self.modbc[:, 3 * D:4 * D], self.modbc[:, 5 * D:6 * D]
        with ExitStack() as st:
            sb = lambda n, s, d=F32: self.sb(st, n, s, d)
            wr = sb('wr', [128, 8, 36])
            k.dma('sp', wr[:, :, 0:4], I['moe_w_grp'][li].rearrange("(kc k) n -> k kc n", k=128), writes=['wr'], allow_slow_non_contiguous=True)
            k.dma('sp', wr[:, :, 4:36], I['moe_w_exp'][li].rearrange("(kc k) n -> k kc n", k=128), reads=['wr'], writes=['wr'], allow_slow_non_contiguous=True)
            br = sb('br', [1, 36])
            k.dma('sp', br[:, 0:4], I['moe_b_grp'][li:li + 1, :], writes=['br'])
            k.dma('sp', br[:, 4:36], I['moe_b_exp'][li:li + 1, :], reads=['br'], writes=['br'])
            hT = sb('mhT', [128, 8, HT], BF16)
            GT = sb('mGT', [32, HT])
            acc = sb('macc', [128, HTT, D])
            bufs = self.norm_bufs(st)
            hT32 = sb('hT32', [128, 8, 128])
            R = {n: sb('rt_' + n, [128, 32]) for n in ('lg', 'e', 'oh', 'x', 'oh1', 'oh2', 'G')}
            S = {n: sb('rs_' + n, [128, 4]) for n in ('m', 's', 'm1', 'm2', 'g1', 'g2')}
            Wg = [sb('mWg%d' % i, [128, 8, 512], BF16) for i in range(2)]
            Wu = [sb('mWu%d' % i, [128, 8, 512], BF16) for i in range(2)]
            Wd = [sb('mWd%d' % i, [128, 4, 1024], BF16) for i in range(2)]
            gb = [sb('mgb%d' % i, [128, TB]) for i in range(2)]
            sl = [sb('msl%d' % i, [128, TB]) for i in range(2)]
            a1 = [sb('ma1%d' % i, [128, TB]) for i in range(2)]
            aT = [sb('maT%d' % i, [128, 4, TB], BF16) for i in range(2)]
            xo = [sb('mxo%d' % i, [128, D]) for i in range(2)]
            fw = None
            if final:
                fw = sb('finw', [128, D])
                k.dma('sp', fw[:], I['final_norm_w'].partition_broadcast(128).rearrange("p o n -> p (o n)"), writes=['finw'])
            RK = ['route']
            for half in range(NH):
                t0 = half * HTT
                for j in range(HTT):
                    k.checkpoint()
                    tile = t0 + j
                    if DBG['norm'] >= 0:
                        self.norm_tile(bufs, xin, tile, gA, gB, hT[:, :, j * 128:(j + 1) * 128], hT32_dst=hT32, keys=['mhT'])
                    lvl = getattr(self, 'moe_level', 3)
                    if lvl <= -4:
                        continue
                    p, pk = self.ps[4], 'ps4'
                    for kc in range(8):
                        k.op('pe', 'matmul', p[:, 0:36], hT32[:, kc, :], wr[:, kc, :], start=(kc == 0), stop=False, reads=['hT32', 'wr'], writes=[pk])
                    k.op('pe', 'matmul', p[:, 0:36], self.ones[0:1, :], br[0:1, :], start=False, stop=True, reads=['ones', 'br'], writes=[pk])

                    def o(eng, f, *a, **kw):
                        k.op(eng, f, *a, reads=RK, writes=RK, **kw)
                    k.op('dve', 'tensor_copy', R['lg'][:, 0:4], p[:, 0:4], reads=[pk] + RK, writes=RK)
                    k.op('dve', 'tensor_copy', R['x'][:], p[:, 4:36], reads=[pk] + RK, writes=RK)
                    if lvl <= -3:
                        continue
                    lg = R['lg'][:, 0:4]
                    o('dve', 'tensor_reduce', S['m'][:, 0:1], lg, AX.X, ALU.max)
                    o('dve', 'tensor_scalar', R['oh'][:, 0:4], lg, S['m'][:, 0:1], None, ALU.is_equal)
                    o('dve', 'tensor_scalar', R['e'][:, 0:4], lg, S['m'][:, 0:1], None, ALU.subtract)
                    o('act', 'activation', R['e'][:, 0:4], R['e'][:, 0:4], AF.Exp)
                    o('dve', 'tensor_reduce', S['s'][:, 0:1], R['e'][:, 0:4], AX.X, ALU.add)
                    o('dve', 'reciprocal', S['s'][:, 0:1], S['s'][:, 0:1])
                    o('dve', 'tensor_scalar', R['oh'][:, 0:4], R['oh'][:, 0:4], -1.0, 1e30, ALU.add, ALU.mult)
                    o('dve', 'tensor_tensor', R['x'][:].rearrange("p (g e) -> p g e", g=4), R['x'][:].rearrange("p (g e) -> p g e", g=4),
                      R['oh'][:, 0:4].unsqueeze(2).to_broadcast([128, 4, 8]), ALU.add)
                    o('dve', 'tensor_reduce', S['m1'][:, 0:1], R['x'][:], AX.X, ALU.max)
                    o('dve', 'tensor_scalar', R['oh1'][:], R['x'][:], S['m1'][:, 0:1], None, ALU.is_equal)
                    o('dve', 'scalar_tensor_tensor', R['e'][:], R['oh1'][:], -1e30, R['x'][:], ALU.mult, ALU.add)
                    o('dve', 'tensor_reduce', S['m2'][:, 0:1], R['e'][:], AX.X, ALU.max)
                    o('dve', 'tensor_scalar', R['oh2'][:], R['e'][:], S['m2'][:, 0:1], None, ALU.is_equal)
                    o('dve', 'tensor_tensor', S['g1'][:, 0:1], S['m2'][:, 0:1], S['m1'][:, 0:1], ALU.subtract)
                    o('act', 'activation', S['g1'][:, 0:1], S['g1'][:, 0:1], AF.Exp)
                    o('dve', 'tensor_scalar', S['g1'][:, 0:1], S['g1'][:, 0:1], 1.0, None, ALU.add)
                    o('dve', 'reciprocal', S['g1'][:, 0:1], S['g1'][:, 0:1])
                    o('dve', 'tensor_scalar', S['g2'][:, 0:1], S['g1'][:, 0:1], -1.0, 1.0, ALU.mult, ALU.add)
                    o('dve', 'tensor_tensor', S['g1'][:, 0:1], S['g1'][:, 0:1], S['s'][:, 0:1], ALU.mult)
                    o('dve', 'tensor_tensor', S['g2'][:, 0:1], S['g2'][:, 0:1], S['s'][:, 0:1], ALU.mult)
                    o('dve', 'tensor_scalar', R['G'][:], R['oh1'][:], S['g1'][:, 0:1], None, ALU.mult)
                    o('dve', 'scalar_tensor_tensor', R['G'][:], R['oh2'][:], S['g2'][:, 0:1], R['G'][:], ALU.mult, ALU.add)
                    if lvl <= -2:
                        continue
                    p5, p5k = self.ps[5], 'ps5'
                    k.op('pe', 'transpose', p5[0:32, 0:128], R['G'][:], self.ident[:], reads=RK + ['ident'], writes=[p5k])
                    k.op('act', 'activation', GT[:, j * 128:(j + 1) * 128], p5[0:32, 0:128], AF.Copy, reads=[p5k], writes=['mGT'])
                if 'gates' in self.dbg:
                    k.dma('sp', self.dbg_out['gates'][:, half * HT:(half + 1) * HT], GT[:], reads=['mGT'], writes=['dbgg'])
                lvl = getattr(self, 'moe_level', 3)
                if lvl < 3 or self.nexp == 0:
                    k.op('dve', 'memset', acc[:], 0.0, writes=['macc'])
                for e in range(self.nexp):
                    k.checkpoint()
                    b = e % 2
                    if lvl < 1:
                        continue
                    wk = 'mW%d' % b
                    k.dma('pool', Wg[b][:], I['moe_w_gate'][li, e].rearrange("(kc k) n -> k kc n", k=128), writes=[wk + 'g'])
                    k.dma('pool', Wu[b][:], I['moe_w_up'][li, e].rearrange("(kc k) n -> k kc n", k=128), writes=[wk + 'u'])
                    k.dma('pool', Wd[b][:], I['moe_w_down'][li, e].rearrange("(kc k) n -> k kc n", k=128), writes=[wk + 'd'])
                    for tb in range(HNB):
                        if lvl < 2:
                            continue
                        self.uid += 1
                        u = self.uid % 2
                        tsl = slice(tb * TB, (tb + 1) * TB)
                        pg_, pgk = self.ps[6], 'ps6'
                        k.op('pe', 'matmul', pg_[:, 0:TB], self.ident[0:32, e:e + 1].to_broadcast([32, 128]), GT[:, tsl], start=True, stop=True,
                             reads=['ident', 'mGT'], writes=[pgk])
                        k.op('act', 'activation', gb[u][:], pg_[:, 0:TB], AF.Copy, reads=[pgk], writes=['mgb%d' % u])
                        for hm in range(4):
                            v = hm % 2
                            p1, p1k, p2, p2k = self.ps[v], 'ps%d' % v, self.ps[2 + v], 'ps%d' % (2 + v)
                            for kc in range(8):
                                k.op('pe', 'matmul', p1[:, 0:TB], Wg[b][:, kc, hm * 128:(hm + 1) * 128], hT[:, kc, tsl],
                                     start=(kc == 0), stop=(kc == 7), reads=[wk + 'g', 'mhT'], writes=[p1k])
                            for kc in range(8):
                                k.op('pe', 'matmul', p2[:, 0:TB], Wu[b][:, kc, hm * 128:(hm + 1) * 128], hT[:, kc, tsl],
                                     start=(kc == 0), stop=(kc == 7), reads=[wk + 'u', 'mhT'], writes=[p2k])
                            k.op('act', 'activation', sl[v][:], p1[:, 0:TB], AF.Silu, reads=[p1k], writes=['msl%d' % v])
                            k.op('dve', 'tensor_tensor', a1[v][:], p2[:, 0:TB], sl[v][:], ALU.mult, reads=[p2k, 'msl%d' % v], writes=['ma1%d' % v])
                            k.op('pool', 'tensor_tensor', aT[u][:, hm, :], a1[v][:], gb[u][:], ALU.mult, reads=['ma1%d' % v, 'mgb%d' % u], writes=['maT%d' % u])
                        for j in range(TPB):
                            if lvl < 3:
                                continue
                            tl = tb * TPB + j
                            for nh in range(2):
                                self.uid += 1
                                pi = 4 + self.uid % 2
                                p, pk = self.ps[pi], 'ps%d' % pi
                                for hm in range(4):
                                    k.op('pe', 'matmul', p[:], aT[u][:, hm, j * 128:(j + 1) * 128], Wd[b][:, hm, nh * 512:(nh + 1) * 512],
                                         start=(hm == 0), stop=(hm == 3), reads=['maT%d' % u, wk + 'd'], writes=[pk])
                                dst = acc[:, tl, nh * 512:(nh + 1) * 512]
                                if e == 0:
                                    k.op('dve', 'tensor_copy', dst, p[:], reads=[pk], writes=['macc'])
                                else:
                                    k.op('dve', 'tensor_tensor', dst, dst, p[:], ALU.add, reads=[pk, 'macc'], writes=['macc'])
                for j in range(HTT):
                    k.checkpoint()
                    tile = t0 + j
                    b = j % 2
                    xk = 'mxo%d' % b
                    k.dma('sp', xo[b][:], xin[tile * 128:(tile + 1) * 128, :], reads=['xres'], writes=[xk])
                    k.op('dve', 'tensor_tensor', acc[:, j, :], acc[:, j, :], g2, ALU.mult, reads=['macc', 'modbc'], writes=['macc'])
                    k.op('pool', 'tensor_tensor', xo[b][:], xo[b][:], acc[:, j, :], ALU.add, reads=['macc', xk], writes=[xk])
                    if final:
                        sq, ss = bufs['sq'], bufs['ss']
                        c = ss[:, 0:1]
                        k.op('dve', 'tensor_tensor', sq[:], xo[b][:], xo[b][:], ALU.mult, reads=[xk, 'sq'], writes=['sq'])
                        k.op('dve', 'tensor_reduce', c, sq[:], AX.X, ALU.add, reads=['sq', 'ss'], writes=['ss'])
                        k.op('dve', 'tensor_scalar', c, c, 1.0 / D, 1e-6, ALU.mult, ALU.add, reads=['ss'], writes=['ss'])
                        k.op('act', 'activation', c, c, AF.Sqrt, reads=['ss'], writes=['ss'])
                        k.op('dve', 'reciprocal', c, c, reads=['ss'], writes=['ss'])
                        k.op('dve', 'scalar_tensor_tensor', xo[b][:], xo[b][:], c, fw[:], ALU.mult, ALU.mult, reads=[xk, 'ss', 'finw'], writes=[xk])
                    k.dma('sp', xout[tile * 128:(tile + 1) * 128, :], xo[b][:], reads=[xk], writes=['xres2'])
            k.barrier()


def build(T, dbg=(), nlayers=2, nexp=32, stages=('mix0', 'moe0', 'mix1', 'moe1'), moe_level=3, lsel=None):
    m = Mod(T, dbg, nlayers, nexp, lsel)
    m.moe_level = moe_level
    k = m.k
    x1 = m.scr('x1', [T, D])
    x2 = m.scr('x2', [T, D])
    x3 = m.scr('x3', [T, D])
    for n, shp in (('x1', [T, D]), ('x2', [T, D]), ('gates', [32, T]), ('x3', [T, D])):
        m.tap(n, shp)
    with ExitStack() as st:
        m.consts(st)
        k.barrier()
        last = stages[-1]
        if 'consts' in stages:
            k.dma('sp', m.out[0:128, 0:128], m.negtri[:], reads=['negtri'], writes=['o'])
            k.dma('sp', m.out[0:128, 128:256], m.ident[:], reads=['ident'], writes=['o'])
            k.dma('sp', m.out[0:128, 256:384], m.blk[:], reads=['blk'], writes=['o'])
            k.finish()
            return m
        m.adaln(0)
        for _ in range(DBG.get('rep', 0)):
            m.adaln(0)
        if 'adaln' in stages:
            for i in range(min(6, T // 128)):
                k.dma('sp', m.out[i * 128:(i + 1) * 128, :], m.modbc[:, i * D:(i + 1) * D], reads=['modbc'], writes=['o'])
            k.finish()
            return m
        if 'mix0' in stages:
            m.even_mixer(m.I['x'], m.dbg_out.get('x1', m.out if last == 'mix0' else x1))
        if 'moe0' in stages:
            src = x1 if 'mix0' in stages and 'x1' not in m.dbg_out else (m.dbg_out.get('x1') if 'mix0' in stages else m.I['x'])
            m.moe(0, src, m.out if last == 'moe0' else x2, final=(last == 'moe0' and nlayers == 1))
        if 'mix1' in stages or 'moe1' in stages:
            m.adaln(1)
        if 'mix1' in stages:
            src = x2 if 'moe0' in stages else m.I['x']
            m.odd_mixer(src, m.out if last == 'mix1' else x3)
        if 'moe1' in stages:
            src = x3 if 'mix1' in stages else m.I['x']
            m.moe(1, src, m.out, final=True)
        k.finish()
    return m


PER_LAYER = ('ada_w', 'ada_b', 'norm_mix_w', 'norm_ffn_w', 'moe_w_grp', 'moe_b_grp', 'moe_w_exp', 'moe_b_exp',
             'moe_w_gate', 'moe_w_up', 'moe_w_down')


def _in_maps(inputs, xs, layer):
    in_maps = []
    for b in range(8):
        d = {}
        for name in INPUT_SHAPES:
            if name == 'x':
                a = xs[b]
            else:
                a = np.asarray(inputs[name], dtype=np.float32)
                if name == 'c':
                    a = a[b:b + 1]
                elif name == 'final_norm_w':
                    a = a.reshape(1, D)
                elif name in PER_LAYER:
                    a = a[layer:layer + 1]
            d[name] = np.ascontiguousarray(a, dtype=np.float32)
        in_maps.append(d)
    return in_maps


def kernel(**inputs):
    T = 4096
    xs = np.asarray(inputs['x'], dtype=np.float32)
    m = build(T)
    in_maps = []
    for b in range(8):
        d = {}
        for name in INPUT_SHAPES:
            if name == 'x':
                a = xs[b]
            else:
                a = np.asarray(inputs[name], dtype=np.float32)
                if name == 'c':
                    a = a[b:b + 1]
                elif name == 'final_norm_w':
                    a = a.reshape(1, D)
            d[name] = np.ascontiguousarray(a, dtype=np.float32)
        in_maps.append(d)
    res = run_bass_kernel_spmd(m.nc, in_maps, core_ids=list(range(8)))
    return np.stack([np.asarray(r['out'], dtype=np.float32) for r in res.results])
```

```python
import math
from contextlib import ExitStack
import numpy as np
import concourse.bass as bass
import concourse.mybir as mybir
from concourse.bass_utils import run_bass_kernel_spmd

F32 = mybir.dt.float32
BF16 = mybir.dt.bfloat16
I32 = mybir.dt.int32
ALU = mybir.AluOpType
AF = mybir.ActivationFunctionType
AX = mybir.AxisListType
PI = math.pi
D = 1024
DBG = {'norm': 9}


class KB:
    NDMA = 10
    THRESH = 10 ** 9

    def __init__(self, nc):
        self.nc = nc
        self.E = {'pe': nc.tensor, 'act': nc.scalar, 'dve': nc.vector, 'pool': nc.gpsimd, 'sp': nc.sync}
        names = ['pe', 'act', 'dve', 'pool']
        self.dq = {}
        for q in ('sp', 'act', 'pool'):
            dn = ['d_%s%d' % (q, j) for j in range(self.NDMA)]
            names += dn
            self.dq[q] = [dn, 0]
        self.banks = [{n: nc.alloc_semaphore('%s_b%d' % (n, b)) for n in names} for b in range(2)]
        self.bank = 0
        self.semh = self.banks[0]
        self.cnt = {n: 0 for n in names}
        self.seen = {e: {} for e in self.E}
        self.lastw = {}
        self.readers = {}
        self.dummy = None
        self.nswitch = 0

    def _wait(self, eng, deps):
        for s, v in deps.items():
            if eng == 'pe' and s == 'pe':
                continue
            if self.seen[eng].get(s, 0) < v:
                self.E[eng].wait_ge(self.semh[s], v)
                self.seen[eng][s] = v

    def _deps(self, reads, writes):
        deps = {}
        for r in reads:
            for s, v in self.lastw.get(r, {}).items():
                deps[s] = max(deps.get(s, 0), v)
        for w in writes:
            for s, v in self.lastw.get(w, {}).items():
                deps[s] = max(deps.get(s, 0), v)
            for s, v in self.readers.get(w, {}).items():
                deps[s] = max(deps.get(s, 0), v)
        return deps

    def _record(self, ev, reads, writes):
        s, v = ev
        for r in reads:
            d = self.readers.setdefault(r, {})
            d[s] = max(d.get(s, 0), v)
        for w in writes:
            self.lastw[w] = {s: v}
            self.readers[w] = {}

    def op(self, eng, fname, *args, reads=(), writes=(), **kw):
        self._wait(eng, self._deps(reads, writes))
        inst = getattr(self.E[eng], fname)(*args, **kw)
        self.cnt[eng] += 1
        inst.then_inc(self.semh[eng], 1)
        self._record((eng, self.cnt[eng]), reads, writes)
        return inst

    def dma(self, q, out, in_, reads=(), writes=(), **kw):
        names, idx = self.dq[q]
        n = names[idx % len(names)]
        self.dq[q][1] = idx + 1
        deps = self._deps(reads, writes)
        if self.cnt[n] > 0:
            deps[n] = max(deps.get(n, 0), self.cnt[n])
        self._wait(q, deps)
        inst = self.E[q].dma_start(out=out, in_=in_, **kw)
        self.cnt[n] += 16
        inst.then_inc(self.semh[n], 16)
        self._record((n, self.cnt[n]), reads, writes)
        return inst

    def barrier(self, sw=True):
        allv = {s: v for s, v in self.cnt.items() if v > 0}
        for e in self.E:
            self._wait(e, dict(allv))
        self.lastw = {}
        self.readers = {}
        if sw and max(self.cnt.values()) > self.THRESH:
            self._switch()

    def checkpoint(self):
        if max(self.cnt.values()) > self.THRESH:
            self.barrier()

    def _switch(self):
        other = 1 - self.bank
        pool = self.E['pool']
        for h in self.banks[other].values():
            pool.sem_clear(h)
        inst = pool.memset(self.dummy[:], 0.0)
        self.cnt['pool'] += 1
        inst.then_inc(self.semh['pool'], 1)
        v = self.cnt['pool']
        for e in ('pe', 'act', 'dve', 'sp'):
            self.E[e].wait_ge(self.semh['pool'], v)
        self.bank = other
        self.semh = self.banks[other]
        for n in self.cnt:
            self.cnt[n] = 0
        self.seen = {e: {} for e in self.E}
        self.lastw = {}
        self.readers = {}
        self.nswitch += 1

    def finish(self):
        allv = {s: v for s, v in self.cnt.items() if v > 0}
        self._wait('sp', allv)


INPUT_SHAPES = {
    'x': None, 'c': [1, D], 'ada_w': [2, D, 6 * D], 'ada_b': [2, 6 * D], 'norm_mix_w': [2, D], 'norm_ffn_w': [2, D],
    'even_w_in': [1, D, 2048], 'even_w_out': [1, D, D], 's5_a_re': [1, 32, 64], 's5_a_im': [1, 32, 64],
    's5_log_dt': [1, 32], 's5_b_re': [1, 32, 64, 16], 's5_b_im': [1, 32, 64, 16], 's5_c_re': [1, 32, 16, 64],
    's5_c_im': [1, 32, 16, 64], 's5_d': [1, 32, 16], 's5_w_glu': [1, 512, 1024], 'odd_w_in': [1, D, 3336],
    'odd_w_out': [1, D, D], 'rw_mu': [1, 1792], 'rw_w0': [1, 512], 'rw_w2': [1, 64, 512], 'rw_a0': [1, 512],
    'rw_a2': [1, 64, 512], 'rw_g2': [1, 128, 512], 'rw_k_k': [1, 512], 'rw_k_a': [1, 512], 'rw_r_k': [1, 8, 64],
    'rw_ln_w': [1, 512], 'rw_ln_b': [1, 512], 'm2_conv_w': [1, 4, 1024], 'm2_conv_b': [1, 1024], 'm2_dt_bias': [1, 8],
    'm2_a_log': [1, 8], 'm2_d': [1, 8], 'm2_norm_w': [1, 512], 'moe_w_grp': [2, D, 4], 'moe_b_grp': [2, 4],
    'moe_w_exp': [2, D, 32], 'moe_b_exp': [2, 32], 'moe_w_gate': [2, 32, D, 512], 'moe_w_up': [2, 32, D, 512],
    'moe_w_down': [2, 32, 512, D], 'final_norm_w': [1, D],
}


class Mod:
    def __init__(self, T, dbg=(), nlayers=2, nexp=32, lsel=None):
        self.T = T
        self.lsel = lsel
        self.NT = T // 128
        self.TB = min(512, T)
        self.NB = T // self.TB
        self.TPB = self.TB // 128
        self.dbg = set(dbg)
        self.nlayers = nlayers
        self.nexp = nexp
        nc = self.nc = bass.Bass("TRN2", target_bir_lowering=False)
        self.k = KB(nc)
        self.I = {}
        for name, shp in INPUT_SHAPES.items():
            shp = [T, D] if name == 'x' else shp
            if nexp == 0 and name in ('moe_w_gate', 'moe_w_up', 'moe_w_down'):
                shp = [2, 1] + list(shp[2:])
            if lsel is not None and shp[0] == 2:
                shp = [1] + list(shp[1:])
            self.I[name] = nc.dram_tensor(name, list(shp), F32, kind="ExternalInput").ap()
        self.out = nc.dram_tensor('out', [T, D], F32, kind="ExternalOutput").ap()
        self.dbg_out = {}
        self.ps = [nc.alloc_psum_tensor('ps%d' % i, [128, 512], F32) for i in range(8)]
        self.uid = 0

    def scr(self, name, shape, dt=F32):
        return self.nc.dram_tensor(name, list(shape), dt, kind="Internal").ap()

    def tap(self, name, shape):
        if name in self.dbg:
            t = self.nc.dram_tensor('dbg_' + name, list(shape), F32, kind="ExternalOutput").ap()
            self.dbg_out[name] = t
            return t
        return None

    def sb(self, st, name, shape, dt=F32):
        self.nid = getattr(self, 'nid', 0) + 1
        return st.enter_context(self.nc.sbuf_tensor('%s_%d' % (name, self.nid), list(shape), dt))

    def consts(self, st):
        k = self.k
        self.ident = self.sb(st, 'ident', [128, 128])
        k.op('pool', 'memset', self.ident[:], 1.0, writes=['ident'])
        k.op('pool', 'affine_select', self.ident[:], self.ident[:], pattern=[[1, 128]], compare_op=ALU.is_equal,
             fill=0.0, base=0, channel_multiplier=-1, reads=['ident'], writes=['ident'])
        self.ones = self.sb(st, 'ones', [128, 128])
        k.op('pool', 'memset', self.ones[:], 1.0, writes=['ones'])
        self.negones = self.sb(st, 'negones', [128, 128])
        k.op('pool', 'memset', self.negones[:], -1.0, writes=['negones'])
        self.negtri = self.sb(st, 'negtri', [128, 128])
        k.op('pool', 'memset', self.negtri[:], -1.0, writes=['negtri'])
        k.op('pool', 'affine_select', self.negtri[:], self.negtri[:], pattern=[[-1, 128]], compare_op=ALU.is_ge,
             fill=0.0, base=0, channel_multiplier=1, reads=['negtri'], writes=['negtri'])
        self.blk = self.sb(st, 'blk', [128, 128])
        k.op('pool', 'memset', self.blk[:], 0.0, writes=['blk'])
        k.op('pool', 'memset', self.blk[0:64, 0:64], 1.0, reads=['blk'], writes=['blk'])
        k.op('pool', 'memset', self.blk[64:128, 64:128], 1.0, reads=['blk'], writes=['blk'])
        self.cst = self.sb(st, 'cst', [128, 4])
        k.dummy = self.sb(st, 'kdummy', [128, 4])
        k.op('pool', 'memset', self.cst[:, 0:1], -PI, writes=['cst'])
        k.op('pool', 'memset', self.cst[:, 1:2], 1.0, reads=['cst'], writes=['cst'])
        k.op('pool', 'memset', self.cst[:, 2:3], 0.0, reads=['cst'], writes=['cst'])
        self.modbc = self.sb(st, 'modbc', [128, 6 * D])

    def adaln(self, li):
        k, nc = self.k, self.nc
        li = 0 if self.lsel is not None else li
        with ExitStack() as st:
            ccol = self.sb(st, 'ccol', [128, 8])
            condbc = self.sb(st, 'condbc', [128, 8, 128])
            adab = self.sb(st, 'adab', [1, 6 * D])
            wch = [self.sb(st, 'adaw%d' % i, [128, 8, 512]) for i in range(2)]
            self.normw = self.sb(st, 'normw', [128, 2 * D])
            k.dma('sp', ccol[:], self.I['c'].rearrange("o (kc k) -> k (o kc)", k=128), writes=['ccol'],
                  allow_slow_non_contiguous=True)
            k.op('act', 'activation', ccol[:], ccol[:], AF.Silu, reads=['ccol'], writes=['ccol'])
            for kc in range(8):
                k.op('dve', 'tensor_scalar', condbc[:, kc, :], self.ones[:], ccol[:, kc:kc + 1], None, ALU.mult,
                     reads=['ccol', 'ones'], writes=['condbc'])
            k.dma('sp', adab[:], self.I['ada_b'][li:li + 1, :], writes=['adab'])
            wv = self.I['ada_w'][li].rearrange("(kc k) n -> k kc n", k=128)
            for n in range(12):
                k.checkpoint()
                b = n % 2
                k.dma('pool' if DBG.get('pooldma') else ('sp' if b == 0 else 'act'), wch[b][:], wv[:, :, n * 512:(n + 1) * 512], writes=['adaw%d' % b])
                p = self.ps[n % 2]
                pk = 'ps%d' % (n % 2)
                for kc in range(8):
                    k.op('pe', 'matmul', p[:], condbc[:, kc, :], wch[b][:, kc, :], start=(kc == 0), stop=False,
                         reads=['condbc', 'adaw%d' % b], writes=[pk])
                k.op('pe', 'matmul', p[:], self.ones[0:1, :], adab[0:1, n * 512:(n + 1) * 512], start=False, stop=True,
                     reads=['ones', 'adab'], writes=[pk])
                k.op('act', 'activation', self.modbc[:, n * 512:(n + 1) * 512], p[:], AF.Copy, reads=[pk], writes=['modbc'])
            k.dma('sp', self.normw[:, 0:D], self.I['norm_mix_w'][li:li + 1, :].partition_broadcast(128).rearrange("p o n -> p (o n)"),
                  writes=['normw'])
            k.dma('sp', self.normw[:, D:2 * D], self.I['norm_ffn_w'][li:li + 1, :].partition_broadcast(128).rearrange("p o n -> p (o n)"),
                  reads=['normw'], writes=['normw'])
            for slot, wo in ((1, 0), (4, D)):
                k.op('dve', 'scalar_tensor_tensor', self.modbc[:, slot * D:(slot + 1) * D], self.modbc[:, slot * D:(slot + 1) * D],
                     1.0, self.normw[:, wo:wo + D], ALU.add, ALU.mult, reads=['modbc', 'normw'], writes=['modbc'])
            k.barrier()

    def norm_tile(self, bufs, xsrc, tile, gA, gB, hT_dst, hT32_dst=None, keys=()):
        k = self.k
        i = self.uid
        self.uid += 1
        b = i % 2
        xt, h32, sq, ss = bufs['xt'][b], bufs['h32'][b], bufs['sq'], bufs['ss']
        kx, kh = 'xt%d' % b, 'h32%d' % b
        k.dma('sp', xt[:], xsrc[tile * 128:(tile + 1) * 128, :], reads=['xres'], writes=[kx])
        k.op('dve', 'tensor_tensor', sq[:], xt[:], xt[:], ALU.mult, reads=[kx], writes=['sq'])
        c = ss[:, (i % 8):(i % 8) + 1]
        k.op('dve', 'tensor_reduce', c, sq[:], AX.X, ALU.add, reads=['sq'], writes=['ss'])
        k.op('dve', 'tensor_scalar', c, c, 1.0 / D, 1e-6, ALU.mult, ALU.add, reads=['ss'], writes=['ss'])
        k.op('act', 'activation', c, c, AF.Sqrt, reads=['ss'], writes=['ss'])
        k.op('dve', 'reciprocal', c, c, reads=['ss'], writes=['ss'])
        k.op('dve', 'scalar_tensor_tensor', h32[:], xt[:], c, gA, ALU.mult, ALU.mult, reads=[kx, 'ss', 'modbc'], writes=[kh])
        if DBG['norm'] < 1:
            return
        k.op('pool', 'tensor_tensor', h32[:], h32[:], gB, ALU.add, reads=[kh, 'modbc'], writes=[kh])
        if DBG['norm'] < 2:
            return
        pa = (i % 2) * 2
        for half in range(2):
            p = self.ps[pa + half]
            pk = 'ps%d' % (pa + half)
            for j in range(4):
                kc = half * 4 + j
                k.op('pe', 'transpose', p[:, j * 128:(j + 1) * 128], h32[:, kc * 128:(kc + 1) * 128], self.ident[:],
                     reads=[kh, 'ident'], writes=[pk])
            pv = p[:].rearrange("p (a b) -> p a b", a=4)
            if hT32_dst is None:
                k.op('act', 'activation', hT_dst[:, half * 4:half * 4 + 4, :], pv, AF.Copy, reads=[pk], writes=list(keys))
            else:
                k.op('dve', 'tensor_copy', hT32_dst[:, half * 4:half * 4 + 4, :], pv, reads=[pk], writes=['hT32'])
                k.op('act', 'activation', hT_dst[:, half * 4:half * 4 + 4, :], hT32_dst[:, half * 4:half * 4 + 4, :], AF.Copy,
                     reads=['hT32'], writes=list(keys))

    def norm_bufs(self, st):
        return {'xt': [self.sb(st, 'xt%d' % i, [128, D]) for i in range(2)],
                'h32': [self.sb(st, 'h32%d' % i, [128, D]) for i in range(2)],
                'sq': self.sb(st, 'sq', [128, D]), 'ss': self.sb(st, 'ss', [128, 8])}

    def even_mixer(self, xin, xout):
        k, nc, T, NT, TB, NB, TPB = self.k, self.nc, self.T, self.NT, self.TB, self.NB, self.TPB
        I = self.I
        yb_d = self.scr('yb_d', [8, 64, T], BF16)
        uT_d = self.scr('uT_d', [4, 128, T], BF16)
        ya_d = self.scr('ya_d', [4, 128, T], BF16)
        with ExitStack() as st0:
            with ExitStack() as st1:
                qT = self.sb(st1, 'qT', [128, 4, T], BF16)
                kT = self.sb(st1, 'kT', [128, 4, T], BF16)
                vtok = self.sb(st1, 'vtok', [128, NT, 512], BF16)
                with ExitStack() as st:
                    win = self.sb(st, 'win', [128, 8, 2048], BF16)
                    wv = I['even_w_in'][0].rearrange("(kc k) n -> k kc n", k=128)
                    for kc in range(8):
                        k.dma('pool', win[:, kc, :], wv[:, kc, :], writes=['win'])
                    bufs = self.norm_bufs(st)
                    hT = [self.sb(st, 'hT%d' % i, [128, 8, TB], BF16) for i in range(2)]
                    ustg = [self.sb(st, 'ustg%d' % i, [128, TB], BF16) for i in range(2)]
                    for tb in range(NB):
                        k.checkpoint()
                        hb = hT[tb % 2]
                        hk = 'hT%d' % (tb % 2)
                        for j in range(TPB):
                            self.norm_tile(bufs, xin, tb * TPB + j, self.modbc[:, D:2 * D], self.modbc[:, 0:D],
                                           hb[:, :, j * 128:(j + 1) * 128], keys=[hk])
                        tsl = slice(tb * TB, (tb + 1) * TB)
                        cnt = 0
                        for dst, c0, scale, dk in ((None, 0, 1.0, 'uT'), (qT, 512, 0.125, 'qT'), (kT, 1024, 1.0, 'kT')):
                            for mt in range(4):
                                pi = 4 + cnt % 4
                                cnt += 1
                                p, pk = self.ps[pi], 'ps%d' % pi
                                for kc in range(8):
                                    k.op('pe', 'matmul', p[:, 0:TB], win[:, kc, c0 + mt * 128:c0 + (mt + 1) * 128], hb[:, kc, :],
                                         start=(kc == 0), stop=(kc == 7), reads=['win', hk], writes=[pk])
                                if dst is None:
                                    us, usk = ustg[mt % 2], 'ustg%d' % (mt % 2)
                                    k.op('act', 'activation', us[:], p[:, 0:TB], AF.Copy, reads=[pk], writes=[usk])
                                    k.dma('sp', uT_d[mt, :, tsl], us[:], reads=[usk], writes=['uT_d'])
                                else:
                                    k.op('act', 'activation', dst[:, mt, tsl], p[:, 0:TB], AF.Copy, scale=scale, reads=[pk], writes=[dk])
                        for j in range(TPB):
                            pi = 4 + cnt % 4
                            cnt += 1
                            p, pk = self.ps[pi], 'ps%d' % pi
                            for kc in range(8):
                                k.op('pe', 'matmul', p[:], hb[:, kc, j * 128:(j + 1) * 128], win[:, kc, 1536:2048],
                                     start=(kc == 0), stop=(kc == 7), reads=['win', hk], writes=[pk])
                            k.op('dve', 'tensor_copy', vtok[:, tb * TPB + j, :], p[:], reads=[pk], writes=['vtok'])
                    k.barrier()
                with ExitStack() as st:
                    E = [self.sb(st, 'sbE%d' % i, [128, TB]) for i in range(2)]
                    SP = [self.sb(st, 'sbSP%d' % i, [128, TB]) for i in range(2)]
                    ACC = [self.sb(st, 'sbAcc%d' % i, [128, TB]) for i in range(2)]
                    W = [self.sb(st, 'sbW%d' % i, [128, TB], BF16) for i in range(2)]
                    YO = [self.sb(st, 'sbY%d' % i, [64, TB], BF16) for i in range(2)]
                    it = 0
                    hq = 0
                    for h in range(8):
                        ft, pr = h // 2, slice(64 * (h % 2), 64 * (h % 2) + 64)
                        for qb in range(NB):
                            k.checkpoint()
                            hq += 1
                            pc, pck = self.ps[6 + hq % 2], 'ps%d' % (6 + hq % 2)
                            acc, ack = ACC[hq % 2], 'sbAcc%d' % (hq % 2)
                            qsl = slice(qb * TB, (qb + 1) * TB)
                            kts = list(range((qb + 1) * TPB - 1, -1, -1))
                            for n_, kt in enumerate(kts):
                                it += 1
                                b = it % 2
                                pz, pzk = self.ps[b], 'ps%d' % b
                                pa, pak = self.ps[2 + b], 'ps%d' % (2 + b)
                                m = kt - qb * TPB
                                ksl = slice(kt * 128, (kt + 1) * 128)
                                k.op('pe', 'matmul', pz[:, 0:TB], kT[pr, ft, ksl], qT[pr, ft, qsl], start=True, stop=True,
                                     reads=['kT', 'qT'], writes=[pzk])
                                k.op('act', 'activation', E[b][:], pz[:, 0:TB], AF.Exp, reads=[pzk], writes=['sbE%d' % b])
                                k.op('act', 'activation', SP[b][:], E[b][:], AF.Ln, bias=1.0, reads=['sbE%d' % b], writes=['sbSP%d' % b])
                                if m >= 0:
                                    k.op('pool', 'affine_select', SP[b][:], SP[b][:], pattern=[[1, TB]], compare_op=ALU.is_gt,
                                         fill=0.0, base=-128 * m, channel_multiplier=-1, reads=['sbSP%d' % b], writes=['sbSP%d' % b])
                                k.op('pe', 'matmul', pa[:, 0:TB], kT[pr, ft, ksl], qT[pr, ft, qsl], start=True, stop=False,
                                     reads=['kT', 'qT'], writes=[pak])
                                k.op('pe', 'matmul', pa[:, 0:TB], self.negtri[:], SP[b][:], start=False, stop=(n_ == 0),
                                     reads=['negtri', 'sbSP%d' % b], writes=[pak])
                                if n_ > 0:
                                    k.op('pe', 'matmul', pa[:, 0:TB], self.negones[:], acc[:], start=False, stop=True,
                                         reads=['negones', ack], writes=[pak])
                                k.op('act', 'activation', W[b][:], pa[:, 0:TB], AF.Exp, reads=[pak], writes=['sbW%d' % b])
                                if m >= 0:
                                    k.op('pool', 'affine_select', W[b][:], W[b][:], pattern=[[1, TB]], compare_op=ALU.is_gt,
                                         fill=0.0, base=-128 * m, channel_multiplier=-1, reads=['sbW%d' % b], writes=['sbW%d' % b])
                                if n_ == 0:
                                    k.op('pool', 'tensor_copy', acc[:], SP[b][:], reads=['sbSP%d' % b], writes=[ack])
                                elif n_ < len(kts) - 1:
                                    k.op('pool', 'tensor_tensor', acc[:], acc[:], SP[b][:], ALU.add, reads=['sbSP%d' % b, ack], writes=[ack])
                                k.op('pe', 'matmul', pc[0:64, 0:TB], vtok[:, kt, h * 64:(h + 1) * 64], W[b][:],
                                     start=(n_ == 0), stop=(n_ == len(kts) - 1), reads=['vtok', 'sbW%d' % b], writes=[pck])
                            yo, yok = YO[hq % 2], 'sbY%d' % (hq % 2)
                            k.op('dve', 'tensor_copy', yo[:], pc[0:64, 0:TB], reads=[pck], writes=[yok])
                            k.dma('sp', yb_d[h, :, qsl], yo[:], reads=[yok], writes=['yb_d'])
                    k.barrier()
            with ExitStack() as st1:
                with ExitStack() as st:
                    uT = self.sb(st, 'uT', [128, 4, T], BF16)
                    k.dma('sp', uT[:], uT_d.rearrange("i p t -> p i t"), reads=['uT_d'], writes=['uT'])
                    self.s5(uT, ya_d)
                with ExitStack() as st:
                    wglu = self.sb(st, 'wglu', [128, 4, 1024], BF16)
                    wout = self.sb(st, 'wout', [128, 8, 1024], BF16)
                    k.dma('pool', wglu[:], I['s5_w_glu'][0].rearrange("(kc k) n -> k kc n", k=128), writes=['wglu'])
                    wov = I['even_w_out'][0].rearrange("(kc k) n -> k kc n", k=128)
                    for kc in range(8):
                        k.dma('pool', wout[:, kc, :], wov[:, kc, :], writes=['wout'])
                    ymT = [self.sb(st, 'ymT%d' % i, [128, 8, TB], BF16) for i in range(2)]
                    yaB = [self.sb(st, 'yaB%d' % i, [128, 4, TB], BF16) for i in range(2)]
                    sg = [self.sb(st, 'sg%d' % i, [128, TB]) for i in range(2)]
                    xt = [self.sb(st, 'oxt%d' % i, [128, D]) for i in range(2)]
                    tmp = [self.sb(st, 'otmp%d' % i, [128, D]) for i in range(2)]
                    cnt = 0
                    for tb in range(NB):
                        k.checkpoint()
                        ym, ymk = ymT[tb % 2], 'ymT%d' % (tb % 2)
                        tsl = slice(tb * TB, (tb + 1) * TB)
                        k.dma('act', ym[:, 4:8, :], yb_d.rearrange("(ft hp) d t -> (hp d) ft t", hp=2)[:, :, tsl],
                              reads=['yb_d'], writes=[ymk])
                        yaT, yak = yaB[tb % 2], 'yaB%d' % (tb % 2)
                        k.dma('sp', yaT[:], ya_d.rearrange("i p t -> p i t")[:, :, tsl], reads=['ya_d'], writes=[yak])
                        for mt in range(4):
                            cnt += 1
                            b = cnt % 2
                            p1, p1k, p2, p2k = self.ps[b], 'ps%d' % b, self.ps[2 + b], 'ps%d' % (2 + b)
                            for kc in range(4):
                                k.op('pe', 'matmul', p1[:, 0:TB], wglu[:, kc, mt * 128:(mt + 1) * 128], yaT[:, kc, :],
                                     start=(kc == 0), stop=(kc == 3), reads=['wglu', yak], writes=[p1k])
                            for kc in range(4):
                                k.op('pe', 'matmul', p2[:, 0:TB], wglu[:, kc, 512 + mt * 128:512 + (mt + 1) * 128], yaT[:, kc, :],
                                     start=(kc == 0), stop=(kc == 3), reads=['wglu', yak], writes=[p2k])
                            k.op('act', 'activation', sg[b][:], p2[:, 0:TB], AF.Sigmoid, reads=[p2k], writes=['sg%d' % b])
                            k.op('dve', 'tensor_tensor', ym[:, mt, :], p1[:, 0:TB], sg[b][:], ALU.mult, reads=[p1k, 'sg%d' % b], writes=[ymk])
                        for j in range(TPB):
                            tile = tb * TPB + j
                            cnt += 1
                            b = cnt % 2
                            k.dma('sp', xt[b][:], xin[tile * 128:(tile + 1) * 128, :], reads=['xres'], writes=['oxt%d' % b])
                            for nh in range(2):
                                p, pk = self.ps[4 + 2 * b + nh], 'ps%d' % (4 + 2 * b + nh)
                                for kc in range(8):
                                    k.op('pe', 'matmul', p[:], ym[:, kc, j * 128:(j + 1) * 128], wout[:, kc, nh * 512:(nh + 1) * 512],
                                         start=(kc == 0), stop=(kc == 7), reads=[ymk, 'wout'], writes=[pk])
                                k.op('dve', 'tensor_tensor', tmp[b][:, nh * 512:(nh + 1) * 512], p[:], self.modbc[:, 2 * D + nh * 512:2 * D + (nh + 1) * 512],
                                     ALU.mult, reads=[pk, 'modbc'], writes=['otmp%d' % b])
                            k.op('pool', 'tensor_tensor', tmp[b][:], tmp[b][:], xt[b][:], ALU.add, reads=['otmp%d' % b, 'oxt%d' % b], writes=['otmp%d' % b])
                            k.dma('sp', xout[tile * 128:(tile + 1) * 128, :], tmp[b][:], reads=['otmp%d' % b], writes=['xres2'])
                    k.barrier()

    def s5(self, uT, ya_d):
        k, nc, T, NT, TB, NB = self.k, self.nc, self.T, self.NT, self.TB, self.NB
        I = self.I
        with ExitStack() as st:
            sb = lambda n, s, d=F32: self.sb(st, n, s, d)
            are, aim, dt = sb('are', [128, 16]), sb('aim', [128, 16]), sb('s5dt', [128, 16])
            for gl in range(2):
                ps_ = slice(64 * gl, 64 * gl + 64)
                k.dma('sp', are[ps_, :], I['s5_a_re'][0].rearrange("(pr gl) n -> gl n pr", gl=2)[gl], writes=['are'], allow_slow_non_contiguous=True)
                k.dma('sp', aim[ps_, :], I['s5_a_im'][0].rearrange("(pr gl) n -> gl n pr", gl=2)[gl], writes=['aim'], allow_slow_non_contiguous=True)
                k.dma('sp', dt[ps_, :].rearrange("p (o n) -> p o n", o=1),
                      I['s5_log_dt'].rearrange("o (pr gl) -> gl o pr", gl=2)[gl].partition_broadcast(64), writes=['s5dt'],
                      allow_slow_non_contiguous=True)
            V = {n: sb('s5_' + n, [128, 16]) for n in ('rho', 'th', 'q', 'cs', 'sn', 'abr', 'abi', 'den', 't1', 't2', 'cr', 'ci', 'ncr', 'nci')}
            qi = sb('s5qi', [128, 16], I32)
            KS = ['s5v']

            def o(eng, f, *a, **kw):
                k.op(eng, f, *a, reads=KS + ['are', 'aim', 's5dt', 'cst'], writes=KS, **kw)
            o('dve', 'tensor_scalar', are[:], are[:], -1e-4, None, ALU.min)
            o('act', 'activation', dt[:], dt[:], AF.Exp)
            o('dve', 'tensor_tensor', V['rho'][:], dt[:], are[:], ALU.mult)
            o('act', 'activation', V['rho'][:], V['rho'][:], AF.Exp)
            o('dve', 'tensor_tensor', V['th'][:], dt[:], aim[:], ALU.mult)

            def sincos(dst_s, dst_c, src, qf, qint, shape_key):
                o('dve', 'tensor_scalar', qint, src, 1.0 / (2 * PI), None, ALU.mult)
                o('dve', 'scalar_tensor_tensor', qf, qint, -2 * PI, src, ALU.mult, ALU.add)
                o('dve', 'tensor_scalar', qf, qf, PI, -PI, ALU.min, ALU.max)
                o('act', 'activation', dst_s, qf, AF.Sin)
                o('dve', 'scalar_tensor_tensor', qf, qf, -1.0, qf, ALU.mult, ALU.max)
                o('act', 'activation', dst_c, qf, AF.Sin, scale=-1.0, bias=self.cst[:, 3:4])
            k.op('pool', 'memset', self.cst[:, 3:4], PI / 2, reads=['cst'], writes=['cst'])
            sincos(V['sn'][:], V['cs'][:], V['th'][:], V['q'][:], qi[:], None)
            o('dve', 'tensor_tensor', V['abr'][:], V['rho'][:], V['cs'][:], ALU.mult)
            o('dve', 'tensor_tensor', V['abi'][:], V['rho'][:], V['sn'][:], ALU.mult)
            o('dve', 'tensor_scalar', V['abr'][:], V['abr'][:], -1.0, None, ALU.add)
            o('dve', 'tensor_tensor', V['den'][:], are[:], are[:], ALU.mult)
            o('dve', 'tensor_tensor', V['t1'][:], aim[:], aim[:], ALU.mult)
            o('dve', 'tensor_tensor', V['den'][:], V['den'][:], V['t1'][:], ALU.add)
            o('dve', 'reciprocal', V['den'][:], V['den'][:])
            o('dve', 'tensor_tensor', V['t1'][:], V['abr'][:], are[:], ALU.mult)
            o('dve', 'tensor_tensor', V['t2'][:], V['abi'][:], aim[:], ALU.mult)
            o('dve', 'tensor_tensor', V['t1'][:], V['t1'][:], V['t2'][:], ALU.add)
            o('dve', 'tensor_tensor', V['cr'][:], V['t1'][:], V['den'][:], ALU.mult)
            o('dve', 'tensor_tensor', V['t1'][:], V['abi'][:], are[:], ALU.mult)
            o('dve', 'tensor_tensor', V['t2'][:], V['abr'][:], aim[:], ALU.mult)
            o('dve', 'tensor_tensor', V['t1'][:], V['t1'][:], V['t2'][:], ALU.subtract)
            o('dve', 'tensor_tensor', V['ci'][:], V['t1'][:], V['den'][:], ALU.mult)
            o('dve', 'tensor_scalar', V['ncr'][:], V['cr'][:], -1.0, None, ALU.mult)
            o('dve', 'tensor_scalar', V['nci'][:], V['ci'][:], -1.0, None, ALU.mult)
            CmA, CmB = sb('CmA', [128, 16, 128], BF16), sb('CmB', [128, 16, 128], BF16)
            BmR, BmI = sb('BmR', [128, 16, 128], BF16), sb('BmI', [128, 16, 128], BF16)
            k.op('pool', 'memset', CmA[:], 0.0, writes=['CmA'])
            k.op('pool', 'memset', CmB[:], 0.0, writes=['CmB'])
            ctr, cti = sb('ctr', [128, 16, 16]), sb('cti', [128, 16, 16])
            stg = [sb('bstg%d' % i, [128, 128]) for i in range(2)]
            ct1, ct2 = sb('ct1', [128, 16]), sb('ct2', [128, 16])
            dcol = sb('s5dcol', [128, 4])
            k.dma('sp', dcol[:], I['s5_d'].rearrange("o g p -> o (g p)").rearrange("o (ft p) -> p (o ft)", p=128), writes=['s5dcol'],
                  allow_slow_non_contiguous=True)
            for gl in range(2):
                ps_ = slice(64 * gl, 64 * gl + 64)
                for pr in range(16):
                    k.checkpoint()
                    k.dma('sp', ctr[ps_, pr, :], I['s5_c_re'][0, 2 * pr + gl].rearrange("p n -> n p"), writes=['ctr'], allow_slow_non_contiguous=True)
                    k.dma('act', cti[ps_, pr, :], I['s5_c_im'][0, 2 * pr + gl].rearrange("p n -> n p"), writes=['cti'], allow_slow_non_contiguous=True)
            for pr in range(16):
                k.checkpoint()
                c0 = 32 * (pr % 4)
                k.op('dve', 'tensor_scalar', ct1[:], ctr[:, pr, :], V['cr'][:, pr:pr + 1], None, ALU.mult, reads=KS + ['ctr'], writes=['ct1'])
                k.op('dve', 'scalar_tensor_tensor', ct1[:], cti[:, pr, :], V['nci'][:, pr:pr + 1], ct1[:], ALU.mult, ALU.add,
                     reads=KS + ['cti', 'ct1'], writes=['ct1'])
                k.op('dve', 'tensor_scalar', ct2[:], ctr[:, pr, :], V['nci'][:, pr:pr + 1], None, ALU.mult, reads=KS + ['ctr'], writes=['ct2'])
                k.op('dve', 'scalar_tensor_tensor', ct2[:], cti[:, pr, :], V['ncr'][:, pr:pr + 1], ct2[:], ALU.mult, ALU.add,
                     reads=KS + ['cti', 'ct2'], writes=['ct2'])
                for gl in range(2):
                    ps_ = slice(64 * gl, 64 * gl + 64)
                    k.op('dve', 'tensor_copy', CmA[ps_, pr, c0 + 16 * gl:c0 + 16 * gl + 16], ct1[ps_, :], reads=['ct1', 'CmA'], writes=['CmA'])
                    k.op('dve', 'tensor_copy', CmB[ps_, pr, c0 + 16 * gl:c0 + 16 * gl + 16], ct2[ps_, :], reads=['ct2', 'CmB'], writes=['CmB'])
                for ri, (src, dstm, dk) in enumerate((('s5_b_re', BmR, 'BmR'), ('s5_b_im', BmI, 'BmI'))):
                    sg_, sk = stg[ri], 'bstg%d' % ri
                    k.op('pool', 'memset', sg_[:], 0.0, writes=[sk])
                    for gl in range(2):
                        r0 = c0 + 16 * gl
                        k.dma('sp', sg_[r0:r0 + 16, 64 * gl:64 * gl + 64], I[src][0, 2 * pr + gl].rearrange("n p -> p n"),
                              reads=[sk], writes=[sk], allow_slow_non_contiguous=True)
                    k.op('dve', 'tensor_copy', dstm[:, pr, :], sg_[:], reads=[sk], writes=[dk])
            itf = sb('s5itf', [128, T])
            with ExitStack() as stt:
                iti0 = self.sb(stt, 's5iti', [128, T], I32)
                k.op('pool', 'iota', iti0[:], pattern=[[1, T]], base=0, channel_multiplier=0, writes=['s5iti'])
                k.op('dve', 'tensor_copy', itf[:], iti0[:], reads=['s5iti'], writes=['s5itf'])
                k.barrier()
            Ct, St = sb('s5C', [128, T]), sb('s5S', [128, T])
            Xr, Xi = sb('s5Xr', [128, T]), sb('s5Xi', [128, T])
            yo = [sb('s5yo%d' % i, [128, T], BF16) for i in range(1)]
            yacc = sb('s5yacc', [128, T])
            tm = [sb('s5tm%d' % i, [128, TB]) for i in range(4)]
            Hr = [sb('s5Hr%d' % i, [128, TB], BF16) for i in range(2)]
            Hi = [sb('s5Hi%d' % i, [128, TB], BF16) for i in range(2)]
            for pr in range(16):
                k.checkpoint()
                ft = pr // 4
                k.op('dve', 'tensor_scalar', Xr[:], itf[:], V['th'][:, pr:pr + 1], None, ALU.mult, reads=KS + ['s5itf', 's5Xr'], writes=['s5Xr'])
                k.op('dve', 'tensor_scalar', Xi[:].bitcast(I32), Xr[:], 1.0 / (2 * PI), None, ALU.mult, reads=['s5Xr', 's5Xi'], writes=['s5Xi'])
                k.op('dve', 'scalar_tensor_tensor', Xr[:], Xi[:].bitcast(I32), -2 * PI, Xr[:], ALU.mult, ALU.add, reads=['s5Xi', 's5Xr'], writes=['s5Xr'])
                k.op('dve', 'tensor_scalar', Xr[:], Xr[:], PI, -PI, ALU.min, ALU.max, reads=['s5Xr'], writes=['s5Xr'])
                k.op('act', 'activation', St[:], Xr[:], AF.Sin, reads=['s5Xr', 's5S'], writes=['s5S'])
                k.op('dve', 'scalar_tensor_tensor', Xr[:], Xr[:], -1.0, Xr[:], ALU.mult, ALU.max, reads=['s5Xr', 's5S'], writes=['s5Xr'])
                k.op('act', 'activation', Ct[:], Xr[:], AF.Sin, scale=-1.0, bias=self.cst[:, 3:4], reads=['s5Xr', 's5C', 'cst'], writes=['s5C'])
                for tb in range(NB):
                    k.checkpoint()
                    tsl = slice(tb * TB, (tb + 1) * TB)
                    pA, pAk, pB, pBk = self.ps[(tb % 2) * 2], 'ps%d' % ((tb % 2) * 2), self.ps[(tb % 2) * 2 + 1], 'ps%d' % ((tb % 2) * 2 + 1)
                    k.op('pe', 'matmul', pA[:, 0:TB], BmR[:, pr, :], uT[:, ft, tsl], start=True, stop=True, reads=['BmR', 'uT'], writes=[pAk])
                    k.op('pe', 'matmul', pB[:, 0:TB], BmI[:, pr, :], uT[:, ft, tsl], start=True, stop=True, reads=['BmI', 'uT'], writes=[pBk])
                    k.op('dve', 'tensor_tensor', tm[0][:], pA[:, 0:TB], Ct[:, tsl], ALU.mult, reads=[pAk, 's5C'], writes=['s5tm0'])
                    k.op('dve', 'tensor_tensor', tm[1][:], pB[:, 0:TB], St[:, tsl], ALU.mult, reads=[pBk, 's5S'], writes=['s5tm1'])
                    k.op('pool', 'tensor_tensor', Xr[:, tsl], tm[0][:], tm[1][:], ALU.add, reads=['s5tm0', 's5tm1', 's5Xr'], writes=['s5Xr'])
                    k.op('dve', 'tensor_tensor', tm[2][:], pB[:, 0:TB], Ct[:, tsl], ALU.mult, reads=[pBk, 's5C'], writes=['s5tm2'])
                    k.op('dve', 'tensor_tensor', tm[3][:], pA[:, 0:TB], St[:, tsl], ALU.mult, reads=[pAk, 's5S'], writes=['s5tm3'])
                    k.op('pool', 'tensor_tensor', Xi[:, tsl], tm[2][:], tm[3][:], ALU.subtract, reads=['s5tm2', 's5tm3', 's5Xi'], writes=['s5Xi'])
                rb = V['rho'][:, pr:pr + 1].to_broadcast([128, T])
                k.op('dve', 'tensor_tensor_scan', Xr[:], rb, Xr[:], 0.0, ALU.mult, ALU.add, reads=KS + ['s5Xr'], writes=['s5Xr'])
                k.op('dve', 'tensor_tensor_scan', Xi[:], rb, Xi[:], 0.0, ALU.mult, ALU.add, reads=KS + ['s5Xi'], writes=['s5Xi'])
                for tb in range(NB):
                    k.checkpoint()
                    tsl = slice(tb * TB, (tb + 1) * TB)
                    b = tb % 2
                    k.op('dve', 'tensor_tensor', tm[0][:], Xr[:, tsl], Ct[:, tsl], ALU.mult, reads=['s5Xr', 's5C'], writes=['s5tm0'])
                    k.op('pool', 'tensor_tensor', tm[1][:], Xi[:, tsl], St[:, tsl], ALU.mult, reads=['s5Xi', 's5S'], writes=['s5tm1'])
                    k.op('dve', 'tensor_tensor', Hr[b][:], tm[0][:], tm[1][:], ALU.subtract, reads=['s5tm0', 's5tm1'], writes=['s5Hr%d' % b])
                    k.op('dve', 'tensor_tensor', tm[2][:], Xr[:, tsl], St[:, tsl], ALU.mult, reads=['s5Xr', 's5S'], writes=['s5tm2'])
                    k.op('pool', 'tensor_tensor', tm[3][:], Xi[:, tsl], Ct[:, tsl], ALU.mult, reads=['s5Xi', 's5C'], writes=['s5tm3'])
                    k.op('dve', 'tensor_tensor', Hi[b][:], tm[2][:], tm[3][:], ALU.add, reads=['s5tm2', 's5tm3'], writes=['s5Hi%d' % b])
                    p, pk = self.ps[4 + b], 'ps%d' % (4 + b)
                    k.op('pe', 'matmul', p[:, 0:TB], CmA[:, pr, :], Hr[b][:], start=True, stop=False, reads=['CmA', 's5Hr%d' % b], writes=[pk])
                    k.op('pe', 'matmul', p[:, 0:TB], CmB[:, pr, :], Hi[b][:], start=False, stop=True, reads=['CmB', 's5Hi%d' % b], writes=[pk])
                    if pr % 4 == 0:
                        k.op('dve', 'scalar_tensor_tensor', yacc[:, tsl], uT[:, ft, tsl], dcol[:, ft:ft + 1], p[:, 0:TB], ALU.mult, ALU.add,
                             reads=['uT', 's5dcol', pk, 's5yacc'], writes=['s5yacc'])
                    else:
                        k.op('dve', 'tensor_tensor', yacc[:, tsl], yacc[:, tsl], p[:, 0:TB], ALU.add, reads=[pk, 's5yacc'], writes=['s5yacc'])
                if pr % 4 == 3:
                    k.op('pool', 'tensor_tensor', Xr[:], yacc[:], yacc[:], ALU.mult, reads=['s5yacc', 's5Xr'], writes=['s5Xr'])
                    k.op('dve', 'tensor_scalar', Xr[:], Xr[:], 0.044715, 1.0, ALU.mult, ALU.add, reads=['s5Xr'], writes=['s5Xr'])
                    k.op('pool', 'tensor_tensor', Xr[:], Xr[:], yacc[:], ALU.mult, reads=['s5yacc', 's5Xr'], writes=['s5Xr'])
                    k.op('act', 'activation', Xr[:], Xr[:], AF.Sigmoid, scale=1.5957691216, reads=['s5Xr'], writes=['s5Xr'])
                    k.op('dve', 'tensor_tensor', yo[0][:], Xr[:], yacc[:], ALU.mult, reads=['s5Xr', 's5yacc', 's5yo'], writes=['s5yo'])
                    k.dma('sp', ya_d[ft], yo[0][:], reads=['s5yo'], writes=['ya_d'])
            k.barrier()


    def outproj(self, wname, xin, xout, fill):
        k, T, TB, NB, TPB = self.k, self.T, self.TB, self.NB, self.TPB
        with ExitStack() as st:
            wout = self.sb(st, 'wout', [128, 8, 1024], BF16)
            wov = self.I[wname][0].rearrange("(kc k) n -> k kc n", k=128)
            for kc in range(8):
                k.dma('pool', wout[:, kc, :], wov[:, kc, :], writes=['wout'])
            ymT = [self.sb(st, 'ymT%d' % i, [128, 8, TB], BF16) for i in range(2)]
            xt = [self.sb(st, 'oxt%d' % i, [128, D]) for i in range(2)]
            tmp = [self.sb(st, 'otmp%d' % i, [128, D]) for i in range(2)]
            cnt = 0
            for tb in range(NB):
                k.checkpoint()
                ym, ymk = ymT[tb % 2], 'ymT%d' % (tb % 2)
                fill(st, tb, ym, ymk)
                for j in range(TPB):
                    tile = tb * TPB + j
                    cnt += 1
                    b = cnt % 2
                    k.dma('sp', xt[b][:], xin[tile * 128:(tile + 1) * 128, :], reads=['xres'], writes=['oxt%d' % b])
                    for nh in range(2):
                        p, pk = self.ps[4 + 2 * b + nh], 'ps%d' % (4 + 2 * b + nh)
                        for kc in range(8):
                            k.op('pe', 'matmul', p[:], ym[:, kc, j * 128:(j + 1) * 128], wout[:, kc, nh * 512:(nh + 1) * 512],
                                 start=(kc == 0), stop=(kc == 7), reads=[ymk, 'wout'], writes=[pk])
                        k.op('dve', 'tensor_tensor', tmp[b][:, nh * 512:(nh + 1) * 512], p[:], self.modbc[:, 2 * D + nh * 512:2 * D + (nh + 1) * 512],
                             ALU.mult, reads=[pk, 'modbc'], writes=['otmp%d' % b])
                    k.op('pool', 'tensor_tensor', tmp[b][:], tmp[b][:], xt[b][:], ALU.add, reads=['otmp%d' % b, 'oxt%d' % b], writes=['otmp%d' % b])
                    k.dma('sp', xout[tile * 128:(tile + 1) * 128, :], tmp[b][:], reads=['otmp%d' % b], writes=['xres2'])
            k.barrier()

    def odd_mixer(self, xin, xout):
        k, nc, T, NT, TB, NB, TPB = self.k, self.nc, self.T, self.NT, self.TB, self.NB, self.TPB
        I = self.I
        pT = self.scr('projT', [27, 128, T])
        F5 = self.scr('F5', [20, 128, T])
        vT_d = self.scr('vT_d', [4, 128, T])
        gT_d = self.scr('gT_d', [4, 128, T])
        bon_d = self.scr('bon_d', [4, 128, T])
        yr_d = self.scr('yr_d', [4, 128, T])
        ys_d = self.scr('ys_d', [8, 64, T])
        ymr_d = self.scr('ymr_d', [4, 128, T], BF16)
        yms_d = self.scr('yms_d', [4, 128, T], BF16)

        def col(st, name, src, n):
            t = self.sb(st, name, [128, n])
            k.dma('sp', t[:], src, writes=[name], allow_slow_non_contiguous=True)
            return t
        with ExitStack() as st:
            win = self.sb(st, 'owin', [128, 8, 3336], BF16)
            wv = I['odd_w_in'][0].rearrange("(kc k) n -> k kc n", k=128)
            for kc in range(8):
                k.dma('pool', win[:, kc, :], wv[:, kc, :], writes=['owin'])
            bufs = self.norm_bufs(st)
            hT = [self.sb(st, 'hT%d' % i, [128, 8, TB], BF16) for i in range(2)]
            stg = [self.sb(st, 'pstg%d' % i, [128, TB]) for i in range(4)]
            cnt = 0
            for tb in range(NB):
                k.checkpoint()
                hb, hk = hT[tb % 2], 'hT%d' % (tb % 2)
                for j in range(TPB):
                    self.norm_tile(bufs, xin, tb * TPB + j, self.modbc[:, D:2 * D], self.modbc[:, 0:D], hb[:, :, j * 128:(j + 1) * 128], keys=[hk])
                tsl = slice(tb * TB, (tb + 1) * TB)
                for mt in range(27):
                    M = 128 if mt < 26 else 8
                    cnt += 1
                    pi = 4 + cnt % 4
                    p, pk = self.ps[pi], 'ps%d' % pi
                    sg_, sk = stg[cnt % 4], 'pstg%d' % (cnt % 4)
                    for kc in range(8):
                        k.op('pe', 'matmul', p[0:M, 0:TB], win[:, kc, mt * 128:mt * 128 + M], hb[:, kc, :], start=(kc == 0), stop=(kc == 7),
                             reads=['owin', hk], writes=[pk])
                    if cnt % 2:
                        k.op('act', 'activation', sg_[0:M, :], p[0:M, 0:TB], AF.Copy, reads=[pk], writes=[sk])
                    else:
                        k.op('dve', 'tensor_copy', sg_[0:M, :], p[0:M, 0:TB], reads=[pk], writes=[sk])
                    k.dma('sp' if cnt % 2 else 'act', pT[mt, 0:M, tsl], sg_[0:M, :], reads=[sk], writes=['projT'])
            k.barrier()
        with ExitStack() as st:
            sb = lambda n, s, d=F32: self.sb(st, n, s, d)
            mu = col(st, 'rwmu', I['rw_mu'].rearrange("o (mt p) -> p (o mt)", p=128), 14)
            omu = sb('rwomu', [128, 14])
            k.op('dve', 'tensor_scalar', omu[:], mu[:], -1.0, 1.0, ALU.mult, ALU.add, reads=['rwmu'], writes=['rwomu'])
            cw0 = col(st, 'rww0', I['rw_w0'].rearrange("o (mt p) -> p (o mt)", p=128), 4)
            ca0 = col(st, 'rwa0', I['rw_a0'].rearrange("o (mt p) -> p (o mt)", p=128), 4)
            ckk = col(st, 'rwkk', I['rw_k_k'].rearrange("o (mt p) -> p (o mt)", p=128), 4)
            cka = col(st, 'rwka', I['rw_k_a'].rearrange("o (mt p) -> p (o mt)", p=128), 4)
            crk = col(st, 'rwrk', I['rw_r_k'].rearrange("o h n -> o (h n)").rearrange("o (mt p) -> p (o mt)", p=128), 4)
            omka = sb('rwomka', [128, 4])
            k.op('dve', 'tensor_scalar', omka[:], cka[:], -1.0, 1.0, ALU.mult, ALU.add, reads=['rwka'], writes=['rwomka'])
            w2 = sb('rww2', [64, 512], BF16)
            a2 = sb('rwa2', [128, 512], BF16)
            g2 = sb('rwg2', [128, 512], BF16)
            k.dma('pool', w2[:], I['rw_w2'][0], writes=['rww2'])
            k.dma('pool', a2[64:128, :], I['rw_a2'][0], writes=['rwa2'])
            k.dma('pool', g2[:], I['rw_g2'][0], writes=['rwg2'])
            P = [sb('rwP%d' % i, [128, TB]) for i in range(14)]
            psh = [sb('rwsh%d' % i, [128, TB]) for i in range(2)]
            thb = sb('rwth', [128, TB], BF16)
            sgb = sb('rwsg', [128, TB], BF16)
            tA, tB_, tC, tD = sb('rwtA', [128, TB]), sb('rwtB', [128, TB]), sb('rwtC', [128, TB]), sb('rwtD', [128, TB])
            aT = sb('rwaT', [128, TB])
            PK = ['rwP']
            for tb in range(NB):
                k.checkpoint()
                t0 = tb * TB
                tsl = slice(t0, t0 + TB)
                for mt in range(14):
                    sh, shk = psh[mt % 2], 'rwsh%d' % (mt % 2)
                    k.dma('sp', P[mt][:], pT[mt, :, tsl], reads=['projT'], writes=['rwP%d' % mt])
                    if tb == 0:
                        k.op('pool', 'memset', sh[:, 0:1], 0.0, writes=[shk])
                        k.dma('act', sh[:, 1:TB], pT[mt, :, 0:TB - 1], reads=['projT', shk], writes=[shk])
                    else:
                        k.dma('act', sh[:, :], pT[mt, :, t0 - 1:t0 + TB - 1], reads=['projT'], writes=[shk])
                    k.op('dve', 'tensor_scalar', P[mt][:], P[mt][:], omu[:, mt:mt + 1], None, ALU.mult, reads=['rwP%d' % mt, 'rwomu'], writes=['rwP%d' % mt])
                    k.op('dve', 'scalar_tensor_tensor', P[mt][:], sh[:], mu[:, mt:mt + 1], P[mt][:], ALU.mult, ALU.add,
                         reads=[shk, 'rwmu', 'rwP%d' % mt], writes=['rwP%d' % mt])
                Pk = lambda i: 'rwP%d' % i
                k.op('act', 'activation', thb[0:64, :], P[12][0:64, :], AF.Tanh, reads=[Pk(12)], writes=['rwth'])
                k.op('act', 'activation', thb[64:128, :], P[12][64:128, :], AF.Copy, reads=[Pk(12), 'rwth'], writes=['rwth'])
                k.op('act', 'activation', sgb[:], P[13][:], AF.Sigmoid, reads=[Pk(13)], writes=['rwsg'])
                for ft in range(4):
                    fsl = slice(ft * 128, (ft + 1) * 128)
                    r_, k_, v_ = P[ft], P[4 + ft], P[8 + ft]
                    p0, p1, p2, p3 = self.ps[0], self.ps[1], self.ps[2], self.ps[3]
                    k.op('pe', 'matmul', p0[:, 0:TB], w2[0:64, fsl], thb[0:64, :], start=True, stop=True, reads=['rww2', 'rwth'], writes=['ps0'])
                    k.op('act', 'activation', tA[:], p0[:, 0:TB], AF.Sigmoid, bias=cw0[:, ft:ft + 1], reads=['ps0', 'rww0', 'rwtA'], writes=['rwtA'])
                    k.op('act', 'activation', tA[:], tA[:], AF.Exp, scale=-0.6065306597, reads=['rwtA'], writes=['rwtA'])
                    k.dma('sp', F5[4 + ft, :, tsl], tA[:], reads=['rwtA'], writes=['F5'])
                    k.op('pe', 'matmul', p1[:, 0:TB], a2[64:128, fsl], thb[64:128, :], start=True, stop=True, reads=['rwa2', 'rwth'], writes=['ps1'])
                    k.op('act', 'activation', aT[:], p1[:, 0:TB], AF.Sigmoid, bias=ca0[:, ft:ft + 1], reads=['ps1', 'rwa0', 'rwaT'], writes=['rwaT'])
                    k.op('pe', 'matmul', p2[:, 0:TB], g2[:, fsl], sgb[:], start=True, stop=True, reads=['rwg2', 'rwsg'], writes=['ps2'])
                    k.op('act', 'activation', tB_[:], p2[:, 0:TB], AF.Copy, reads=['ps2', 'rwtB'], writes=['rwtB'])
                    k.dma('act', gT_d[ft, :, tsl], tB_[:], reads=['rwtB'], writes=['gT_d'])
                    k.op('dve', 'tensor_scalar', tC[:], k_[:], ckk[:, ft:ft + 1], None, ALU.mult, reads=[Pk(4 + ft), 'rwkk', 'rwtC'], writes=['rwtC'])
                    k.op('pool', 'tensor_tensor', tD[:], tC[:], tC[:], ALU.mult, reads=['rwtC', 'rwtD'], writes=['rwtD'])
                    k.op('pe', 'matmul', p3[:, 0:TB], self.blk[:], tD[:], start=True, stop=True, reads=['blk', 'rwtD'], writes=['ps3'])
                    k.op('act', 'activation', tD[:], p3[:, 0:TB], AF.Sqrt, reads=['ps3', 'rwtD'], writes=['rwtD'])
                    k.op('dve', 'tensor_scalar', tD[:], tD[:], 1e-12, None, ALU.max, reads=['rwtD'], writes=['rwtD'])
                    k.op('dve', 'reciprocal', tD[:], tD[:], reads=['rwtD'], writes=['rwtD'])
                    k.op('dve', 'tensor_tensor', tC[:], tC[:], tD[:], ALU.mult, reads=['rwtC', 'rwtD'], writes=['rwtC'])
                    k.op('dve', 'tensor_tensor', tD[:], tC[:], aT[:], ALU.mult, reads=['rwtC', 'rwaT', 'rwtD'], writes=['rwtD'])
                    k.dma('sp', F5[8 + ft, :, tsl], tD[:], reads=['rwtD'], writes=['F5'])
                    k.op('dve', 'tensor_scalar', tC[:], tC[:], -1.0, None, ALU.mult, reads=['rwtC'], writes=['rwtC'])
                    k.dma('act', F5[0 + ft, :, tsl], tC[:], reads=['rwtC'], writes=['F5'])
                    k.op('dve', 'tensor_scalar', aT[:], aT[:], cka[:, ft:ft + 1], omka[:, ft:ft + 1], ALU.mult, ALU.add, reads=['rwaT', 'rwka', 'rwomka'], writes=['rwaT'])
                    k.op('dve', 'tensor_tensor', k_[:], k_[:], aT[:], ALU.mult, reads=[Pk(4 + ft), 'rwaT'], writes=[Pk(4 + ft)])
                    k.dma('sp', F5[12 + ft, :, tsl], k_[:], reads=[Pk(4 + ft)], writes=['F5'])
                    k.dma('act', F5[16 + ft, :, tsl], r_[:], reads=[Pk(ft)], writes=['F5'])
                    k.dma('sp', vT_d[ft, :, tsl], v_[:], reads=[Pk(8 + ft)], writes=['vT_d'])
                    k.op('dve', 'scalar_tensor_tensor', tA[:], r_[:], crk[:, ft:ft + 1], k_[:], ALU.mult, ALU.mult, reads=[Pk(ft), Pk(4 + ft), 'rwrk', 'rwtA'], writes=['rwtA'])
                    k.op('pe', 'matmul', p0[:, 0:TB], self.blk[:], tA[:], start=True, stop=True, reads=['blk', 'rwtA'], writes=['ps0'])
                    k.op('dve', 'tensor_tensor', tB_[:], p0[:, 0:TB], v_[:], ALU.mult, reads=['ps0', Pk(8 + ft), 'rwtB'], writes=['rwtB'])
                    k.dma('act', bon_d[ft, :, tsl], tB_[:], reads=['rwtB'], writes=['bon_d'])
            k.barrier()
        with ExitStack() as st:
            sb = lambda n, s, d=F32: self.sb(st, n, s, d)
            SEL = sb('rwSEL', [128, 64, 128], BF16)
            k.op('pool', 'memset', SEL[:], 0.0, writes=['rwSEL'])
            k.op('pool', 'memset', SEL[0:64, :, 0:64], 1.0, reads=['rwSEL'], writes=['rwSEL'])
            k.op('pool', 'memset', SEL[64:128, :, 64:128], 1.0, reads=['rwSEL'], writes=['rwSEL'])
            k.op('pool', 'affine_select', SEL[0:64, :, 0:64], SEL[0:64, :, 0:64], pattern=[[1, 64], [0, 64]], compare_op=ALU.is_equal,
                 fill=0.0, base=0, channel_multiplier=-1, reads=['rwSEL'], writes=['rwSEL'])
            k.op('pool', 'affine_select', SEL[64:128, :, 64:128], SEL[64:128, :, 64:128], pattern=[[1, 64], [0, 64]], compare_op=ALU.is_equal,
                 fill=0.0, base=0, channel_multiplier=-1, reads=['rwSEL'], writes=['rwSEL'])
            Fb = [sb('rwF%d' % i, [128, 20, 128]) for i in range(2)]
            vcb = [sb('rwvc%d' % i, [128, 4, 64]) for i in range(2)]
            RBh = [sb('rwRh%d' % i, [128, 20, 64], BF16) for i in range(2)]
            RBl = [sb('rwRl%d' % i, [128, 20, 64], BF16) for i in range(2)]
            yb = [sb('rwyb%d' % i, [128, 4, 64]) for i in range(2)]
            S = sb('rwS', [128, 4, 64])
            kv = [sb('rwkv%d' % i, [128, 4, 64]) for i in range(2)]
            tmp = sb('rwtmp', [128, 4, 64])
            sa = sb('rwsa', [128, 4])
            k.op('dve', 'memset', S[:], 0.0, writes=['rwS'])
            SK = ['rwS']
            F5v = F5.rearrange("i p t -> p i t")
            for tl in range(T // 64):
                k.checkpoint()
                b = tl % 2
                tsl = slice(tl * 64, tl * 64 + 64)
                Fk, vk, Rk, yk = 'rwF%d' % b, 'rwvc%d' % b, 'rwR%d' % b, 'rwyb%d' % b
                for g4 in range(5):
                    k.dma('sp', Fb[b][:, g4 * 4:g4 * 4 + 4, 0:64], F5v[:, g4 * 4:g4 * 4 + 4, tsl], reads=['F5', Fk], writes=[Fk])
                    k.dma('act', Fb[b][:, g4 * 4:g4 * 4 + 4, 64:128], F5v[:, g4 * 4:g4 * 4 + 4, tsl], reads=['F5', Fk], writes=[Fk])
                k.dma('sp', vcb[b][:], vT_d.rearrange("i p t -> p i t")[:, :, tsl], reads=['vT_d'], writes=[vk])
                for g4 in range(5):
                    p, pk = self.ps[g4 % 2], 'ps%d' % (g4 % 2)
                    for j in range(4):
                        i = g4 * 4 + j
                        k.op('pe', 'transpose', p[:, j * 128:(j + 1) * 128], Fb[b][:, i, :], self.ident[:], reads=[Fk, 'ident'], writes=[pk])
                    pv = p[:].rearrange("p (a b) -> p a b", a=4)
                    for hp in range(2):
                        ps_ = slice(64 * hp, 64 * hp + 64)
                        k.op('act', 'activation', RBh[b][ps_, g4 * 4:g4 * 4 + 4, :], pv[ps_, :, 64 * hp:64 * hp + 64], AF.Copy, reads=[pk, Rk], writes=[Rk])
                    for hp in range(2):
                        ps_ = slice(64 * hp, 64 * hp + 64)
                        k.op('dve', 'tensor_tensor', RBl[b][ps_, g4 * 4:g4 * 4 + 4, :], pv[ps_, :, 64 * hp:64 * hp + 64], RBh[b][ps_, g4 * 4:g4 * 4 + 4, :],
                             ALU.subtract, reads=[pk, Rk], writes=[Rk])
                for s_ in range(64):
                    bs = s_ % 2
                    banks = [(self.ps[2 + 3 * bs + i], 'ps%d' % (2 + 3 * bs + i)) for i in range(3)]
                    for bi, c0, i0, n in ((0, 0, 0, 8), (1, 0, 8, 4), (1, 256, 16, 4), (2, 0, 12, 4)):
                        p, pk = banks[bi]
                        w_ = n * 64
                        k.op('pe', 'matmul', p[:, c0:c0 + w_], SEL[:, s_, :], RBh[b][:, i0:i0 + n, :], start=True, stop=False, reads=['rwSEL', Rk], writes=[pk])
                        k.op('pe', 'matmul', p[:, c0:c0 + w_], SEL[:, s_, :], RBl[b][:, i0:i0 + n, :], start=False, stop=True, reads=['rwSEL', Rk], writes=[pk])
                    v3 = lambda p, c0: p[:, c0:c0 + 256].rearrange("p (a b) -> p a b", a=4)
                    a_bc, d_bc = v3(banks[0][0], 0), v3(banks[0][0], 256)
                    b_bc, r_bc = v3(banks[1][0], 0), v3(banks[1][0], 256)
                    k_bc = v3(banks[2][0], 0)
                    k0, k1, k2 = banks[0][1], banks[1][1], banks[2][1]
                    for hq in range(4):
                        k.op('act', 'activation', kv[bs][:, hq, :], k_bc[:, hq, :], AF.Copy, scale=vcb[b][:, hq, s_:s_ + 1],
                             reads=[k2, vk, 'rwkv%d' % bs], writes=['rwkv%d' % bs])
                    k.op('dve', 'tensor_tensor', tmp[:], S[:], a_bc, ALU.mult, reads=SK + [k0, 'rwtmp'], writes=['rwtmp'])
                    k.op('dve', 'tensor_reduce', sa[:], tmp[:], AX.X, ALU.add, reads=['rwtmp', 'rwsa'], writes=['rwsa'])
                    k.op('dve', 'tensor_tensor', S[:], S[:], d_bc, ALU.mult, reads=SK + [k0], writes=SK)
                    k.op('dve', 'tensor_tensor', tmp[:], b_bc, sa[:].unsqueeze(2).to_broadcast([128, 4, 64]), ALU.mult,
                         reads=[k1, 'rwsa', 'rwtmp'], writes=['rwtmp'])
                    k.op('dve', 'tensor_tensor', S[:], S[:], tmp[:], ALU.add, reads=SK + ['rwtmp'], writes=SK)
                    k.op('dve', 'tensor_tensor', S[:], S[:], kv[bs][:], ALU.add, reads=SK + ['rwkv%d' % bs], writes=SK)
                    k.op('dve', 'tensor_tensor', tmp[:], S[:], r_bc, ALU.mult, reads=SK + [k1, 'rwtmp'], writes=['rwtmp'])
                    k.op('dve', 'tensor_reduce', yb[b][:, :, s_:s_ + 1], tmp[:], AX.X, ALU.add, reads=['rwtmp', yk], writes=[yk])
                k.dma('sp', yr_d.rearrange("i p t -> p i t")[:, :, tsl], yb[b][:], reads=[yk], writes=['yr_d'])
            k.barrier()
        with ExitStack() as st:
            sb = lambda n, s, d=F32: self.sb(st, n, s, d)
            lnw = col(st, 'rwlnw', I['rw_ln_w'].rearrange("o (mt p) -> p (o mt)", p=128), 4)
            lnb = col(st, 'rwlnb', I['rw_ln_b'].rearrange("o (mt p) -> p (o mt)", p=128), 4)
            blks = sb('blks', [128, 128])
            k.op('dve', 'tensor_scalar', blks[:], self.blk[:], 1.0 / 64, None, ALU.mult, reads=['blk'], writes=['blks'])
            Y = [sb('c2y%d' % i, [128, TB]) for i in range(2)]
            Q = [sb('c2q%d' % i, [128, TB]) for i in range(2)]
            Bn = [sb('c2b%d' % i, [128, TB]) for i in range(2)]
            G = [sb('c2g%d' % i, [128, TB]) for i in range(2)]
            O = [sb('c2o%d' % i, [128, TB], BF16) for i in range(2)]
            it = 0
            for tb in range(NB):
                k.checkpoint()
                tsl = slice(tb * TB, (tb + 1) * TB)
                for ft in range(4):
                    it += 1
                    b = it % 2
                    yk, qk, bk, gk, ok = 'c2y%d' % b, 'c2q%d' % b, 'c2b%d' % b, 'c2g%d' % b, 'c2o%d' % b
                    p0, p0k, p1, p1k = self.ps[b], 'ps%d' % b, self.ps[2 + b], 'ps%d' % (2 + b)
                    k.dma('sp', Y[b][:], yr_d[ft, :, tsl], reads=['yr_d'], writes=[yk])
                    k.dma('act', Bn[b][:], bon_d[ft, :, tsl], reads=['bon_d'], writes=[bk])
                    k.dma('sp', G[b][:], gT_d[ft, :, tsl], reads=['gT_d'], writes=[gk])
                    k.op('pe', 'matmul', p0[:, 0:TB], blks[:], Y[b][:], start=True, stop=True, reads=['blks', yk], writes=[p0k])
                    k.op('dve', 'tensor_tensor', Y[b][:], Y[b][:], p0[:, 0:TB], ALU.subtract, reads=[yk, p0k], writes=[yk])
                    k.op('pool', 'tensor_tensor', Q[b][:], Y[b][:], Y[b][:], ALU.mult, reads=[yk, qk], writes=[qk])
                    k.op('pe', 'matmul', p1[:, 0:TB], blks[:], Q[b][:], start=True, stop=True, reads=['blks', qk], writes=[p1k])
                    k.op('dve', 'tensor_scalar', Q[b][:], p1[:, 0:TB], 64e-5, None, ALU.add, reads=[p1k, qk], writes=[qk])
                    k.op('act', 'activation', Q[b][:], Q[b][:], AF.Sqrt, reads=[qk], writes=[qk])
                    k.op('dve', 'reciprocal', Q[b][:], Q[b][:], reads=[qk], writes=[qk])
                    k.op('dve', 'tensor_tensor', Y[b][:], Y[b][:], Q[b][:], ALU.mult, reads=[yk, qk], writes=[yk])
                    k.op('dve', 'tensor_scalar', Y[b][:], Y[b][:], lnw[:, ft:ft + 1], lnb[:, ft:ft + 1], ALU.mult, ALU.add, reads=[yk, 'rwlnw', 'rwlnb'], writes=[yk])
                    k.op('pool', 'tensor_tensor', Y[b][:], Y[b][:], Bn[b][:], ALU.add, reads=[yk, bk], writes=[yk])
                    k.op('dve', 'tensor_tensor', O[b][:], Y[b][:], G[b][:], ALU.mult, reads=[yk, gk, ok], writes=[ok])
                    k.dma('act', ymr_d[ft, :, tsl], O[b][:], reads=[ok], writes=['ymr_d'])
            k.barrier()
        self.ssd(pT, ys_d, yms_d)

        def fill(st, tb, ym, ymk):
            tsl = slice(tb * TB, (tb + 1) * TB)
            k.dma('sp', ym[:, 0:4, :], ymr_d.rearrange("i p t -> p i t")[:, :, tsl], reads=['ymr_d'], writes=[ymk])
            k.dma('act', ym[:, 4:8, :], yms_d.rearrange("i p t -> p i t")[:, :, tsl], reads=['yms_d'], writes=[ymk])
        self.outproj('odd_w_out', xin, xout, fill)

    def ssd(self, pT, ys_d, yms_d):
        k, nc, T, NT, TB, NB, TPB = self.k, self.nc, self.T, self.NT, self.TB, self.NB, self.TPB
        I = self.I
        xc_d = self.scr('xc_d', [4, 128, T])
        with ExitStack() as st0:
            sb0 = lambda n, s, d=F32: self.sb(st0, n, s, d)
            BT, CT = sb0('ssBT', [128, 2, T], BF16), sb0('ssCT', [128, 2, T], BF16)
            xdt = sb0('ssxdt', [128, NT, 512], BF16)
            cs = sb0('sscs', [8, T])
            dtT = sb0('ssdt', [8, T])
            ncsT = sb0('ssncsT', [128, NT, 8])
            dtk = sb0('ssdtk', [128, NT, 8])
            with ExitStack() as st:
                sb = lambda n, s, d=F32: self.sb(st, n, s, d)
                cw = sb('sscw', [128, 8, 4])
                for w in range(4):
                    k.dma('sp', cw[:, :, w], I['m2_conv_w'][0, w:w + 1, :].rearrange("o (mt p) -> p (o mt)", p=128), reads=['sscw'], writes=['sscw'],
                          allow_slow_non_contiguous=True)
                cb = sb('sscb', [128, 8])
                k.dma('sp', cb[:], I['m2_conv_b'].rearrange("o (mt p) -> p (o mt)", p=128), writes=['sscb'], allow_slow_non_contiguous=True)
                dtb = sb('ssdtb', [8, 1])
                alog = sb('ssalog', [8, 1])
                k.dma('sp', dtb[:], I['m2_dt_bias'].rearrange("o h -> h o"), writes=['ssdtb'], allow_slow_non_contiguous=True)
                k.dma('sp', alog[:], I['m2_a_log'].rearrange("o h -> h o"), writes=['ssalog'], allow_slow_non_contiguous=True)
                k.op('act', 'activation', alog[:], alog[:], AF.Exp, reads=['ssalog'], writes=['ssalog'])
                k.op('dve', 'tensor_scalar', alog[:], alog[:], -1.0, None, ALU.mult, reads=['ssalog'], writes=['ssalog'])
                k.dma('sp', dtT[:], pT[26, 0:8, :], reads=['projT'], writes=['ssdt'])
                k.op('act', 'activation', dtT[:], dtT[:], AF.Exp, bias=dtb[:, 0:1], reads=['ssdt', 'ssdtb'], writes=['ssdt'])
                k.op('act', 'activation', dtT[:], dtT[:], AF.Ln, bias=1.0, reads=['ssdt'], writes=['ssdt'])
                k.op('dve', 'tensor_scalar', cs[:], dtT[:], alog[:, 0:1], None, ALU.mult, reads=['ssdt', 'ssalog'], writes=['sscs'])
                k.op('dve', 'tensor_tensor_scan', cs[:], self.cst[0:8, 1:2].to_broadcast([8, T]), cs[:], 0.0, ALU.mult, ALU.add, reads=['sscs', 'cst'], writes=['sscs'])
                for tl in range(NT):
                    p, pk = self.ps[tl % 2], 'ps%d' % (tl % 2)
                    k.op('pe', 'transpose', p[:, 0:8], cs[0:8, tl * 128:(tl + 1) * 128], self.ident[0:8, 0:8], reads=['sscs', 'ident'], writes=[pk])
                    k.op('pe', 'transpose', p[:, 8:16], dtT[0:8, tl * 128:(tl + 1) * 128], self.ident[0:8, 0:8], reads=['ssdt', 'ident'], writes=[pk])
                    k.op('dve', 'tensor_scalar', ncsT[:, tl, :], p[:, 0:8], -1.0, None, ALU.mult, reads=[pk], writes=['ssncsT'])
                    k.op('dve', 'tensor_copy', dtk[:, tl, :], p[:, 8:16], reads=[pk], writes=['ssdtk'])
                inb = [sb('ssin%d' % i, [128, TB + 3]) for i in range(2)]
                acc = [sb('ssacc%d' % i, [128, TB]) for i in range(2)]
                xcb = [sb('ssxc%d' % i, [128, TB]) for i in range(2)]
                it = 0
                for tb in range(NB):
                    k.checkpoint()
                    t0 = tb * TB
                    tsl = slice(t0, t0 + TB)
                    for c8 in range(8):
                        it += 1
                        b = it % 2
                        ik, ak, xk = 'ssin%d' % b, 'ssacc%d' % b, 'ssxc%d' % b
                        mt = 18 + c8
                        if tb == 0:
                            k.op('pool', 'memset', inb[b][:, 0:3], 0.0, writes=[ik])
                            k.dma('sp', inb[b][:, 3:TB + 3], pT[mt, :, 0:TB], reads=['projT', ik], writes=[ik])
                        else:
                            k.dma('sp', inb[b][:], pT[mt, :, t0 - 3:t0 + TB], reads=['projT'], writes=[ik])
                        k.op('dve', 'tensor_scalar', acc[b][:], inb[b][:, 3:TB + 3], cw[:, c8, 3:4], None, ALU.mult, reads=[ik, 'sscw', ak], writes=[ak])
                        for w in range(3):
                            k.op('dve', 'scalar_tensor_tensor', acc[b][:], inb[b][:, w:w + TB], cw[:, c8, w:w + 1], acc[b][:], ALU.mult, ALU.add,
                                 reads=[ik, 'sscw', ak], writes=[ak])
                        if c8 < 4:
                            k.op('act', 'activation', xcb[b][:], acc[b][:], AF.Silu, bias=cb[:, c8:c8 + 1], reads=[ak, 'sscb', xk], writes=[xk])
                            k.dma('act', xc_d[c8, :, tsl], xcb[b][:], reads=[xk], writes=['xc_d'])
                            for j in range(TPB):
                                tl = tb * TPB + j
                                p, pk = self.ps[2 + (it + j) % 2], 'ps%d' % (2 + (it + j) % 2)
                                k.op('pe', 'transpose', p[:, 0:128], xcb[b][:, j * 128:(j + 1) * 128], self.ident[:], reads=[xk, 'ident'], writes=[pk])
                                k.op('dve', 'tensor_tensor', xdt[:, tl, c8 * 128:(c8 + 1) * 128].rearrange("p (a b) -> p a b", a=2),
                                     p[:, 0:128].rearrange("p (a b) -> p a b", a=2),
                                     dtk[:, tl, 2 * c8:2 * c8 + 2].unsqueeze(2).to_broadcast([128, 2, 64]), ALU.mult, reads=[pk, 'ssdtk'], writes=['ssxdt'])
                        else:
                            dst = BT if c8 < 6 else CT
                            k.op('act', 'activation', dst[:, c8 % 2, tsl], acc[b][:], AF.Silu, bias=cb[:, c8:c8 + 1], reads=[ak, 'sscb'], writes=['ssBT' if c8 < 6 else 'ssCT'])
                k.barrier()
            with ExitStack() as st:
                sb = lambda n, s, d=F32: self.sb(st, n, s, d)
                csb = [sb('sscsb%d' % i, [128, TB]) for i in range(2)]
                dec = [sb('ssdec%d' % i, [128, TB]) for i in range(2)]
                Gm = [sb('ssG%d' % i, [128, TB], BF16) for i in range(2)]
                YO = [sb('ssY%d' % i, [64, TB]) for i in range(2)]
                it = 0
                hq = 0
                for h in range(8):
                    g = h // 4
                    for qb in range(NB):
                        k.checkpoint()
                        hq += 1
                        u = hq % 2
                        qsl = slice(qb * TB, (qb + 1) * TB)
                        pc, pck = self.ps[6 + u], 'ps%d' % (6 + u)
                        pb_, pbk = self.ps[4 + u], 'ps%d' % (4 + u)
                        k.op('pe', 'matmul', pb_[:, 0:TB], self.ident[0:8, h:h + 1].to_broadcast([8, 128]), cs[0:8, qsl], start=True, stop=True,
                             reads=['ident', 'sscs'], writes=[pbk])
                        k.op('act', 'activation', csb[u][:], pb_[:, 0:TB], AF.Copy, reads=[pbk], writes=['sscsb%d' % u])
                        nk = (qb + 1) * TPB
                        for kt in range(nk):
                            it += 1
                            b = it % 2
                            m = kt - qb * TPB
                            pz, pzk = self.ps[b], 'ps%d' % b
                            ksl = slice(kt * 128, (kt + 1) * 128)
                            k.op('pe', 'matmul', pz[:, 0:TB], BT[:, g, ksl], CT[:, g, qsl], start=True, stop=True, reads=['ssBT', 'ssCT'], writes=[pzk])
                            k.op('dve', 'tensor_scalar', dec[b][:], csb[u][:], ncsT[:, kt, h:h + 1], 0.0, ALU.add, ALU.min, reads=['sscsb%d' % u, 'ssncsT', 'ssdec%d' % b], writes=['ssdec%d' % b])
                            k.op('act', 'activation', dec[b][:], dec[b][:], AF.Exp, reads=['ssdec%d' % b], writes=['ssdec%d' % b])
                            if m >= 0:
                                k.op('pool', 'affine_select', dec[b][:], dec[b][:], pattern=[[1, TB]], compare_op=ALU.is_ge, fill=0.0,
                                     base=-128 * m, channel_multiplier=-1, reads=['ssdec%d' % b], writes=['ssdec%d' % b])
                            k.op('dve', 'tensor_tensor', Gm[b][:], pz[:, 0:TB], dec[b][:], ALU.mult, reads=[pzk, 'ssdec%d' % b, 'ssG%d' % b], writes=['ssG%d' % b])
                            k.op('pe', 'matmul', pc[0:64, 0:TB], xdt[:, kt, h * 64:(h + 1) * 64], Gm[b][:], start=(kt == 0), stop=(kt == nk - 1),
                                 reads=['ssxdt', 'ssG%d' % b], writes=[pck])
                        k.op('dve', 'tensor_copy', YO[u][:], pc[0:64, 0:TB], reads=[pck, 'ssY%d' % u], writes=['ssY%d' % u])
                        k.dma('sp', ys_d[h, :, qsl], YO[u][:], reads=['ssY%d' % u], writes=['ys_d'])
                k.barrier()
        with ExitStack() as st:
            sb = lambda n, s, d=F32: self.sb(st, n, s, d)
            dcol = sb('ssdcol', [128, 4])
            for hp in range(2):
                k.dma('sp', dcol[64 * hp:64 * hp + 64, :].rearrange("p (o n) -> p o n", o=1),
                      I['m2_d'].rearrange("o (ft hp) -> hp o ft", hp=2)[hp].partition_broadcast(64), writes=['ssdcol'], allow_slow_non_contiguous=True)
            nw = sb('ssnw', [128, 4])
            k.dma('sp', nw[:], I['m2_norm_w'].rearrange("o (mt p) -> p (o mt)", p=128), writes=['ssnw'], allow_slow_non_contiguous=True)
            Y = [sb('spy%d' % i, [128, TB]) for i in range(4)]
            X = [sb('spx%d' % i, [128, TB]) for i in range(2)]
            Z = [sb('spz%d' % i, [128, TB]) for i in range(2)]
            Q = [sb('spq%d' % i, [128, TB]) for i in range(2)]
            O = [sb('spo%d' % i, [128, TB], BF16) for i in range(2)]
            ysv = ys_d.rearrange("(ft hp) d t -> (hp d) ft t", hp=2)
            it = 0
            for tb in range(NB):
                k.checkpoint()
                tsl = slice(tb * TB, (tb + 1) * TB)
                for g in range(2):
                    p, pk = self.ps[(tb * 2 + g) % 2], 'ps%d' % ((tb * 2 + g) % 2)
                    for f2 in range(2):
                        ft = 2 * g + f2
                        it += 1
                        b = it % 2
                        yk, xk, zk, qk = 'spy%d' % ft, 'spx%d' % b, 'spz%d' % b, 'spq%d' % b
                        k.dma('sp', Y[ft][:], ysv[:, ft, tsl], reads=['ys_d'], writes=[yk])
                        k.dma('act', X[b][:], xc_d[ft, :, tsl], reads=['xc_d'], writes=[xk])
                        k.dma('sp', Z[b][:], pT[14 + ft, :, tsl], reads=['projT'], writes=[zk])
                        k.op('dve', 'scalar_tensor_tensor', Y[ft][:], X[b][:], dcol[:, ft:ft + 1], Y[ft][:], ALU.mult, ALU.add, reads=[xk, 'ssdcol', yk], writes=[yk])
                        k.op('act', 'activation', Z[b][:], Z[b][:], AF.Silu, reads=[zk], writes=[zk])
                        k.op('dve', 'tensor_tensor', Y[ft][:], Y[ft][:], Z[b][:], ALU.mult, reads=[yk, zk], writes=[yk])
                        k.op('pool', 'tensor_tensor', Q[b][:], Y[ft][:], Y[ft][:], ALU.mult, reads=[yk, qk], writes=[qk])
                        k.op('pe', 'matmul', p[:, 0:TB], self.ones[:], Q[b][:], start=(f2 == 0), stop=(f2 == 1), reads=['ones', qk], writes=[pk])
                    k.op('dve', 'tensor_scalar', Q[0][:], p[:, 0:TB], 1.0 / 256, 1e-6, ALU.mult, ALU.add, reads=[pk, 'spq0'], writes=['spq0'])
                    k.op('act', 'activation', Q[0][:], Q[0][:], AF.Sqrt, reads=['spq0'], writes=['spq0'])
                    k.op('dve', 'reciprocal', Q[0][:], Q[0][:], reads=['spq0'], writes=['spq0'])
                    for f2 in range(2):
                        ft = 2 * g + f2
                        ok = 'spo%d' % f2
                        k.op('dve', 'scalar_tensor_tensor', O[f2][:], Y[ft][:], nw[:, ft:ft + 1], Q[0][:], ALU.mult, ALU.mult,
                             reads=['spy%d' % ft, 'ssnw', 'spq0', ok], writes=[ok])
                        k.dma('act', yms_d[ft, :, tsl], O[f2][:], reads=[ok], writes=['yms_d'])
            k.barrier()

    def moe(self, li, xin, xout, final=False):
        k, nc, T, NT, TB = self.k, self.nc, self.T, self.NT, self.TB
        li = 0 if self.lsel is not None else li
        I = self.I
        HT = min(1024, T)
        NH = T // HT
        HTT = HT // 128
        HNB = HT // TB
        TPB = self.TPB
        gA, gB, g2 = self.modbc[:, 4 * D:5 * D], self.modbc[:, 3 * D:4 * D], self.modbc[:, 5 * D:6 * D]
        with ExitStack() as st:
            sb = lambda n, s, d=F32: self.sb(st, n, s, d)
            wr = sb('wr', [128, 8, 36])
            k.dma('sp', wr[:, :, 0:4], I['moe_w_grp'][li].rearrange("(kc k) n -> k kc n", k=128), writes=['wr'], allow_slow_non_contiguous=True)
            k.dma('sp', wr[:, :, 4:36], I['moe_w_exp'][li].rearrange("(kc k) n -> k kc n", k=128), reads=['wr'], writes=['wr'], allow_slow_non_contiguous=True)
            br = sb('br', [1, 36])
            k.dma('sp', br[:, 0:4], I['moe_b_grp'][li:li + 1, :], writes=['br'])
            k.dma('sp', br[:, 4:36], I['moe_b_exp'][li:li + 1, :], reads=['br'], writes=['br'])
            hT = sb('mhT', [128, 8, HT], BF16)
            GT = sb('mGT', [32, HT])
            acc = sb('macc', [128, HTT, D])
            bufs = self.norm_bufs(st)
            hT32 = sb('hT32', [128, 8, 128])
            R = {n: sb('rt_' + n, [128, 32]) for n in ('lg', 'e', 'oh', 'x', 'oh1', 'oh2', 'G')}
            S = {n: sb('rs_' + n, [128, 4]) for n in ('m', 's', 'm1', 'm2', 'g1', 'g2')}
            Wg = [sb('mWg%d' % i, [128, 8, 512], BF16) for i in range(2)]
            Wu = [sb('mWu%d' % i, [128, 8, 512], BF16) for i in range(2)]
            Wd = [sb('mWd%d' % i, [128, 4, 1024], BF16) for i in range(2)]
            gb = [sb('mgb%d' % i, [128, TB]) for i in range(2)]
            sl = [sb('msl%d' % i, [128, TB]) for i in range(2)]
            a1 = [sb('ma1%d' % i, [128, TB]) for i in range(2)]
            aT = [sb('maT%d' % i, [128, 4, TB], BF16) for i in range(2)]
            xo = [sb('mxo%d' % i, [128, D]) for i in range(2)]
            fw = None
            if final:
                fw = sb('finw', [128, D])
                k.dma('sp', fw[:], I['final_norm_w'].partition_broadcast(128).rearrange("p o n -> p (o n)"), writes=['finw'])
            RK = ['route']
            for half in range(NH):
                t0 = half * HTT
                for j in range(HTT):
                    k.checkpoint()
                    tile = t0 + j
                    if DBG['norm'] >= 0:
                        self.norm_tile(bufs, xin, tile, gA, gB, hT[:, :, j * 128:(j + 1) * 128], hT32_dst=hT32, keys=['mhT'])
                    lvl = getattr(self, 'moe_level', 3)
                    if lvl <= -4:
                        continue
                    p, pk = self.ps[4], 'ps4'
                    for kc in range(8):
                        k.op('pe', 'matmul', p[:, 0:36], hT32[:, kc, :], wr[:, kc, :], start=(kc == 0), stop=False, reads=['hT32', 'wr'], writes=[pk])
                    k.op('pe', 'matmul', p[:, 0:36], self.ones[0:1, :], br[0:1, :], start=False, stop=True, reads=['ones', 'br'], writes=[pk])

                    def o(eng, f, *a, **kw):
                        k.op(eng, f, *a, reads=RK, writes=RK, **kw)
                    k.op('dve', 'tensor_copy', R['lg'][:, 0:4], p[:, 0:4], reads=[pk] + RK, writes=RK)
                    k.op('dve', 'tensor_copy', R['x'][:], p[:, 4:36], reads=[pk] + RK, writes=RK)
                    if lvl <= -3:
                        continue
                    lg = R['lg'][:, 0:4]
                    o('dve', 'tensor_reduce', S['m'][:, 0:1], lg, AX.X, ALU.max)
                    o('dve', 'tensor_scalar', R['oh'][:, 0:4], lg, S['m'][:, 0:1], None, ALU.is_equal)
                    o('dve', 'tensor_scalar', R['e'][:, 0:4], lg, S['m'][:, 0:1], None, ALU.subtract)
                    o('act', 'activation', R['e'][:, 0:4], R['e'][:, 0:4], AF.Exp)
                    o('dve', 'tensor_reduce', S['s'][:, 0:1], R['e'][:, 0:4], AX.X, ALU.add)
                    o('dve', 'reciprocal', S['s'][:, 0:1], S['s'][:, 0:1])
                    o('dve', 'tensor_scalar', R['oh'][:, 0:4], R['oh'][:, 0:4], -1.0, 1e30, ALU.add, ALU.mult)
                    o('dve', 'tensor_tensor', R['x'][:].rearrange("p (g e) -> p g e", g=4), R['x'][:].rearrange("p (g e) -> p g e", g=4),
                      R['oh'][:, 0:4].unsqueeze(2).to_broadcast([128, 4, 8]), ALU.add)
                    o('dve', 'tensor_reduce', S['m1'][:, 0:1], R['x'][:], AX.X, ALU.max)
                    o('dve', 'tensor_scalar', R['oh1'][:], R['x'][:], S['m1'][:, 0:1], None, ALU.is_equal)
                    o('dve', 'scalar_tensor_tensor', R['e'][:], R['oh1'][:], -1e30, R['x'][:], ALU.mult, ALU.add)
                    o('dve', 'tensor_reduce', S['m2'][:, 0:1], R['e'][:], AX.X, ALU.max)
                    o('dve', 'tensor_scalar', R['oh2'][:], R['e'][:], S['m2'][:, 0:1], None, ALU.is_equal)
                    o('dve', 'tensor_tensor', S['g1'][:, 0:1], S['m2'][:, 0:1], S['m1'][:, 0:1], ALU.subtract)
                    o('act', 'activation', S['g1'][:, 0:1], S['g1'][:, 0:1], AF.Exp)
                    o('dve', 'tensor_scalar', S['g1'][:, 0:1], S['g1'][:, 0:1], 1.0, None, ALU.add)
                    o('dve', 'reciprocal', S['g1'][:, 0:1], S['g1'][:, 0:1])
                    o('dve', 'tensor_scalar', S['g2'][:, 0:1], S['g1'][:, 0:1], -1.0, 1.0, ALU.mult, ALU.add)
                    o('dve', 'tensor_tensor', S['g1'][:, 0:1], S['g1'][:, 0:1], S['s'][:, 0:1], ALU.mult)
                    o('dve', 'tensor_tensor', S['g2'][:, 0:1], S['g2'][:, 0:1], S['s'][:, 0:1], ALU.mult)
                    o('dve', 'tensor_scalar', R['G'][:], R['oh1'][:], S['g1'][:, 0:1], None, ALU.mult)
                    o('dve', 'scalar_tensor_tensor', R['G'][:], R['oh2'][:], S['g2'][:, 0:1], R['G'][:], ALU.mult, ALU.add)
                    if lvl <= -2:
                        continue
                    p5, p5k = self.ps[5], 'ps5'
                    k.op('pe', 'transpose', p5[0:32, 0:128], R['G'][:], self.ident[:], reads=RK + ['ident'], writes=[p5k])
                    k.op('act', 'activation', GT[:, j * 128:(j + 1) * 128], p5[0:32, 0:128], AF.Copy, reads=[p5k], writes=['mGT'])
                if 'gates' in self.dbg:
                    k.dma('sp', self.dbg_out['gates'][:, half * HT:(half + 1) * HT], GT[:], reads=['mGT'], writes=['dbgg'])
                lvl = getattr(self, 'moe_level', 3)
                if lvl < 3 or self.nexp == 0:
                    k.op('dve', 'memset', acc[:], 0.0, writes=['macc'])
                for e in range(self.nexp):
                    k.checkpoint()
                    b = e % 2
                    if lvl < 1:
                        continue
                    wk = 'mW%d' % b
                    k.dma('pool', Wg[b][:], I['moe_w_gate'][li, e].rearrange("(kc k) n -> k kc n", k=128), writes=[wk + 'g'])
                    k.dma('pool', Wu[b][:], I['moe_w_up'][li, e].rearrange("(kc k) n -> k kc n", k=128), writes=[wk + 'u'])
                    k.dma('pool', Wd[b][:], I['moe_w_down'][li, e].rearrange("(kc k) n -> k kc n", k=128), writes=[wk + 'd'])
                    for tb in range(HNB):
                        if lvl < 2:
                            continue
                        self.uid += 1
                        u = self.uid % 2
                        tsl = slice(tb * TB, (tb + 1) * TB)
                        pg_, pgk = self.ps[6], 'ps6'
                        k.op('pe', 'matmul', pg_[:, 0:TB], self.ident[0:32, e:e + 1].to_broadcast([32, 128]), GT[:, tsl], start=True, stop=True,
                             reads=['ident', 'mGT'], writes=[pgk])
                        k.op('act', 'activation', gb[u][:], pg_[:, 0:TB], AF.Copy, reads=[pgk], writes=['mgb%d' % u])
                        for hm in range(4):
                            v = hm % 2
                            p1, p1k, p2, p2k = self.ps[v], 'ps%d' % v, self.ps[2 + v], 'ps%d' % (2 + v)
                            for kc in range(8):
                                k.op('pe', 'matmul', p1[:, 0:TB], Wg[b][:, kc, hm * 128:(hm + 1) * 128], hT[:, kc, tsl],
                                     start=(kc == 0), stop=(kc == 7), reads=[wk + 'g', 'mhT'], writes=[p1k])
                            for kc in range(8):
                                k.op('pe', 'matmul', p2[:, 0:TB], Wu[b][:, kc, hm * 128:(hm + 1) * 128], hT[:, kc, tsl],
                                     start=(kc == 0), stop=(kc == 7), reads=[wk + 'u', 'mhT'], writes=[p2k])
                            k.op('act', 'activation', sl[v][:], p1[:, 0:TB], AF.Silu, reads=[p1k], writes=['msl%d' % v])
                            k.op('dve', 'tensor_tensor', a1[v][:], p2[:, 0:TB], sl[v][:], ALU.mult, reads=[p2k, 'msl%d' % v], writes=['ma1%d' % v])
                            k.op('pool', 'tensor_tensor', aT[u][:, hm, :], a1[v][:], gb[u][:], ALU.mult, reads=['ma1%d' % v, 'mgb%d' % u], writes=['maT%d' % u])
                        for j in range(TPB):
                            if lvl < 3:
                                continue
                            tl = tb * TPB + j
                            for nh in range(2):
                                self.uid += 1
                                pi = 4 + self.uid % 2
                                p, pk = self.ps[pi], 'ps%d' % pi
                                for hm in range(4):
                                    k.op('pe', 'matmul', p[:], aT[u][:, hm, j * 128:(j + 1) * 128], Wd[b][:, hm, nh * 512:(nh + 1) * 512],
                                         start=(hm == 0), stop=(hm == 3), reads=['maT%d' % u, wk + 'd'], writes=[pk])
                                dst = acc[:, tl, nh * 512:(nh + 1) * 512]
                                if e == 0:
                                    k.op('dve', 'tensor_copy', dst, p[:], reads=[pk], writes=['macc'])
                                else:
                                    k.op('dve', 'tensor_tensor', dst, dst, p[:], ALU.add, reads=[pk, 'macc'], writes=['macc'])
                for j in range(HTT):
                    k.checkpoint()
                    tile = t0 + j
                    b = j % 2
                    xk = 'mxo%d' % b
                    k.dma('sp', xo[b][:], xin[tile * 128:(tile + 1) * 128, :], reads=['xres'], writes=[xk])
                    k.op('dve', 'tensor_tensor', acc[:, j, :], acc[:, j, :], g2, ALU.mult, reads=['macc', 'modbc'], writes=['macc'])
                    k.op('pool', 'tensor_tensor', xo[b][:], xo[b][:], acc[:, j, :], ALU.add, reads=['macc', xk], writes=[xk])
                    if final:
                        sq, ss = bufs['sq'], bufs['ss']
                        c = ss[:, 0:1]
                        k.op('dve', 'tensor_tensor', sq[:], xo[b][:], xo[b][:], ALU.mult, reads=[xk, 'sq'], writes=['sq'])
                        k.op('dve', 'tensor_reduce', c, sq[:], AX.X, ALU.add, reads=['sq', 'ss'], writes=['ss'])
                        k.op('dve', 'tensor_scalar', c, c, 1.0 / D, 1e-6, ALU.mult, ALU.add, reads=['ss'], writes=['ss'])
                        k.op('act', 'activation', c, c, AF.Sqrt, reads=['ss'], writes=['ss'])
                        k.op('dve', 'reciprocal', c, c, reads=['ss'], writes=['ss'])
                        k.op('dve', 'scalar_tensor_tensor', xo[b][:], xo[b][:], c, fw[:], ALU.mult, ALU.mult, reads=[xk, 'ss', 'finw'], writes=[xk])
                    k.dma('sp', xout[tile * 128:(tile + 1) * 128, :], xo[b][:], reads=[xk], writes=['xres2'])
            k.barrier()


def build(T, dbg=(), nlayers=2, nexp=32, stages=('mix0', 'moe0', 'mix1', 'moe1'), moe_level=3, lsel=None):
    m = Mod(T, dbg, nlayers, nexp, lsel)
    m.moe_level = moe_level
    k = m.k
    x1 = m.scr('x1', [T, D])
    x2 = m.scr('x2', [T, D])
    x3 = m.scr('x3', [T, D])
    for n, shp in (('x1', [T, D]), ('x2', [T, D]), ('gates', [32, T]), ('x3', [T, D])):
        m.tap(n, shp)
    with ExitStack() as st:
        m.consts(st)
        k.barrier()
        last = stages[-1]
        if 'consts' in stages:
            k.dma('sp', m.out[0:128, 0:128], m.negtri[:], reads=['negtri'], writes=['o'])
            k.dma('sp', m.out[0:128, 128:256], m.ident[:], reads=['ident'], writes=['o'])
            k.dma('sp', m.out[0:128, 256:384], m.blk[:], reads=['blk'], writes=['o'])
            k.finish()
            return m
        m.adaln(0)
        for _ in range(DBG.get('rep', 0)):
            m.adaln(0)
        if 'adaln' in stages:
            for i in range(min(6, T // 128)):
                k.dma('sp', m.out[i * 128:(i + 1) * 128, :], m.modbc[:, i * D:(i + 1) * D], reads=['modbc'], writes=['o'])
            k.finish()
            return m
        if 'mix0' in stages:
            m.even_mixer(m.I['x'], m.dbg_out.get('x1', m.out if last == 'mix0' else x1))
        if 'moe0' in stages:
            src = x1 if 'mix0' in stages and 'x1' not in m.dbg_out else (m.dbg_out.get('x1') if 'mix0' in stages else m.I['x'])
            m.moe(0, src, m.out if last == 'moe0' else x2, final=(last == 'moe0' and nlayers == 1))
        if 'mix1' in stages or 'moe1' in stages:
            m.adaln(1)
        if 'mix1' in stages:
            src = x2 if 'moe0' in stages else m.I['x']
            m.odd_mixer(src, m.out if last == 'mix1' else x3)
        if 'moe1' in stages:
            src = x3 if 'mix1' in stages else m.I['x']
            m.moe(1, src, m.out, final=True)
        k.finish()
    return m


PER_LAYER = ('ada_w', 'ada_b', 'norm_mix_w', 'norm_ffn_w', 'moe_w_grp', 'moe_b_grp', 'moe_w_exp', 'moe_b_exp',
             'moe_w_gate', 'moe_w_up', 'moe_w_down')


def _in_maps(inputs, xs, layer):
    in_maps = []
    for b in range(8):
        d = {}
        for name in INPUT_SHAPES:
            if name == 'x':
                a = xs[b]
            else:
                a = np.asarray(inputs[name], dtype=np.float32)
                if name == 'c':
                    a = a[b:b + 1]
                elif name == 'final_norm_w':
                    a = a.reshape(1, D)
                elif name in PER_LAYER:
                    a = a[layer:layer + 1]
            d[name] = np.ascontiguousarray(a, dtype=np.float32)
        in_maps.append(d)
    return in_maps


def kernel(**inputs):
    T = 4096
    xs = np.asarray(inputs['x'], dtype=np.float32)
    m = build(T)
    in_maps = []
    for b in range(8):
        d = {}
        for name in INPUT_SHAPES:
            if name == 'x':
                a = xs[b]
            else:
                a = np.asarray(inputs[name], dtype=np.float32)
                if name == 'c':
                    a = a[b:b + 1]
                elif name == 'final_norm_w':
                    a = a.reshape(1, D)
            d[name] = np.ascontiguousarray(a, dtype=np.float32)
        in_maps.append(d)
    res = run_bass_kernel_spmd(m.nc, in_maps, core_ids=list(range(8)))
    return np.stack([np.asarray(r['out'], dtype=np.float32) for r in res.results])
```
